# Optimizing a Trainium2 kernel written in Bass

```python
import math
import jax
import jax.numpy as jnp
from jax import lax
import numpy as np

D_MODEL = 2048
BATCH = 2
SEQ = 4096
DEPTH = 2

GRID_W = 64
CTX_LEN = 256
N_BRANCH = 4
BRANCH_W = D_MODEL // 4
S5_GROUP_CH = 16
S5_GROUPS = BRANCH_W // S5_GROUP_CH
S5_STATE = 64
GQA_HEAD_DIM = 128
GQA_HEADS = BRANCH_W // GQA_HEAD_DIM
GQA_KV_HEADS = GQA_HEADS // 2
RET_HEADS = 4
RET_V_DIM = BRANCH_W // RET_HEADS
RET_QK_DIM = RET_V_DIM // 2
RET_CHUNK = 128
MLA_HEADS = 4
MLA_Q_LORA = BRANCH_W
MLA_KV_LORA = BRANCH_W // 2
MLA_NOPE_DIM = 128
MLA_ROPE_DIM = 64
MLA_V_DIM = BRANCH_W // MLA_HEADS
N_EXPERTS = 16
EXPERT_FF = D_MODEL // 2
EC_CAPACITY_FACTOR = 2
Q_BLOCK = 128
ROPE_BASE = 10000.0
NORM_EPS = 1e-6
DEEPNORM_ALPHA = (2 * DEPTH) ** 0.25
DEEPNORM_BETA = (8 * DEPTH) ** -0.25
IN_WIDTHS = (
    BRANCH_W,
    GQA_HEADS * GQA_HEAD_DIM, GQA_KV_HEADS * GQA_HEAD_DIM, GQA_KV_HEADS * GQA_HEAD_DIM,
    RET_HEADS * RET_QK_DIM, RET_HEADS * RET_QK_DIM, RET_HEADS * RET_V_DIM, RET_HEADS * RET_V_DIM,
    MLA_Q_LORA, MLA_KV_LORA, MLA_ROPE_DIM,
)
IN_TOTAL = sum(IN_WIDTHS)
IN_OFFSETS = tuple(int(v) for v in np.cumsum(IN_WIDTHS)[:-1])
F32 = jnp.float32

kernel_name = 'hybrid_s5_gqa_retention_mla_ecmoe_dit'


def standardize(x):
    xf = x.astype(F32)
    xc = xf - jnp.mean(xf, -1, keepdims=True)
    return xc * lax.rsqrt(jnp.mean(xc * xc, -1, keepdims=True) + NORM_EPS)


def layer_norm(x, g, b):
    return (standardize(x) * g + b).astype(x.dtype)


def modulate(x, shift, scale):
    return (standardize(x) * (1.0 + scale) + shift).astype(x.dtype)


def rms_norm(x, g):
    xf = x.astype(F32)
    return (xf * lax.rsqrt(jnp.mean(xf * xf, -1, keepdims=True) + NORM_EPS) * g).astype(x.dtype)


def rope_1d(x, pos):
    n = x.shape[-1] // 2
    inv = ROPE_BASE ** (-jnp.arange(n, dtype=F32) / n)
    ang = pos.astype(F32)[:, None] * inv[None, :]
    cos, sin = jnp.cos(ang), jnp.sin(ang)
    xf = x.astype(F32)
    x1, x2 = xf[..., :n], xf[..., n:]
    return jnp.concatenate([x1 * cos - x2 * sin, x1 * sin + x2 * cos], -1)


def axial_rope(x, row, col):
    half = x.shape[-1] // 2
    return jnp.concatenate([rope_1d(x[..., :half], row), rope_1d(x[..., half:], col)], -1).astype(x.dtype)


def split_heads(t, n_heads):
    b, n, _ = t.shape
    return t.reshape(b, n, n_heads, -1).transpose(0, 2, 1, 3)


def merge_heads(t):
    b, h, n, d = t.shape
    return t.transpose(0, 2, 1, 3).reshape(b, n, h * d)


def block_attention(q, k, v, scale):
    b, hk, g, n, dh = q.shape
    nb = n // Q_BLOCK
    qb = jnp.moveaxis(q.reshape(b, hk, g, nb, Q_BLOCK, dh), 3, 0)

    def one_block(qi):
        s = jnp.einsum('bhgqd,bhkd->bhgqk', qi, k).astype(F32) * scale
        p = jax.nn.softmax(s, axis=-1)
        return jnp.einsum('bhgqk,bhkd->bhgqd', p.astype(v.dtype), v)

    o = lax.map(one_block, qb)
    return jnp.moveaxis(o, 0, 3).reshape(b, hk, g, n, v.shape[-1])


def s5_discretise(a_re, a_im, log_dt, b_re, b_im):
    dt = jnp.exp(log_dt.astype(F32))[:, None]
    a_re = a_re.astype(F32)
    a_im = a_im.astype(F32)
    mag = jnp.exp(a_re * dt)
    ab_re, ab_im = mag * jnp.cos(a_im * dt), mag * jnp.sin(a_im * dt)
    den = a_re * a_re + a_im * a_im
    num_re, num_im = ab_re - 1.0, ab_im
    coef_re = (num_re * a_re + num_im * a_im) / den
    coef_im = (num_im * a_re - num_re * a_im) / den
    b_re = b_re.astype(F32)
    b_im = b_im.astype(F32)
    bb_re = coef_re[..., None] * b_re - coef_im[..., None] * b_im
    bb_im = coef_re[..., None] * b_im + coef_im[..., None] * b_re
    return ab_re, ab_im, bb_re, bb_im


def complex_affine_combine(left, right):
    la_re, la_im, lb_re, lb_im = left
    ra_re, ra_im, rb_re, rb_im = right
    return (la_re * ra_re - la_im * ra_im,
            la_re * ra_im + la_im * ra_re,
            ra_re * lb_re - ra_im * lb_im + rb_re,
            ra_re * lb_im + ra_im * lb_re + rb_im)


def s5_scan(u, ab_re, ab_im, bb_re, bb_im, h0_re, h0_im, reverse):
    if reverse:
        u = jnp.flip(u, 1)
    uf = u.astype(F32)
    b_re = jnp.einsum('bngi,gpi->bngp', uf, bb_re)
    b_im = jnp.einsum('bngi,gpi->bngp', uf, bb_im)
    b_re = b_re.at[:, 0].add(ab_re * h0_re - ab_im * h0_im)
    b_im = b_im.at[:, 0].add(ab_re * h0_im + ab_im * h0_re)
    a_re = jnp.broadcast_to(ab_re, b_re.shape)
    a_im = jnp.broadcast_to(ab_im, b_im.shape)
    _, _, h_re, h_im = lax.associative_scan(complex_affine_combine, (a_re, a_im, b_re, b_im), axis=1)
    if reverse:
        h_re, h_im = jnp.flip(h_re, 1), jnp.flip(h_im, 1)
    return h_re, h_im


def s5_readout(h_re, h_im, u, lp):
    b, n, w = u.shape
    y = (jnp.einsum('bngp,gip->bngi', h_re, lp['s5_c_re'].astype(F32))
         - jnp.einsum('bngp,gip->bngi', h_im, lp['s5_c_im'].astype(F32))).reshape(b, n, w)
    y = (y + lp['s5_d'].astype(F32) * u.astype(F32)).astype(u.dtype)
    z = jax.nn.gelu(y)
    return z * jax.nn.sigmoid(z @ lp['s5_w_glu'])


def s5_branch(u, uc, lp, need_ctx):
    b, n, _ = u.shape
    bc, m, _ = uc.shape
    ug = u.reshape(b, n, S5_GROUPS, S5_GROUP_CH)
    ucg = uc.reshape(bc, m, S5_GROUPS, S5_GROUP_CH)
    zero = jnp.zeros((bc, S5_GROUPS, S5_STATE), F32)
    lat_re = lat_im = ctx_re = ctx_im = 0.0
    for sfx, reverse in (('f', False), ('b', True)):
        ab_re, ab_im, bb_re, bb_im = s5_discretise(lp['s5_a_re_' + sfx], lp['s5_a_im_' + sfx],
                                                   lp['s5_log_dt_' + sfx], lp['s5_b_re'], lp['s5_b_im'])
        hc_re, hc_im = s5_scan(ucg, ab_re, ab_im, bb_re, bb_im, zero, zero, reverse)
        end = 0 if reverse else -1
        hl_re, hl_im = s5_scan(ug, ab_re, ab_im, bb_re, bb_im, hc_re[:, end], hc_im[:, end], reverse)
        lat_re = lat_re + hl_re
        lat_im = lat_im + hl_im
        if need_ctx:
            ctx_re = ctx_re + hc_re
            ctx_im = ctx_im + hc_im
    y = s5_readout(lat_re, lat_im, u, lp)
    yc = s5_readout(ctx_re, ctx_im, uc, lp) if need_ctx else None
    return y, yc


def gqa_branch(pl, pc, lp, row, col, need_ctx):
    q, k, v = pl
    qc, kc, vc = pc
    n_grp = GQA_HEADS // GQA_KV_HEADS
    scale = GQA_HEAD_DIM ** -0.5

    def attend(qh, kh, vh):
        b, _, n, d = qh.shape
        o = block_attention(qh.reshape(b, GQA_KV_HEADS, n_grp, n, d), kh, vh, scale)
        return merge_heads(o.reshape(b, GQA_HEADS, n, vh.shape[-1]))

    qn, kn = lp['gqa_q_norm'], lp['gqa_k_norm']
    k_ctx = rms_norm(split_heads(kc, GQA_KV_HEADS), kn)
    v_ctx = split_heads(vc, GQA_KV_HEADS)
    q_lat = axial_rope(rms_norm(split_heads(q, GQA_HEADS), qn), row, col)
    k_lat = axial_rope(rms_norm(split_heads(k, GQA_KV_HEADS), kn), row, col)
    v_lat = split_heads(v, GQA_KV_HEADS)
    y = attend(q_lat, jnp.concatenate([k_lat, k_ctx], 2), jnp.concatenate([v_lat, v_ctx], 2))
    yc = attend(rms_norm(split_heads(qc, GQA_HEADS), qn), k_ctx, v_ctx) if need_ctx else None
    return y, yc


def retention_chunkwise(q, k, v, log_gamma, s0, strict):
    b, h, n, _ = q.shape
    dv = v.shape[-1]
    nc = n // RET_CHUNK
    pos = jnp.arange(RET_CHUNK, dtype=F32)
    diff = pos[:, None] - pos[None, :]
    mask = (diff > 0) if strict else (diff >= 0)
    lg = log_gamma[:, None, None]
    decay_in = jnp.where(mask, jnp.exp(lg * jnp.where(mask, diff, 0.0)), 0.0)
    decay_q = jnp.exp(log_gamma[:, None] * (pos + 1.0))[..., None]
    decay_k = jnp.exp(log_gamma[:, None] * (RET_CHUNK - 1.0 - pos))[..., None]
    decay_c = jnp.exp(lg * RET_CHUNK)

    def chunks(t):
        return jnp.moveaxis(t.astype(F32).reshape(b, h, nc, RET_CHUNK, t.shape[-1]), 2, 0)

    def step(s, qkv):
        qi, ki, vi = qkv
        scores = jnp.einsum('bhqd,bhkd->bhqk', qi, ki) * decay_in
        o = (jnp.einsum('bhqk,bhkv->bhqv', scores, vi)
             + jnp.einsum('bhqd,bhdv->bhqv', qi * decay_q, s))
        s_new = s * decay_c + jnp.einsum('bhkd,bhkv->bhdv', ki * decay_k, vi)
        return s_new, o

    s_fin, o = lax.scan(step, s0, (chunks(q), chunks(k), chunks(v)))
    return jnp.moveaxis(o, 0, 2).reshape(b, h, n, dv), s_fin


def retention_state(k, v, log_gamma):
    n = k.shape[2]
    w = jnp.exp(log_gamma[:, None] * (n - 1.0 - jnp.arange(n, dtype=F32)))
    return jnp.einsum('bhnd,hn,bhnv->bhdv', k.astype(F32), w, v.astype(F32))


def retention_readout(o, g, gain):
    oc = o - jnp.mean(o, -1, keepdims=True)
    on = oc * lax.rsqrt(jnp.mean(oc * oc, -1, keepdims=True) + NORM_EPS)
    return (merge_heads(on) * gain * jax.nn.silu(g.astype(F32))).astype(g.dtype)


def flip_seq(t):
    return jnp.flip(t, 2)


def retention_branch(pl, pc, lp, row, col, need_ctx):
    q, k, v, g = pl
    qc, kc, vc, gc = pc
    sk = RET_QK_DIM ** -0.5
    lg_f = -jnp.exp(lp['ret_decay_f'].astype(F32))
    lg_b = -jnp.exp(lp['ret_decay_b'].astype(F32))
    q_lat = axial_rope(split_heads(q, RET_HEADS), row, col)
    k_lat = axial_rope(split_heads(k, RET_HEADS), row, col) * sk
    v_lat = split_heads(v, RET_HEADS)
    k_ctx = split_heads(kc, RET_HEADS) * sk
    v_ctx = split_heads(vc, RET_HEADS)
    if need_ctx:
        q_ctx = split_heads(qc, RET_HEADS)
        zero = jnp.zeros((qc.shape[0], RET_HEADS, RET_QK_DIM, RET_V_DIM), F32)
        oc_f, s_f = retention_chunkwise(q_ctx, k_ctx, v_ctx, lg_f, zero, False)
        oc_b, s_b = retention_chunkwise(flip_seq(q_ctx), flip_seq(k_ctx), flip_seq(v_ctx), lg_b, zero, True)
        yc = retention_readout(oc_f + flip_seq(oc_b), gc, lp['ret_norm'])
    else:
        s_f = retention_state(k_ctx, v_ctx, lg_f)
        s_b = retention_state(flip_seq(k_ctx), flip_seq(v_ctx), lg_b)
        yc = None
    o_f, _ = retention_chunkwise(q_lat, k_lat, v_lat, lg_f, s_f, False)
    o_b, _ = retention_chunkwise(flip_seq(q_lat), flip_seq(k_lat), flip_seq(v_lat), lg_b, s_b, True)
    y = retention_readout(o_f + flip_seq(o_b), g, lp['ret_norm'])
    return y, yc


def mla_branch(pl, pc, lp, row, col, need_ctx):
    cq, ckv, kr = pl
    cqc, ckvc, krc = pc
    scale = (MLA_NOPE_DIM + MLA_ROPE_DIM) ** -0.5

    def queries(c_q, rotate):
        qh = split_heads(rms_norm(c_q, lp['mla_q_norm']) @ lp['mla_w_uq'], MLA_HEADS)
        q_nope, q_rope = qh[..., :MLA_NOPE_DIM], qh[..., MLA_NOPE_DIM:]
        if rotate:
            q_rope = axial_rope(q_rope, row, col)
        return jnp.concatenate([q_nope, q_rope], -1)[:, :, None]

    def keys_values(c_kv, k_r, rotate):
        kvh = split_heads(rms_norm(c_kv, lp['mla_kv_norm']) @ lp['mla_w_ukv'], MLA_HEADS)
        k_nope, v = kvh[..., :MLA_NOPE_DIM], kvh[..., MLA_NOPE_DIM:]
        k_rope = k_r[:, None]
        if rotate:
            k_rope = axial_rope(k_rope, row, col)
        k_rope = jnp.broadcast_to(k_rope, k_nope.shape[:-1] + (MLA_ROPE_DIM,))
        return jnp.concatenate([k_nope, k_rope], -1), v

    k_ctx, v_ctx = keys_values(ckvc, krc, False)
    k_lat, v_lat = keys_values(ckv, kr, True)
    y = merge_heads(block_attention(queries(cq, True), jnp.concatenate([k_lat, k_ctx], 2),
                                    jnp.concatenate([v_lat, v_ctx], 2), scale)[:, :, 0])
    yc = merge_heads(block_attention(queries(cqc, False), k_ctx, v_ctx, scale)[:, :, 0]) if need_ctx else None
    return y, yc


def merge_branches(h, outs, lp):
    o = jnp.stack(outs, 0)
    proj = jnp.einsum('kbnw,kwd->bnkd', o, lp['w_branch'])
    gates = jax.nn.sigmoid(h @ lp['w_gate'] + lp['b_gate']).reshape(proj.shape)
    return jnp.sum(gates * proj, axis=2) @ lp['w_out']


def token_mixer(h, hc, row, col, lp, need_ctx):
    pl = jnp.split(h @ lp['w_in'], IN_OFFSETS, axis=-1)
    pc = jnp.split(hc @ lp['w_in'], IN_OFFSETS, axis=-1)
    o_s5, oc_s5 = s5_branch(pl[0], pc[0], lp, need_ctx)
    o_gqa, oc_gqa = gqa_branch(pl[1:4], pc[1:4], lp, row, col, need_ctx)
    o_ret, oc_ret = retention_branch(pl[4:8], pc[4:8], lp, row, col, need_ctx)
    o_mla, oc_mla = mla_branch(pl[8:11], pc[8:11], lp, row, col, need_ctx)
    y = merge_branches(h, (o_s5, o_gqa, o_ret, o_mla), lp)
    yc = merge_branches(hc, (oc_s5, oc_gqa, oc_ret, oc_mla), lp) if need_ctx else None
    return y, yc


def expert_choice_ffn(h, lp):
    b, n, d = h.shape
    cap = EC_CAPACITY_FACTOR * n // N_EXPERTS
    aff = jax.nn.softmax((h @ lp['router_w']).astype(F32), axis=-1)
    gate, idx = lax.top_k(jnp.swapaxes(aff, 1, 2), cap)
    xs = jax.vmap(lambda hb, ib: hb[ib])(h, idx)
    a = jnp.einsum('becd,edf->becf', xs, lp['moe_w_gate'])
    u = jnp.einsum('becd,edf->becf', xs, lp['moe_w_up'])
    y = jnp.einsum('becf,efd->becd', jax.nn.silu(a) * u, lp['moe_w_down']) * gate[..., None].astype(h.dtype)
    return jax.vmap(lambda yb, ib: jnp.zeros((n, d), yb.dtype).at[ib.reshape(-1)].add(yb.reshape(-1, d)))(y, idx)


def setup_inputs(seed: int = 0) -> dict:
    key = jax.random.key(seed)
    keys = iter(jax.random.split(key, 48))
    L, D = DEPTH, D_MODEL
    G, P = S5_GROUPS, S5_STATE

    def nrm(shape, std):
        return std * jax.random.normal(next(keys), shape, F32)

    s5_n = jnp.arange(P, dtype=F32)
    ret_h = jnp.arange(RET_HEADS, dtype=F32)
    ret_decay0 = jnp.log(-jnp.log(1.0 - 2.0 ** (-5.0 - ret_h)))
    dt_lo, dt_hi = math.log(1e-3), math.log(1e-1)
    qk_mla = MLA_NOPE_DIM + MLA_ROPE_DIM
    return {
        'x': nrm((BATCH, SEQ, D), 1.0),
        'c': nrm((BATCH, D), 1.0),
        'ctx': nrm((BATCH, CTX_LEN, D), 1.0),
        'c_ctx': nrm((D,), 1.0),
        'ada_w': nrm((L, D, 6 * D), D ** -0.5),
        'ada_b': nrm((L, 6 * D), 0.02),
        'w_in': nrm((L, D, IN_TOTAL), D ** -0.5),
        's5_a_re_f': -0.5 + nrm((L, G, P), 0.01),
        's5_a_im_f': jnp.pi * s5_n + nrm((L, G, P), 0.01),
        's5_log_dt_f': jax.random.uniform(next(keys), (L, G), F32, dt_lo, dt_hi),
        's5_a_re_b': -0.5 + nrm((L, G, P), 0.01),
        's5_a_im_b': jnp.pi * s5_n + nrm((L, G, P), 0.01),
        's5_log_dt_b': jax.random.uniform(next(keys), (L, G), F32, dt_lo, dt_hi),
        's5_b_re': nrm((L, G, P, S5_GROUP_CH), (2 * S5_GROUP_CH) ** -0.5),
        's5_b_im': nrm((L, G, P, S5_GROUP_CH), (2 * S5_GROUP_CH) ** -0.5),
        's5_c_re': nrm((L, G, S5_GROUP_CH, P), P ** -0.5),
        's5_c_im': nrm((L, G, S5_GROUP_CH, P), P ** -0.5),
        's5_d': nrm((L, BRANCH_W), 1.0),
        's5_w_glu': nrm((L, BRANCH_W, BRANCH_W), BRANCH_W ** -0.5),
        'gqa_q_norm': 1.0 + nrm((L, GQA_HEAD_DIM), 0.02),
        'gqa_k_norm': 1.0 + nrm((L, GQA_HEAD_DIM), 0.02),
        'ret_decay_f': ret_decay0 + nrm((L, RET_HEADS), 0.05),
        'ret_decay_b': ret_decay0 + nrm((L, RET_HEADS), 0.05),
        'ret_norm': 1.0 + nrm((L, RET_HEADS * RET_V_DIM), 0.02),
        'mla_q_norm': 1.0 + nrm((L, MLA_Q_LORA), 0.02),
        'mla_kv_norm': 1.0 + nrm((L, MLA_KV_LORA), 0.02),
        'mla_w_uq': nrm((L, MLA_Q_LORA, MLA_HEADS * qk_mla), MLA_Q_LORA ** -0.5),
        'mla_w_ukv': nrm((L, MLA_KV_LORA, MLA_HEADS * (MLA_NOPE_DIM + MLA_V_DIM)), MLA_KV_LORA ** -0.5),
        'w_branch': nrm((L, N_BRANCH, BRANCH_W, D), BRANCH_W ** -0.5),
        'w_gate': nrm((L, D, N_BRANCH * D), D ** -0.5),
        'b_gate': nrm((L, N_BRANCH * D), 0.02),
        'w_out': nrm((L, D, D), DEEPNORM_BETA * D ** -0.5),
        'ln1_g': 1.0 + nrm((L, D), 0.02),
        'ln1_b': nrm((L, D), 0.02),
        'router_w': nrm((L, D, N_EXPERTS), D ** -0.5),
        'moe_w_gate': nrm((L, N_EXPERTS, D, EXPERT_FF), D ** -0.5),
        'moe_w_up': nrm((L, N_EXPERTS, D, EXPERT_FF), D ** -0.5),
        'moe_w_down': nrm((L, N_EXPERTS, EXPERT_FF, D), DEEPNORM_BETA * EXPERT_FF ** -0.5),
        'ln2_g': 1.0 + nrm((L, D), 0.02),
        'ln2_b': nrm((L, D), 0.02),
    }


def reference(x, c, ctx, c_ctx, ada_w, ada_b, w_in,
              s5_a_re_f, s5_a_im_f, s5_log_dt_f, s5_a_re_b, s5_a_im_b, s5_log_dt_b,
              s5_b_re, s5_b_im, s5_c_re, s5_c_im, s5_d, s5_w_glu,
              gqa_q_norm, gqa_k_norm, ret_decay_f, ret_decay_b, ret_norm,
              mla_q_norm, mla_kv_norm, mla_w_uq, mla_w_ukv,
              w_branch, w_gate, b_gate, w_out, ln1_g, ln1_b,
              router_w, moe_w_gate, moe_w_up, moe_w_down, ln2_g, ln2_b):
    n_lat = x.shape[1]
    rows = n_lat // GRID_W
    row = jnp.repeat(jnp.arange(rows, dtype=jnp.int32), GRID_W)
    col = jnp.tile(jnp.arange(GRID_W, dtype=jnp.int32), rows)
    cx = ctx
    for l in range(DEPTH):
        need_ctx = l < DEPTH - 1
        lp = {
            'w_in': w_in[l],
            's5_a_re_f': s5_a_re_f[l], 's5_a_im_f': s5_a_im_f[l], 's5_log_dt_f': s5_log_dt_f[l],
            's5_a_re_b': s5_a_re_b[l], 's5_a_im_b': s5_a_im_b[l], 's5_log_dt_b': s5_log_dt_b[l],
            's5_b_re': s5_b_re[l], 's5_b_im': s5_b_im[l], 's5_c_re': s5_c_re[l], 's5_c_im': s5_c_im[l],
            's5_d': s5_d[l], 's5_w_glu': s5_w_glu[l],
            'gqa_q_norm': gqa_q_norm[l], 'gqa_k_norm': gqa_k_norm[l],
            'ret_decay_f': ret_decay_f[l], 'ret_decay_b': ret_decay_b[l], 'ret_norm': ret_norm[l],
            'mla_q_norm': mla_q_norm[l], 'mla_kv_norm': mla_kv_norm[l],
            'mla_w_uq': mla_w_uq[l], 'mla_w_ukv': mla_w_ukv[l],
            'w_branch': w_branch[l], 'w_gate': w_gate[l], 'b_gate': b_gate[l], 'w_out': w_out[l],
            'router_w': router_w[l], 'moe_w_gate': moe_w_gate[l], 'moe_w_up': moe_w_up[l],
            'moe_w_down': moe_w_down[l],
        }
        mod = jax.nn.silu(c) @ ada_w[l] + ada_b[l]
        mod_c = jax.nn.silu(c_ctx) @ ada_w[l] + ada_b[l]
        sh1, sc1, g1, sh2, sc2, g2 = jnp.split(mod[:, None, :], 6, axis=-1)
        shc1, scc1, gc1, shc2, scc2, gc2 = jnp.split(mod_c, 6)
        h = modulate(x, sh1, sc1)
        hc = modulate(cx, shc1, scc1)
        y, yc = token_mixer(h, hc, row, col, lp, need_ctx)
        x = layer_norm(DEEPNORM_ALPHA * x + g1 * y, ln1_g[l], ln1_b[l])
        h = modulate(x, sh2, sc2)
        x = layer_norm(DEEPNORM_ALPHA * x + g2 * expert_choice_ffn(h, lp), ln2_g[l], ln2_b[l])
        if need_ctx:
            cx = layer_norm(DEEPNORM_ALPHA * cx + gc1 * yc, ln1_g[l], ln1_b[l])
            hc = modulate(cx, shc2, scc2)
            cx = layer_norm(DEEPNORM_ALPHA * cx + gc2 * expert_choice_ffn(hc, lp), ln2_g[l], ln2_b[l])
    return x
```

```python
import numpy as np
import concourse.bass as bass
import concourse.mybir as mybir
from concourse.bass_utils import run_bass_kernel_spmd

F32 = mybir.dt.float32
BF16 = mybir.dt.bfloat16
I32 = mybir.dt.int32
U32 = mybir.dt.uint32
AF = mybir.ActivationFunctionType
ALU = mybir.AluOpType
AX = mybir.AxisListType

NCORES = 8


class V:
    __slots__ = ("t", "ap")

    def __init__(self, t, ap):
        self.t = t
        self.ap = ap

    def __getitem__(self, idx):
        return V(self.t, self.ap[idx])


class T:
    def __init__(self, ctx, name, shape, dtype, psum=False):
        nc = ctx.nc
        if psum:
            self.h = nc.alloc_psum_tensor(name, shape, dtype)
        else:
            self.h = nc.alloc_sbuf_tensor(name, shape, dtype)
        self.lw = None
        self.rd = {}
        self.name = name
        self.psum = psum

    def __getitem__(self, idx):
        return V(self, self.h[idx])

    def v(self, ap):
        return V(self, ap)


def _ap(x):
    if isinstance(x, V):
        return x.ap
    if isinstance(x, T):
        return x.h[:]
    return x


def _tl(x):
    if isinstance(x, V):
        return x.t
    if isinstance(x, T):
        return x
    return None


import os as _os


class Ctx:
    SAME_ENGINE_SYNC = _os.environ.get("MK_SAME_ENGINE_SYNC", "1") == "1"

    def __init__(self):
        self.nc = bass.Bass("TRN2", target_bir_lowering=False)
        nc = self.nc
        self.eng = {"pe": nc.tensor, "act": nc.scalar, "dve": nc.vector,
                    "pool": nc.gpsimd, "sp": nc.sync}
        self.sems = {}
        self.cnt = {}
        for e in ("pe", "act", "dve", "pool"):
            self.sems[e] = nc.alloc_semaphore("sem_" + e)
            self.cnt[e] = 0
        self.known = {e: {} for e in self.eng}
        self.dma_pool = {}
        self.dma_k = {}
        self.ntile = 0
        self.out_deps = []
        self.stream = {e: [] for e in self.eng}
        self.finalized = False

    def sb(self, shape, dtype, name=None):
        self.ntile += 1
        return T(self, name or f"t{self.ntile}", shape, dtype)

    def ps(self, shape, dtype=F32, name=None):
        self.ntile += 1
        return T(self, name or f"p{self.ntile}", shape, dtype, psum=True)

    def dram_in(self, name, shape, dtype=F32):
        return self.nc.dram_tensor(name, list(shape), dtype, kind="ExternalInput").ap()

    def dram_out(self, name, shape, dtype=F32):
        return self.nc.dram_tensor(name, list(shape), dtype, kind="ExternalOutput").ap()

    def _wait(self, e, deps):
        eng = self.eng[e]
        kn = self.known[e]
        best = {}
        for d in deps:
            if d is None:
                continue
            k, val = d
            if best.get(k, 0) < val:
                best[k] = val
        for k, val in best.items():
            if isinstance(k, str):
                if k == e and (e == "pe" or not self.SAME_ENGINE_SYNC):
                    continue
                sem = self.sems[k]
            else:
                sem = k
            if kn.get(k, 0) >= val:
                continue
            self.stream[e].append(("w", sem, val))
            kn[k] = val

    def _deps(self, outs, ins):
        deps = []
        for v in ins:
            t = _tl(v)
            if t is not None:
                deps.append(t.lw)
                if t.psum:
                    deps.extend(t.rd.items())
        for v in outs:
            t = _tl(v)
            if t is not None:
                deps.append(t.lw)
                deps.extend(t.rd.items())
        return deps

    def emit(self, e, outs, ins, f):
        self._wait(e, self._deps(outs, ins))
        self.cnt[e] += 1
        n = self.cnt[e]
        self.stream[e].append(("i", f, self.sems[e], 1))
        for v in ins:
            t = _tl(v)
            if t is not None:
                t.rd[e] = n
        for v in outs:
            t = _tl(v)
            if t is not None:
                t.lw = (e, n)
                t.rd = {}
        return None

    def dma(self, out, in_, q="sp", npool=24, **kw):
        self._wait(q, self._deps([out], [in_]))
        if q not in self.dma_pool:
            self.dma_pool[q] = [[self.nc.alloc_semaphore(f"dq_{q}_{i}"), 0] for i in range(npool)]
            self.dma_k[q] = 0
        pool = self.dma_pool[q]
        slot = pool[self.dma_k[q] % len(pool)]
        self.dma_k[q] += 1
        sem, val = slot
        if val > 0 and self.known[q].get(sem, 0) < val:
            self.stream[q].append(("w", sem, val))
            self.known[q][sem] = val
        slot[1] = val + 16
        o_ap, i_ap = _ap(out), _ap(in_)
        self.stream[q].append(("i", lambda g: g.dma_start(out=o_ap, in_=i_ap, **kw), sem, 16))
        if _tl(in_) is not None:
            _tl(in_).rd[sem] = val + 16
        if _tl(out) is not None:
            _tl(out).lw = (sem, val + 16)
            _tl(out).rd = {}
        else:
            self.out_deps.append((sem, val + 16))
        return None

    def finish(self, e="sp"):
        self._wait(e, self.out_deps)
        assert not self.finalized
        self.finalized = True
        streams = self.stream

        def replay(g, items):
            for it in items:
                if it[0] == "w":
                    g.wait_ge(it[1], it[2])
                else:
                    it[1](g).then_inc(it[2], it[3])

        with self.nc.Block() as block:
            @block.sync
            def _(g):
                replay(g, streams["sp"])

            @block.tensor
            def _(g):
                replay(g, streams["pe"])

            @block.vector
            def _(g):
                replay(g, streams["dve"])

            @block.scalar
            def _(g):
                replay(g, streams["act"])

            @block.gpsimd
            def _(g):
                replay(g, streams["pool"])

    def mm(self, out, lhsT, rhs, start=True, stop=True):
        return self.emit("pe", [out], [lhsT, rhs] + ([] if start else [out]),
                         lambda g: g.matmul(_ap(out), _ap(lhsT), _ap(rhs), start=start, stop=stop))

    def transpose(self, out, in_, ident):
        return self.emit("pe", [out], [in_, ident],
                         lambda g: g.transpose(_ap(out), _ap(in_), _ap(ident)))

    def act(self, out, in_, func, bias=None, scale=None, accum_out=None, e="act"):
        ins = [in_]
        kw = {}
        if bias is not None:
            kw["bias"] = _ap(bias)
            ins.append(bias)
        if scale is not None:
            kw["scale"] = _ap(scale)
            ins.append(scale)
        outs = [out]
        if accum_out is not None:
            kw["accum_out"] = _ap(accum_out)
            outs.append(accum_out)
        return self.emit(e, outs, ins, lambda g: g.activation(_ap(out), _ap(in_), func, **kw))

    def copy(self, out, in_, e="dve"):
        if e == "act":
            return self.emit(e, [out], [in_], lambda g: g.copy(_ap(out), _ap(in_)))
        return self.emit(e, [out], [in_], lambda g: g.tensor_copy(_ap(out), _ap(in_)))

    def tt(self, out, a, b, op, e="dve"):
        return self.emit(e, [out], [a, b], lambda g: g.tensor_tensor(_ap(out), _ap(a), _ap(b), op))

    def ts(self, out, a, s1, s2, op0, op1=None, e="dve", accum_out=None):
        ins = [a] + [s for s in (s1, s2) if isinstance(s, V)]
        outs = [out] + ([accum_out] if accum_out is not None else [])
        kw = {}
        if accum_out is not None:
            kw["accum_out"] = _ap(accum_out)
        if op1 is None:
            return self.emit(e, outs, ins, lambda g: g.tensor_scalar(_ap(out), _ap(a), _ap(s1), None, op0, **kw))
        return self.emit(e, outs, ins, lambda g: g.tensor_scalar(_ap(out), _ap(a), _ap(s1), _ap(s2), op0, op1, **kw))

    def tss(self, out, a, s, op, e="pool"):
        return self.emit(e, [out], [a], lambda g: g.tensor_single_scalar(_ap(out), _ap(a), s, op))

    def stt(self, out, a, s, b, op0, op1, e="dve"):
        ins = [a, b] + ([s] if isinstance(s, V) else [])
        return self.emit(e, [out], ins,
                         lambda g: g.scalar_tensor_tensor(_ap(out), _ap(a), _ap(s), _ap(b), op0, op1))

    def memset(self, out, val, e="dve"):
        return self.emit(e, [out], [], lambda g: g.memset(_ap(out), val))

    def reduce(self, out, in_, op, axis=None, e="dve"):
        axis = axis or AX.X
        return self.emit(e, [out], [in_], lambda g: g.tensor_reduce(_ap(out), _ap(in_), axis, op))


def run(ctx, in_maps):
    res = run_bass_kernel_spmd(ctx.nc, in_maps, core_ids=list(range(NCORES)))
    return res.results


D = 2048
KT = 16
NT = 9
NTOK = NT * 128
EPS = 1e-6


class Common:
    def __init__(self, c):
        self.c = c
        self.ident_d = c.dram_in("ident", [128, 128])
        self.ident = c.sb([128, 128], F32, "ident_sb")
        c.dma(self.ident[:], self.ident_d[:, :])
        self.eps = c.sb([128, 1], F32, "eps_sb")
        c.memset(self.eps[:], EPS)


def ln_hat(c, cm, xt, xh, width=D, tmp=None):
    nch = (width + 511) // 512
    st = tmp["st"]
    mv = tmp["mv"]
    rs = tmp["rs"]
    sq = tmp["sq"]
    for i in range(nch):
        lo, hi = i * 512, min(width, (i + 1) * 512)
        c.emit("dve", [st], [xt], lambda g, i=i, lo=lo, hi=hi: g.bn_stats(st.h[:, i * 6:(i + 1) * 6], xt.ap[:, lo:hi]))
    c.emit("dve", [mv], [st], lambda g: g.bn_aggr(mv.h[:], st.h[:, 0:nch * 6]))
    c.ts(sq[:], mv[:, 1:2], EPS, None, ALU.add)
    c.act(sq[:], sq[:], AF.Sqrt)
    c.emit("dve", [rs], [sq], lambda g: g.reciprocal(rs.h[:], sq.h[:]))
    c.ts(xh, xt, mv[:, 0:1], rs[:], ALU.subtract, ALU.mult)


def ln_tmp(c, tag=""):
    return {"st": c.sb([128, 24], F32, "ln_st" + tag), "mv": c.sb([128, 2], F32, "ln_mv" + tag),
            "rs": c.sb([128, 1], F32, "ln_rs" + tag), "sq": c.sb([128, 1], F32, "ln_sq" + tag)}


def to_fm(c, cm, xh, hT, tile, pst, scale_cols=None, bias_cols=None, nk=KT):
    for k in range(nk):
        p = pst[k % len(pst)]
        c.transpose(p[:, 0:128], xh[:, k * 128:(k + 1) * 128], cm.ident[:])
        dst = hT[:, k, tile * 128:(tile + 1) * 128]
        if scale_cols is not None:
            c.act(dst, p[:, 0:128], AF.Identity, bias=bias_cols[:, k:k + 1], scale=scale_cols[:, k:k + 1])
        else:
            c.copy(dst, p[:, 0:128], e="act")


def stream_linear(c, hT, w_dram, n_total, ntiles, consume, wbufs, pso, kt=KT, nbw=512, tile_list=None):
    wv = w_dram.rearrange("(k p) n -> p k n", p=128)
    nblk = (n_total + nbw - 1) // nbw
    tiles = tile_list if tile_list is not None else list(range(ntiles))
    i = 0
    for nb in range(nblk):
        lo = nb * nbw
        bw = min(nbw, n_total - lo)
        wb = wbufs[nb % len(wbufs)]
        c.dma(wb[:, 0:kt, 0:bw], wv[:, :, lo:lo + bw], q="pool")
        for t in tiles:
            p = pso[i % len(pso)]
            i += 1
            for k in range(kt):
                c.mm(p[:, 0:bw], hT[:, k, t * 128:(t + 1) * 128], wb[:, k, 0:bw],
                     start=(k == 0), stop=(k == kt - 1))
            consume(t, nb, lo, bw, p)


MODC = 2 * 12288 // NCORES


def build_mod():
    c = Ctx()
    cT = c.dram_in("cT", [128, KT, 3])
    w = c.dram_in("w", [D, MODC])
    b = c.dram_in("b", [3, MODC])
    out = c.dram_out("mod", [3, MODC])
    ct = c.sb([128, KT, 3], F32)
    sg = c.sb([128, KT, 3], F32)
    bt = c.sb([3, MODC], F32)
    ot = c.sb([3, MODC], F32)
    c.dma(ct[:], cT[:, :, :])
    c.dma(bt[:], b[:, :])
    c.act(sg[:], ct[:], AF.Sigmoid)
    c.tt(ct[:], ct[:], sg[:], ALU.mult)
    wb = [c.sb([128, KT, 512], F32, f"modw{i}") for i in range(2)]
    ps = [c.ps([128, 512]) for _ in range(2)]
    wv = w.rearrange("(k p) n -> p k n", p=128)
    for nb in range(MODC // 512):
        wt = wb[nb % 2]
        c.dma(wt[:], wv[:, :, nb * 512:(nb + 1) * 512])
        p = ps[nb % 2]
        for k in range(KT):
            c.mm(p[0:3, :], ct[:, k, :], wt[:, k, :], start=(k == 0), stop=(k == KT - 1))
        c.tt(ot[:, nb * 512:(nb + 1) * 512], p[0:3, :], bt[:, nb * 512:(nb + 1) * 512], ALU.add)
    c.dma(out[:, :], ot[:], q="pool")
    c.finish("pool")
    return c


IN_TOTAL = 3904


def load_mod_cols(c, mv_d, n):
    t = c.sb([128, n, KT], F32, "modcols")
    c.dma(t[:], mv_d[:, :, :])
    return t


def build_proj():
    c = Ctx()
    cm = Common(c)
    x = c.dram_in("x", [NTOK, D])
    mv_d = c.dram_in("mv", [128, 4, KT])
    w = c.dram_in("w_in", [D, IN_TOTAL])
    out = c.dram_out("proj", [NTOK, IN_TOTAL])
    mv = load_mod_cols(c, mv_d, 4)
    c.ts(mv[:, 1, :], mv[:, 1, :], 1.0, None, ALU.add)
    c.ts(mv[:, 3, :], mv[:, 3, :], 1.0, None, ALU.add)
    hT = c.sb([128, KT, NTOK], BF16, "hT")
    xts = [c.sb([128, D], F32, f"xt{i}") for i in range(2)]
    xh = c.sb([128, D], F32, "xh")
    tmp = ln_tmp(c)
    pst = [c.ps([128, 512]) for _ in range(2)]
    for t in range(NT):
        xt = xts[t % 2]
        c.dma(xt[:], x[t * 128:(t + 1) * 128, :])
        ln_hat(c, cm, xt[:], xh[:], tmp=tmp)
        j = 0 if t < 8 else 2
        to_fm(c, cm, xh, hT, t, pst, scale_cols=mv[:, j + 1, :], bias_cols=mv[:, j, :])
    wbufs = [c.sb([128, KT, 512], BF16, f"wb{i}") for i in range(2)]
    pso = [c.ps([128, 512]) for _ in range(3)]
    obufs = [c.sb([128, 512], F32, f"ob{i}") for i in range(3)]
    cnt = [0]

    def consume(t, nb, lo, bw, p):
        ob = obufs[cnt[0] % 3]
        cnt[0] += 1
        c.copy(ob[:, 0:bw], p[:, 0:bw], e=("dve" if cnt[0] % 2 else "act"))
        c.dma(out[t * 128:(t + 1) * 128, lo:lo + bw], ob[:, 0:bw], q="sp")

    stream_linear(c, hT, w, IN_TOTAL, NT, consume, wbufs, pso)
    c.finish("sp")
    return c


_PROGS = {}
IDENT = np.eye(128, dtype=np.float32)


def prog(name, builder):
    if name not in _PROGS:
        _PROGS[name] = builder()
    return _PROGS[name]


def fm_cols(vec):
    return np.ascontiguousarray(vec.reshape(-1, 128).T)


def host_mod(cv, c_ctx, ada_w, ada_b):
    cvec = np.concatenate([cv, c_ctx[None, :]], 0)
    cT = np.ascontiguousarray(cvec.reshape(3, KT, 128).transpose(2, 1, 0))
    ims = []
    per = 12288 // NCORES
    for j in range(NCORES):
        w = np.concatenate([ada_w[l][:, j * per:(j + 1) * per] for l in range(2)], 1)
        b = np.concatenate([np.broadcast_to(ada_b[l][None, j * per:(j + 1) * per], (3, per)) for l in range(2)], 1)
        ims.append({"cT": cT, "w": np.ascontiguousarray(w), "b": np.ascontiguousarray(b)})
    res = run(prog("mod", build_mod), ims)
    mods = []
    for l in range(2):
        mods.append(np.concatenate([res[j]["mod"][:, l * per:(l + 1) * per] for j in range(NCORES)], 1))
    return mods


def tok_shard(x_lat, x_ctx):
    outs = []
    for j in range(NCORES):
        b, q = j // 4, j % 4
        a = np.zeros((NTOK, x_lat.shape[-1]), np.float32)
        a[:1024] = x_lat[b, q * 1024:(q + 1) * 1024]
        if j < 4:
            a[1024:] = x_ctx[j // 2, (j % 2) * 128:(j % 2 + 1) * 128]
        outs.append(a)
    return outs


def tok_unshard(parts, width):
    lat = np.zeros((2, 4096, width), np.float32)
    ctx = np.zeros((2, 256, width), np.float32)
    for j in range(NCORES):
        b, q = j // 4, j % 4
        lat[b, q * 1024:(q + 1) * 1024] = parts[j][:1024]
        if j < 4:
            ctx[j // 2, (j % 2) * 128:(j % 2 + 1) * 128] = parts[j][1024:]
    return lat, ctx


def mod_cols(mod, j, idxs):
    b = j // 4
    cols = []
    for i in idxs:
        cols.append(fm_cols(mod[b, i * D:(i + 1) * D]))
    for i in idxs:
        cols.append(fm_cols(mod[2, i * D:(i + 1) * D]))
    return np.ascontiguousarray(np.stack(cols, 1))


def host_proj(x_lat, x_ctx, mod, w_in):
    xs = tok_shard(x_lat, x_ctx)
    ims = []
    for j in range(NCORES):
        ims.append({"ident": IDENT, "x": xs[j], "mv": mod_cols(mod, j, [0, 1]), "w_in": w_in})
    res = run(prog("proj", build_proj), ims)
    return tok_unshard([r["proj"] for r in res], IN_TOTAL)


NLAT = 4096
NALL = 4352
NTA = 34
NTL = 32


def ms_rstd(c, x, w, tmp, eps=EPS):
    st, mv, rs, sq = tmp["st"], tmp["mv"], tmp["rs"], tmp["sq"]
    c.emit("dve", [st], [x], lambda g: g.bn_stats(st.h[:, 0:6], _ap(x)))
    c.emit("dve", [mv], [st], lambda g: g.bn_aggr(mv.h[:], st.h[:, 0:6]))
    c.stt(sq[:], mv[:, 0:1], mv[:, 0:1], mv[:, 1:2], ALU.mult, ALU.add)
    c.ts(sq[:], sq[:], eps, None, ALU.add)
    c.act(sq[:], sq[:], AF.Sqrt)
    c.emit("dve", [rs], [sq], lambda g: g.reciprocal(rs.h[:], sq.h[:]))


def rope_tm(c, x, out, cos, sin, n, t1, t2):
    xv = V(_tl(x), _ap(x).rearrange("p (h t n) -> p h t n", h=2, t=2))
    ov = V(_tl(out), _ap(out).rearrange("p (h t n) -> p h t n", h=2, t=2))
    cv = V(_tl(cos), _ap(cos).rearrange("p (h n) -> p h n", h=2))
    sv = V(_tl(sin), _ap(sin).rearrange("p (h n) -> p h n", h=2))
    a = V(t1, t1.h[:, 0:2 * n].rearrange("p (h n) -> p h n", h=2))
    b = V(t2, t2.h[:, 0:2 * n].rearrange("p (h n) -> p h n", h=2))
    x1, x2 = xv[:, :, 0, :], xv[:, :, 1, :]
    c.tt(a, x1, cv, ALU.mult)
    c.tt(b, x2, sv, ALU.mult)
    c.tt(ov[:, :, 0, :], a, b, ALU.subtract)
    c.tt(a, x1, sv, ALU.mult)
    c.tt(b, x2, cv, ALU.mult)
    c.tt(ov[:, :, 1, :], a, b, ALU.add)


def attention(c, cm, qTs, kTs, vt, q_tiles, k_lo, k_hi, scale, bufs, out_cb):
    S, PT, pss, pst, pso, cmax, mx, rsum = (bufs[k] for k in ("S", "PT", "pss", "pst", "pso", "cmax", "mx", "rsum"))
    Pb, identb = bufs["Pb"], bufs["identb"]
    nk = k_hi - k_lo
    nch = (nk + 511) // 512
    nkt = nk // 128
    for qi, qt in enumerate(q_tiles):
        q0 = qt * 128
        for ch in range(nch):
            lo = k_lo + ch * 512
            w = min(512, k_hi - lo)
            p = pss[ch % len(pss)]
            for i, ((qT, r), (kT, _)) in enumerate(zip(qTs, kTs)):
                c.mm(p[:, 0:w], qT[0:r, q0:q0 + 128], kT[0:r, lo:lo + w], start=(i == 0), stop=(i == len(qTs) - 1))
            c.copy(S[:, ch * 512:ch * 512 + w], p[:, 0:w], e="act")
        for ch in range(nch):
            w = min(512, nk - ch * 512)
            c.reduce(cmax[:, ch:ch + 1], S[:, ch * 512:ch * 512 + w], ALU.max)
        import os
        DBG = int(os.environ.get("ATT_DBG", 9))
        if DBG < 2:
            continue
        c.reduce(mx[:], cmax[:, 0:nch], ALU.max)
        c.ts(mx[:], mx[:], -scale, None, ALU.mult)
        c.act(Pb[:, 0:nk], S[:, 0:nk], AF.Exp, bias=mx[:], scale=scale)
        c.reduce(rsum[:], Pb[:, 0:nk], ALU.add)
        c.emit("dve", [rsum], [rsum], lambda g: g.reciprocal(rsum.h[:], rsum.h[:]))
        po = pso[qi % len(pso)]
        dv = vt.h.shape[-1]
        if DBG < 3:
            continue
        for g4 in range((nkt + 3) // 4):
            pt = pst[g4 % len(pst)]
            n4 = min(4, nkt - g4 * 4)
            for j in range(n4):
                kt = g4 * 4 + j
                c.transpose(pt[:, j * 128:(j + 1) * 128], Pb[:, kt * 128:(kt + 1) * 128], identb[:])
            ptb = PT[g4 % len(PT)]
            c.copy(ptb[:, 0:n4 * 128], pt[:, 0:n4 * 128], e=("dve" if g4 % 2 else "act"))
            for j in range(n4 if DBG >= 4 else 0):
                kt = g4 * 4 + j
                c.mm(po[:, 0:dv], ptb[:, j * 128:(j + 1) * 128], vt[:, k_lo // 128 + kt, :],
                     start=(kt == 0), stop=(kt == nkt - 1))
        if DBG >= 4:
            out_cb(qt, po, rsum)


def build_att():
    c = Ctx()
    cm = Common(c)
    din = c.dram_in
    gq, gk, gv = din("gq", [NALL, 128]), din("gk", [NALL, 128]), din("gv", [NALL, 128])
    gqn, gkn = din("gqn", [128, 128]), din("gkn", [128, 128])
    mcq, mckv, mkr = din("mcq", [NALL, 512]), din("mckv", [NALL, 256]), din("mkr", [NALL, 64])
    mqn, mkvn = din("mqn", [128, 512]), din("mkvn", [128, 256])
    wuq, wukv = din("wuq", [512, 192]), din("wukv", [256, 256])
    rq, rk, rv, rg = din("rq", [NALL, 64]), din("rk", [NALL, 64]), din("rv", [NALL, 128]), din("rg", [NALL, 128])
    rpar, rgain = din("rpar", [128, 2]), din("rgain", [128, 128])
    cos128, sin128 = din("cos128", [NLAT, 64]), din("sin128", [NLAT, 64])
    cos64, sin64 = din("cos64", [NLAT, 32]), din("sin64", [NLAT, 32])
    rconst = din("rconst", [128, 6, 128])
    rcol = din("rcol", [128, 2])
    o_gqa, o_mla, o_ret = c.dram_out("o_gqa", [NALL, 128]), c.dram_out("o_mla", [NALL, 128]), c.dram_out("o_ret", [NALL, 128])

    QT = c.sb([128, NALL], BF16, "QT")
    KTb = c.sb([128, NALL], BF16, "KT")
    VT = c.sb([128, NTA, 128], BF16, "VT")
    QR = c.sb([64, NALL], BF16, "QR")
    KR = c.sb([64, NALL], BF16, "KR")
    identb = c.sb([128, 128], BF16, "identb")
    c.copy(identb[:], cm.ident[:])
    bufs = {"S": c.sb([128, NALL], F32, "S"), "PT": [c.sb([128, 512], BF16, f"PT{i}") for i in range(2)],
            "Pb": c.sb([128, NALL], BF16, "Pb"), "identb": identb,
            "pss": [c.ps([128, 512]) for _ in range(2)], "pst": [c.ps([128, 512], BF16) for _ in range(2)],
            "pso": [c.ps([128, 512]) for _ in range(2)],
            "cmax": c.sb([128, 16], F32, "cmax"), "mx": c.sb([128, 1], F32, "mx"), "rsum": c.sb([128, 1], F32, "rsum")}
    psx = [c.ps([128, 512]) for _ in range(2)]
    tmp = ln_tmp(c)
    xin = [c.sb([128, 512], F32, f"xin{i}") for i in range(3)]
    xa = c.sb([128, 512], F32, "xa")
    xb = c.sb([128, 512], F32, "xb")
    t1 = c.sb([128, 128], F32, "ropet1")
    t2 = c.sb([128, 128], F32, "ropet2")
    cs = [c.sb([128, 2, 64], F32, f"cs{i}") for i in range(2)]
    ob = [c.sb([128, 128], F32, f"ob{i}") for i in range(3)]
    gains = c.sb([128, 128 + 128 + 512 + 256 + 128], F32, "gains")
    G_Q, G_K, G_MQ, G_MKV, G_R = 0, 128, 256, 768, 1024
    c.dma(gains[:, G_Q:G_Q + 128], gqn[:, :])
    c.dma(gains[:, G_K:G_K + 128], gkn[:, :])
    c.dma(gains[:, G_MQ:G_MQ + 512], mqn[:, :])
    c.dma(gains[:, G_MKV:G_MKV + 256], mkvn[:, :])
    c.dma(gains[:, G_R:G_R + 128], rgain[:, :])
    cnt = [0]

    def outer(dst):
        def cb(qt, po, rsum):
            o = ob[cnt[0] % 3]
            cnt[0] += 1
            c.ts(o[:], po[:, 0:128], rsum[:], None, ALU.mult)
            c.dma(dst[qt * 128:(qt + 1) * 128, :], o[:], q="pool")
        return cb

    def load_cs(t, cos_d, sin_d, n2, k):
        t_ = cs[k % 2]
        c.dma(t_[:, 0, 0:n2], cos_d[t * 128:(t + 1) * 128, :])
        c.dma(t_[:, 1, 0:n2], sin_d[t * 128:(t + 1) * 128, :])
        return t_

    def tr_to(dst, src, rows, t):
        p = psx[t % 2]
        c.transpose(p[0:rows, 0:128], src, cm.ident[:])
        c.copy(dst[0:rows, t * 128:(t + 1) * 128], p[0:rows, 0:128], e="act")

    import os
    SECT = os.environ.get("ATT_SECT", "gmr")
    for t in range(NTA if "g" in SECT else 0):
        xq, xk = xin[0], xin[1]
        c.dma(xq[:, 0:128], gq[t * 128:(t + 1) * 128, :])
        c.dma(xk[:, 0:128], gk[t * 128:(t + 1) * 128, :])
        c.dma(VT[:, t, :], gv[t * 128:(t + 1) * 128, :], q="pool")
        for (x, g0, dstT) in ((xq, G_Q, QT), (xk, G_K, KTb)):
            ms_rstd(c, x[:, 0:128], 128, tmp)
            c.stt(xa[:, 0:128], x[:, 0:128], tmp["rs"][:], gains[:, g0:g0 + 128], ALU.mult, ALU.mult)
            if t < NTL:
                cst = load_cs(t, cos128, sin128, 64, t)
                rope_tm(c, xa[:, 0:128], xb[:, 0:128], cst[:, 0, 0:64], cst[:, 1, 0:64], 32, t1, t2)
                tr_to(dstT, xb[:, 0:128], 128, t)
            else:
                tr_to(dstT, xa[:, 0:128], 128, t)
    if "g" in SECT:
        nq = int(os.environ.get("GQA_NQ", NTL))
        attention(c, cm, [(QT, 128)], [(KTb, 128)], VT, list(range(nq)), 0, NALL, 128 ** -0.5, bufs, outer(o_gqa))
        if nq == NTL:
            attention(c, cm, [(QT, 128)], [(KTb, 128)], VT, [32, 33], NLAT, NALL, 128 ** -0.5, bufs, outer(o_gqa))

    wq = c.sb([128, 4, 192], F32, "wq")
    wkv = c.sb([128, 2, 256], F32, "wkv")
    c.dma(wq[:], wuq.rearrange("(k p) n -> p k n", p=128))
    c.dma(wkv[:], wukv.rearrange("(k p) n -> p k n", p=128))
    cT = c.sb([128, 4, 128], F32, "cT")
    for t in range(NTA if "m" in SECT else 0):
        xq, xkv, xr = xin[0], xin[1], xin[2]
        c.dma(xq[:, 0:512], mcq[t * 128:(t + 1) * 128, :])
        c.dma(xkv[:, 0:256], mckv[t * 128:(t + 1) * 128, :])
        c.dma(xr[:, 0:64], mkr[t * 128:(t + 1) * 128, :])
        if t < NTL:
            cst = load_cs(t, cos64, sin64, 32, t)
        ms_rstd(c, xq[:, 0:512], 512, tmp)
        c.stt(xa[:, 0:512], xq[:, 0:512], tmp["rs"][:], gains[:, G_MQ:G_MQ + 512], ALU.mult, ALU.mult)
        for k in range(4):
            p = psx[k % 2]
            c.transpose(p[:, 0:128], xa[:, k * 128:(k + 1) * 128], cm.ident[:])
            c.copy(cT[:, k, :], p[:, 0:128], e="act")
        p = psx[0]
        for k in range(4):
            c.mm(p[:, 0:128], wq[:, k, 0:128], cT[:, k, :], start=(k == 0), stop=(k == 3))
        c.copy(QT[:, t * 128:(t + 1) * 128], p[:, 0:128], e="act")
        p = psx[1]
        for k in range(4):
            c.mm(p[:, 0:64], cT[:, k, :], wq[:, k, 128:192], start=(k == 0), stop=(k == 3))
        c.copy(xb[:, 0:64], p[:, 0:64])
        if t < NTL:
            rope_tm(c, xb[:, 0:64], xb[:, 64:128], cst[:, 0, 0:32], cst[:, 1, 0:32], 16, t1, t2)
            tr_to(QR, xb[:, 64:128], 64, t)
        else:
            tr_to(QR, xb[:, 0:64], 64, t)
        ms_rstd(c, xkv[:, 0:256], 256, tmp)
        c.stt(xa[:, 0:256], xkv[:, 0:256], tmp["rs"][:], gains[:, G_MKV:G_MKV + 256], ALU.mult, ALU.mult)
        for k in range(2):
            p = psx[k % 2]
            c.transpose(p[:, 0:128], xa[:, k * 128:(k + 1) * 128], cm.ident[:])
            c.copy(cT[:, k, :], p[:, 0:128], e="act")
        p = psx[0]
        for k in range(2):
            c.mm(p[:, 0:128], wkv[:, k, 0:128], cT[:, k, :], start=(k == 0), stop=(k == 1))
        c.copy(KTb[:, t * 128:(t + 1) * 128], p[:, 0:128], e="act")
        p = psx[1]
        for k in range(2):
            c.mm(p[:, 0:128], cT[:, k, :], wkv[:, k, 128:256], start=(k == 0), stop=(k == 1))
        c.copy(VT[:, t, :], p[:, 0:128])
        if t < NTL:
            rope_tm(c, xr[:, 0:64], xb[:, 128:192], cst[:, 0, 0:32], cst[:, 1, 0:32], 16, t1, t2)
            tr_to(KR, xb[:, 128:192], 64, t)
        else:
            tr_to(KR, xr[:, 0:64], 64, t)
    sc = 192 ** -0.5
    if "m" in SECT:
        attention(c, cm, [(QT, 128), (QR, 64)], [(KTb, 128), (KR, 64)], VT, list(range(NTL)), 0, NALL, sc, bufs, outer(o_mla))
        attention(c, cm, [(QT, 128), (QR, 64)], [(KTb, 128), (KR, 64)], VT, [32, 33], NLAT, NALL, sc, bufs, outer(o_mla))
    if "r" not in SECT:
        c.finish("pool")
        return c

    RQ = c.sb([64, NALL], F32, "RQ")
    RK = c.sb([64, NALL], F32, "RK")
    RV = c.sb([128, NTA, 128], F32, "RV")
    rc = c.sb([128, 6, 128], F32, "rc")
    rcl = c.sb([128, 2], F32, "rcl")
    rp = c.sb([128, 2], F32, "rp")
    c.dma(rc[:], rconst[:, :, :])
    c.dma(rcl[:], rcol[:, :])
    c.dma(rp[:], rpar[:, :])
    lg = c.sb([128, 2], F32, "lg")
    c.act(lg[:], rp[:], AF.Exp)
    c.ts(lg[:], lg[:], -1.0, None, ALU.mult)
    DT = c.sb([128, 128], F32, "DT")
    dtmp = c.sb([128, 128], F32, "dtmp")
    c.act(DT[:], rc[:, 0, :], AF.Exp, scale=lg[:, 0:1])
    c.tt(DT[:], DT[:], rc[:, 2, :], ALU.mult)
    c.act(dtmp[:], rc[:, 1, :], AF.Exp, scale=lg[:, 1:2])
    c.tt(dtmp[:], dtmp[:], rc[:, 3, :], ALU.mult)
    c.tt(DT[:], DT[:], dtmp[:], ALU.add)
    dq = c.sb([128, 2, 128], F32, "dq")
    c.act(dq[:, 0, :], rc[:, 4, :], AF.Exp, scale=lg[:, 0:1])
    c.act(dq[:, 1, :], rc[:, 5, :], AF.Exp, scale=lg[:, 1:2])
    dk = c.sb([128, 2], F32, "dk")
    c.act(dk[:, 0:1], rcl[:, 0:1], AF.Exp, scale=lg[:, 0:1])
    c.act(dk[:, 1:2], rcl[:, 1:2], AF.Exp, scale=lg[:, 1:2])
    gcd = c.sb([128, 2], F32, "gcd")
    c.ts(gcd[:], lg[:], 128.0, None, ALU.mult)
    c.act(gcd[:], gcd[:], AF.Exp)
    Ktm = c.sb([128, NTA, 64], F32, "Ktm")
    Sf = c.sb([64, NTA + 1, 128], F32, "Sf")
    Sb = c.sb([64, NTA + 1, 128], F32, "Sb")
    kd = c.sb([128, 2, 64], F32, "kd")
    fwd_order = [32, 33] + list(range(NTL))
    for t in range(NTA):
        xq, xk = xin[0], xin[1]
        c.dma(xq[:, 0:64], rq[t * 128:(t + 1) * 128, :])
        c.dma(xk[:, 0:64], rk[t * 128:(t + 1) * 128, :])
        c.dma(RV[:, t, :], rv[t * 128:(t + 1) * 128, :])
        c.ts(xk[:, 0:64], xk[:, 0:64], 0.125, None, ALU.mult)
        if t < NTL:
            cst = load_cs(t, cos64, sin64, 32, t)
            rope_tm(c, xq[:, 0:64], xb[:, 0:64], cst[:, 0, 0:32], cst[:, 1, 0:32], 16, t1, t2)
            rope_tm(c, xk[:, 0:64], Ktm[:, t, :], cst[:, 0, 0:32], cst[:, 1, 0:32], 16, t1, t2)
            tr_to(RQ, xb[:, 0:64], 64, t)
        else:
            c.copy(Ktm[:, t, :], xk[:, 0:64])
            tr_to(RQ, xq[:, 0:64], 64, t)
        tr_to(RK, Ktm[:, t, :], 64, t)
    bwd_order = [33, 32] + list(range(NTL - 1, -1, -1))
    for (S_, order, col) in ((Sf, fwd_order, 0), (Sb, bwd_order, 1)):
        c.memset(S_[:, 0, :], 0.0)
        for i, t in enumerate(order):
            c.ts(kd[:, col, :], Ktm[:, t, :], dk[:, col:col + 1], None, ALU.mult)
            p = psx[i % 2]
            c.mm(p[0:64, 0:128], kd[:, col, :], RV[:, t, :])
            c.stt(S_[:, i + 1, :], S_[:, i, :], gcd[0:64, col:col + 1], p[0:64, 0:128], ALU.mult, ALU.add)
    fpos = {t: i for i, t in enumerate(fwd_order)}
    bpos = {t: i for i, t in enumerate(bwd_order)}
    MT = [c.sb([128, 128], F32, f"MT{i}") for i in range(2)]
    qd = [c.sb([64, 2, 128], F32, f"qd{i}") for i in range(2)]
    gt = [c.sb([128, 128], F32, f"gt{i}") for i in range(2)]
    for t in range(NTA):
        sl = slice(t * 128, (t + 1) * 128)
        p = psx[0]
        c.mm(p[:, 0:128], RK[0:64, sl], RQ[0:64, sl])
        m = MT[t % 2]
        c.tt(m[:], p[:, 0:128], DT[:], ALU.mult)
        q_ = qd[t % 2]
        c.tt(q_[:, 0, :], RQ[0:64, sl], dq[0:64, 0, :], ALU.mult)
        c.tt(q_[:, 1, :], RQ[0:64, sl], dq[0:64, 1, :], ALU.mult)
        po = psx[1]
        c.mm(po[:, 0:128], m[:], RV[:, t, :], start=True, stop=False)
        c.mm(po[:, 0:128], q_[:, 0, :], Sf[:, fpos[t], :], start=False, stop=False)
        c.mm(po[:, 0:128], q_[:, 1, :], Sb[:, bpos[t], :], start=False, stop=True)
        g_ = gt[t % 2]
        c.dma(g_[:], rg[sl, :])
        o = ob[cnt[0] % 3]
        cnt[0] += 1
        c.copy(xa[:, 0:128], po[:, 0:128])
        ln_hat(c, cm, xa[:, 0:128], xb[:, 0:128], width=128, tmp=tmp)
        c.tt(xb[:, 0:128], xb[:, 0:128], gains[:, G_R:G_R + 128], ALU.mult)
        c.act(xa[:, 128:256], g_[:], AF.Sigmoid)
        c.tt(xa[:, 128:256], xa[:, 128:256], g_[:], ALU.mult)
        c.tt(o[:], xb[:, 0:128], xa[:, 128:256], ALU.mult)
        c.dma(o_ret[sl, :], o[:], q="pool")
    c.finish("pool")
    return c


def rope_tables(dim):
    n = dim // 4
    inv = (np.float32(10000.0) ** (-np.arange(n, dtype=np.float32) / np.float32(n))).astype(np.float32)
    t = np.arange(NLAT)
    row = (t // 64).astype(np.float32)
    col = (t % 64).astype(np.float32)
    ang = np.concatenate([row[:, None] * inv[None, :], col[:, None] * inv[None, :]], 1).astype(np.float32)
    return np.cos(ang).astype(np.float32), np.sin(ang).astype(np.float32)


def ret_consts():
    j = np.arange(128, dtype=np.float32)[:, None]
    t = np.arange(128, dtype=np.float32)[None, :]
    z = np.zeros((128, 128), np.float32)
    rc = np.stack([np.maximum(t - j, 0), np.maximum(j - t, 0), (t >= j).astype(np.float32),
                   (j > t).astype(np.float32), t + 1 + z, 128 - t + z], 1).astype(np.float32)
    rcol = np.stack([127 - j[:, 0], j[:, 0]], 1).astype(np.float32)
    return np.ascontiguousarray(rc), np.ascontiguousarray(rcol)


def rep(v, n=128):
    return np.ascontiguousarray(np.broadcast_to(np.asarray(v, np.float32).reshape(1, -1), (n, np.size(v))))


def host_att(pl, pc, P, l):
    c128, s128 = rope_tables(128)
    c64, s64 = rope_tables(64)
    rc, rcol = ret_consts()
    ims = []
    for j in range(NCORES):
        b, h = j // 4, j % 4
        pa = np.concatenate([pl[b], pc[b]], 0)
        kv = h // 2
        cut = lambda o, w: np.ascontiguousarray(pa[:, o:o + w])
        ims.append({
            "ident": IDENT,
            "gq": cut(512 + h * 128, 128), "gk": cut(1024 + kv * 128, 128), "gv": cut(1280 + kv * 128, 128),
            "gqn": rep(P["gqa_q_norm"][l]), "gkn": rep(P["gqa_k_norm"][l]),
            "mcq": cut(3072, 512), "mckv": cut(3584, 256), "mkr": cut(3840, 64),
            "mqn": rep(P["mla_q_norm"][l]), "mkvn": rep(P["mla_kv_norm"][l]),
            "wuq": np.ascontiguousarray(P["mla_w_uq"][l][:, h * 192:(h + 1) * 192]),
            "wukv": np.ascontiguousarray(P["mla_w_ukv"][l][:, h * 256:(h + 1) * 256]),
            "rq": cut(1536 + h * 64, 64), "rk": cut(1792 + h * 64, 64),
            "rv": cut(2048 + h * 128, 128), "rg": cut(2560 + h * 128, 128),
            "rpar": rep([P["ret_decay_f"][l][h], P["ret_decay_b"][l][h]]),
            "rgain": rep(P["ret_norm"][l][h * 128:(h + 1) * 128]),
            "cos128": c128, "sin128": s128, "cos64": c64, "sin64": s64, "rconst": rc, "rcol": rcol,
        })
    res = run(prog("att", build_att), ims)
    outs = {}
    for nm in ("o_gqa", "o_ret", "o_mla"):
        lat = np.zeros((2, NLAT, 512), np.float32)
        ctx = np.zeros((2, 256, 512), np.float32)
        for j in range(NCORES):
            b, h = j // 4, j % 4
            lat[b, :, h * 128:(h + 1) * 128] = res[j][nm][:NLAT]
            ctx[b, :, h * 128:(h + 1) * 128] = res[j][nm][NLAT:]
        outs[nm] = (lat, ctx)
    return outs


PI = float(np.pi)


def rsin(c, out, x, kf, ki):
    c.ts(kf, x, 1.0 / (2 * PI), None, ALU.mult)
    c.copy(ki, kf)
    c.copy(kf, ki)
    c.stt(kf, kf, -2 * PI, x, ALU.mult, ALU.add)
    c.ts(kf, kf, PI, -PI, ALU.min, ALU.max)
    c.act(out, kf, AF.Sin)


def build_s5():
    c = Ctx()
    cm = Common(c)
    uT = c.dram_in("uT", [2, 2, 64, NALL])
    apar = c.dram_in("apar", [128, 2, 2, 3])
    bw_d = c.dram_in("bw", [128, 2, 2, 32])
    cw_d = c.dram_in("cw", [128, 2, 2, 32])
    tidx_d = c.dram_in("tidx", [128, NALL])
    yT = c.dram_out("yT", [2, 2, 64, NALL])
    T_ = NALL
    tidx = c.sb([128, T_], F32, "tidx_sb")
    c.dma(tidx[:], tidx_d[:, :])
    ap_ = c.sb([128, 2, 2, 3], F32, "apar_sb")
    bw = c.sb([128, 2, 2, 32], F32, "bw_sb")
    cw = c.sb([128, 2, 2, 32], F32, "cw_sb")
    c.dma(ap_[:], apar[:, :, :, :])
    c.dma(bw[:], bw_d[:, :, :, :])
    c.dma(cw[:], cw_d[:, :, :, :])
    ncw = c.sb([128, 2, 32], F32, "ncw")
    c.ts(ncw[:], cw[:, :, 1, :], -1.0, None, ALU.mult)
    tabc = c.sb([128, T_], F32, "tabc")
    tabs = c.sb([128, T_], F32, "tabs")
    A1 = c.sb([128, T_], F32, "A1")
    A2 = c.sb([128, T_], F32, "A2")
    G1 = c.sb([128, T_], F32, "G1")
    G2 = c.sb([128, T_], F32, "G2")
    RT = c.sb([128, T_], F32, "RT")
    uts = [c.sb([32, 512], F32, f"ut{i}") for i in range(3)]
    KI = c.sb([128, T_], I32, "KI")
    ski = c.sb([128, 2], I32, "ski")
    sc = c.sb([128, 24], F32, "s5sc")
    pib = c.sb([128, 1], F32, "pib")
    c.memset(pib[:], PI)
    bb = c.sb([128, 2, 32], F32, "bb")
    bbT = c.sb([32, 2, 128], F32, "bbT")
    m1 = [c.sb([128, 512], F32, f"m1_{i}") for i in range(2)]
    m2 = [c.sb([128, 512], F32, f"m2_{i}") for i in range(2)]
    yo = [c.sb([32, 512], F32, f"yo{i}") for i in range(2)]
    psr = [c.ps([128, 512]) for _ in range(2)]
    psi = [c.ps([128, 512]) for _ in range(2)]
    psy = [c.ps([128, 512]) for _ in range(2)]
    pst = c.ps([128, 512])
    S = lambda i: sc[:, i:i + 1]
    nblk = (T_ + 511) // 512
    k = 0
    for pt in range(2):
        for d in range(2):
            a_re, a_im, ldt = ap_[:, pt, d, 0:1], ap_[:, pt, d, 1:2], ap_[:, pt, d, 2:3]
            c.act(S(0), ldt, AF.Exp)
            c.tt(S(1), a_re, S(0), ALU.mult)
            c.act(S(1), S(1), AF.Exp)
            c.tt(S(2), a_im, S(0), ALU.mult)
            rsin(c, S(6), S(2), S(8), ski[:, 0:1])
            c.ts(S(9), S(2), PI / 2, None, ALU.add)
            rsin(c, S(7), S(9), S(8), ski[:, 0:1])
            c.tt(S(10), S(1), S(7), ALU.mult)
            c.tt(S(11), S(1), S(6), ALU.mult)
            c.tt(S(12), a_re, a_re, ALU.mult)
            c.tt(S(13), a_im, a_im, ALU.mult)
            c.tt(S(12), S(12), S(13), ALU.add)
            c.emit("dve", [sc], [sc], lambda g: g.reciprocal(sc.h[:, 12:13], sc.h[:, 12:13]))
            c.ts(S(14), S(10), -1.0, None, ALU.add)
            c.tt(S(15), S(14), a_re, ALU.mult)
            c.tt(S(16), S(11), a_im, ALU.mult)
            c.tt(S(15), S(15), S(16), ALU.add)
            c.tt(S(15), S(15), S(12), ALU.mult)
            c.tt(S(16), S(11), a_re, ALU.mult)
            c.tt(S(17), S(14), a_im, ALU.mult)
            c.tt(S(16), S(16), S(17), ALU.subtract)
            c.tt(S(16), S(16), S(12), ALU.mult)
            c.ts(S(17), S(16), -1.0, None, ALU.mult)
            c.ts(bb[:, 0, :], bw[:, pt, 0, :], S(15), None, ALU.mult)
            c.stt(bb[:, 0, :], bw[:, pt, 1, :], S(17), bb[:, 0, :], ALU.mult, ALU.add)
            c.ts(bb[:, 1, :], bw[:, pt, 1, :], S(15), None, ALU.mult)
            c.stt(bb[:, 1, :], bw[:, pt, 0, :], S(16), bb[:, 1, :], ALU.mult, ALU.add)
            for ri in range(2):
                c.transpose(pst[0:32, ri * 128:(ri + 1) * 128], bb[:, ri, :], cm.ident[:])
            c.copy(bbT[:, 0, :], pst[0:32, 0:128], e="act")
            c.copy(bbT[:, 1, :], pst[0:32, 128:256], e="act")
            c.ts(A1[:], tidx[:], S(2), None, ALU.mult)
            rsin(c, tabs[:], A1[:], G1[:], KI[:])
            c.ts(A1[:], A1[:], PI / 2, None, ALU.add)
            rsin(c, tabc[:], A1[:], G1[:], KI[:])
            c.ts(RT[:], tidx[:], 0.0, S(1), ALU.mult, ALU.add)
            for b in range(2):
                for nb in range(nblk):
                    lo = nb * 512
                    w = min(512, T_ - lo)
                    pr, pi_ = psr[nb % 2], psi[nb % 2]
                    ut = uts[nb % 3]
                    c.dma(ut[:, 0:w], uT[d, b, pt * 32:(pt + 1) * 32, lo:lo + w])
                    c.mm(pr[:, 0:w], bbT[:, 0, :], ut[:, 0:w])
                    c.mm(pi_[:, 0:w], bbT[:, 1, :], ut[:, 0:w])
                    a, b_ = m1[nb % 2], m2[nb % 2]
                    cc, ss = tabc[:, lo:lo + w], tabs[:, lo:lo + w]
                    c.tt(a[:, 0:w], pr[:, 0:w], cc, ALU.mult)
                    c.tt(b_[:, 0:w], pi_[:, 0:w], ss, ALU.mult, e="pool" if False else "dve")
                    c.tt(A1[:, lo:lo + w], a[:, 0:w], b_[:, 0:w], ALU.add)
                    c.tt(a[:, 0:w], pi_[:, 0:w], cc, ALU.mult)
                    c.tt(b_[:, 0:w], pr[:, 0:w], ss, ALU.mult)
                    c.tt(A2[:, lo:lo + w], a[:, 0:w], b_[:, 0:w], ALU.subtract)
                c.emit("dve", [G1], [RT, A1], lambda g: g.tensor_tensor_scan(G1.h[:], RT.h[:], A1.h[:], 0.0, ALU.mult, ALU.add))
                c.emit("dve", [G2], [RT, A2], lambda g: g.tensor_tensor_scan(G2.h[:], RT.h[:], A2.h[:], 0.0, ALU.mult, ALU.add))
                for nb in range(nblk):
                    lo = nb * 512
                    w = min(512, T_ - lo)
                    a, b_ = m1[nb % 2], m2[nb % 2]
                    cc, ss = tabc[:, lo:lo + w], tabs[:, lo:lo + w]
                    e2 = "pool"
                    c.tt(a[:, 0:w], G1[:, lo:lo + w], cc, ALU.mult, e=e2)
                    c.tt(b_[:, 0:w], G2[:, lo:lo + w], ss, ALU.mult, e=e2)
                    c.tt(A1[:, lo:lo + w], a[:, 0:w], b_[:, 0:w], ALU.subtract, e=e2)
                    c.tt(a[:, 0:w], G1[:, lo:lo + w], ss, ALU.mult, e=e2)
                    c.tt(b_[:, 0:w], G2[:, lo:lo + w], cc, ALU.mult, e=e2)
                    c.tt(A2[:, lo:lo + w], a[:, 0:w], b_[:, 0:w], ALU.add, e=e2)
                    py = psy[nb % 2]
                    c.mm(py[0:32, 0:w], cw[:, pt, 0, :], A1[:, lo:lo + w], start=True, stop=False)
                    c.mm(py[0:32, 0:w], ncw[:, pt, :], A2[:, lo:lo + w], start=False, stop=True)
                    o = yo[k % 2]
                    k += 1
                    c.copy(o[:, 0:w], py[0:32, 0:w], e="act")
                    c.dma(yT[d, b, pt * 32:(pt + 1) * 32, lo:lo + w], o[:, 0:w], q="sp")
    c.finish("sp")
    return c


def host_s5(pl, pc, P, l):
    ims = []
    tidx = rep(np.arange(NALL, dtype=np.float32))
    for j in range(NCORES):
        uT = np.zeros((2, 2, 64, NALL), np.float32)
        for b in range(2):
            ul = pl[b][:, j * 64:(j + 1) * 64]
            uc = pc[b][:, j * 64:(j + 1) * 64]
            uT[0, b] = np.concatenate([uc, ul], 0).T
            uT[1, b] = np.concatenate([uc[::-1], ul[::-1]], 0).T
        apar = np.zeros((128, 2, 2, 3), np.float32)
        bw = np.zeros((128, 2, 2, 32), np.float32)
        cw = np.zeros((128, 2, 2, 32), np.float32)
        for pt in range(2):
            for gl in range(2):
                g = j * 4 + pt * 2 + gl
                rows = slice(gl * 64, (gl + 1) * 64)
                for d, sfx in enumerate(("f", "b")):
                    apar[rows, pt, d, 0] = P["s5_a_re_" + sfx][l][g]
                    apar[rows, pt, d, 1] = P["s5_a_im_" + sfx][l][g]
                    apar[rows, pt, d, 2] = P["s5_log_dt_" + sfx][l][g]
                bw[rows, pt, 0, gl * 16:(gl + 1) * 16] = P["s5_b_re"][l][g]
                bw[rows, pt, 1, gl * 16:(gl + 1) * 16] = P["s5_b_im"][l][g]
                cw[rows, pt, 0, gl * 16:(gl + 1) * 16] = P["s5_c_re"][l][g].T
                cw[rows, pt, 1, gl * 16:(gl + 1) * 16] = P["s5_c_im"][l][g].T
        ims.append({"ident": IDENT, "uT": uT, "apar": apar, "bw": bw, "cw": cw, "tidx": tidx})
    res = run(prog("s5", build_s5), ims)
    outs = []
    for d in range(2):
        lat = np.zeros((2, NLAT, 512), np.float32)
        ctx = np.zeros((2, 256, 512), np.float32)
        for j in range(NCORES):
            for b in range(2):
                y = res[j]["yT"][d, b].T
                yc, yl = y[:256], y[256:]
                if d == 1:
                    yc, yl = yc[::-1], yl[::-1]
                lat[b, :, j * 64:(j + 1) * 64] = yl
                ctx[b, :, j * 64:(j + 1) * 64] = yc
        outs.append((lat, ctx))
    return outs


def build_merge_a():
    c = Ctx()
    cm = Common(c)
    x = c.dram_in("x", [NTOK, D])
    mv_d = c.dram_in("mv", [128, 4, KT])
    yf, yb, u = c.dram_in("yf", [NTOK, 512]), c.dram_in("yb", [NTOK, 512]), c.dram_in("u", [NTOK, 512])
    s5d = c.dram_in("s5d", [128, 512])
    wglu = c.dram_in("wglu", [512, 512])
    obr = c.dram_in("obr", [3, NTOK, 512])
    wbr = c.dram_in("wbr", [4, 512, D])
    wg = c.dram_in("wg", [D, 4 * D])
    bg = c.dram_in("bg", [1, 4 * D])
    m_out = c.dram_out("m", [NTOK, D])
    mv = load_mod_cols(c, mv_d, 4)
    c.ts(mv[:, 1, :], mv[:, 1, :], 1.0, None, ALU.add)
    c.ts(mv[:, 3, :], mv[:, 3, :], 1.0, None, ALU.add)
    dr = c.sb([128, 512], F32, "s5d_sb")
    c.dma(dr[:], s5d[:, :])
    wgl = c.sb([128, 4, 512], F32, "wglu_sb")
    c.dma(wgl[:], wglu.rearrange("(k p) n -> p k n", p=128))
    ones = c.sb([1, 128], F32, "ones1")
    c.memset(ones[:], 1.0)
    bgts = [c.sb([1, 512], F32, f"bg_sb{i}") for i in range(2)]
    TP = NT
    hT = c.sb([128, KT, TP * 128], BF16, "hT")
    oT = c.sb([128, 4, 4, TP * 128], BF16, "oT")
    macc = c.sb([128, TP, 512], F32, "macc")
    xt = c.sb([128, D], F32, "xt")
    tmp = ln_tmp(c)
    a = [c.sb([128, 512], F32, f"ma{i}") for i in range(4)]
    zT = c.sb([128, 4, 128], F32, "zT")
    pst = [c.ps([128, 512]) for _ in range(2)]
    psg = [c.ps([128, 512]) for _ in range(2)]
    psp = [c.ps([128, 512]) for _ in range(2)]
    wgb = [c.sb([128, KT, 512], BF16, f"wgb{i}") for i in range(2)]
    wbb = [c.sb([128, 4, 512], BF16, f"wbb{i}") for i in range(2)]
    gsb = [c.sb([128, 512], F32, f"gsb{i}") for i in range(2)]
    wgv = wg.rearrange("(k p) n -> p k n", p=128)
    for p0 in range(0, NT, TP):
        tiles = list(range(p0, min(NT, p0 + TP)))
        for li, t in enumerate(tiles):
            rows = slice(t * 128, (t + 1) * 128)
            c.dma(xt[:], x[rows, :])
            ln_hat(c, cm, xt[:], xt[:], tmp=tmp)
            j = 0 if t < 8 else 2
            to_fm(c, cm, xt, hT, li, pst, scale_cols=mv[:, j + 1, :], bias_cols=mv[:, j, :])
            c.dma(a[0][:], yf[rows, :])
            c.dma(a[1][:], yb[rows, :])
            c.dma(a[2][:], u[rows, :])
            c.tt(a[0][:], a[0][:], a[1][:], ALU.add)
            c.tt(a[2][:], a[2][:], dr[:], ALU.mult)
            c.tt(a[0][:], a[0][:], a[2][:], ALU.add)
            c.tt(a[1][:], a[0][:], a[0][:], ALU.mult)
            c.ts(a[1][:], a[1][:], 0.044715, 1.0, ALU.mult, ALU.add)
            c.tt(a[1][:], a[1][:], a[0][:], ALU.mult)
            c.act(a[1][:], a[1][:], AF.Tanh, scale=0.7978845608028654)
            c.stt(a[1][:], a[1][:], 1.0, a[0][:], ALU.add, ALU.mult)
            c.ts(a[1][:], a[1][:], 0.5, None, ALU.mult)
            for k in range(4):
                p = pst[k % 2]
                c.transpose(p[:, 0:128], a[1][:, k * 128:(k + 1) * 128], cm.ident[:])
                c.copy(zT[:, k, :], p[:, 0:128], e="act")
            p = psg[0]
            for k in range(4):
                c.mm(p[:, 0:512], zT[:, k, :], wgl[:, k, :], start=(k == 0), stop=(k == 3))
            c.act(a[2][:], p[:, 0:512], AF.Sigmoid)
            c.tt(a[3][:], a[1][:], a[2][:], ALU.mult)
            for k in range(4):
                p = pst[k % 2]
                c.transpose(p[:, 0:128], a[3][:, k * 128:(k + 1) * 128], cm.ident[:])
                c.copy(oT[:, 0, k, li * 128:(li + 1) * 128], p[:, 0:128], e="act")
            for br in range(3):
                c.dma(a[0][:], obr[br, rows, :])
                for k in range(4):
                    p = pst[k % 2]
                    c.transpose(p[:, 0:128], a[0][:, k * 128:(k + 1) * 128], cm.ident[:])
                    c.copy(oT[:, br + 1, k, li * 128:(li + 1) * 128], p[:, 0:128], e="act")
        i = 0
        for nb in range(4):
            for k in range(4):
                wgt, wbt = wgb[i % 2], wbb[i % 2]
                i += 1
                col = k * D + nb * 512
                bgt = bgts[i % 2]
                c.dma(bgt[:], bg[:, col:col + 512])
                c.dma(wgt[:], wgv[:, :, col:col + 512], q="pool")
                c.dma(wbt[:], wbr[k, :, nb * 512:(nb + 1) * 512].rearrange("(k p) n -> p k n", p=128), q="pool")
                for li, t in enumerate(tiles):
                    pg, pp = psg[li % 2], psp[li % 2]
                    for kk in range(KT):
                        c.mm(pg[:, 0:512], hT[:, kk, li * 128:(li + 1) * 128], wgt[:, kk, :], start=(kk == 0), stop=False)
                    c.mm(pg[:, 0:512], ones[:, :], bgt[:, :], start=False, stop=True)
                    g = gsb[li % 2]
                    c.act(g[:], pg[:, 0:512], AF.Sigmoid)
                    for kk in range(4):
                        c.mm(pp[:, 0:512], oT[:, k, kk, li * 128:(li + 1) * 128], wbt[:, kk, :], start=(kk == 0), stop=(kk == 3))
                    if k == 0:
                        c.tt(macc[:, li, :], g[:], pp[:, 0:512], ALU.mult)
                    else:
                        c.tt(g[:], g[:], pp[:, 0:512], ALU.mult)
                        c.tt(macc[:, li, :], macc[:, li, :], g[:], ALU.add)
            for li, t in enumerate(tiles):
                c.dma(m_out[t * 128:(t + 1) * 128, nb * 512:(nb + 1) * 512], macc[:, li, :], q="sp")
    c.finish("sp")
    return c


def host_merge_a(x_lat, x_ctx, mod, pl, pc, s5o, atto, P, l):
    xs = tok_shard(x_lat, x_ctx)
    (lf, cf), (lb, cb) = s5o
    yfs, ybs = tok_shard(lf, cf), tok_shard(lb, cb)
    us = tok_shard(pl[:, :, :512], pc[:, :, :512])
    brs = [tok_shard(*atto[nm]) for nm in ("o_gqa", "o_ret", "o_mla")]
    ims = []
    for j in range(NCORES):
        ims.append({"ident": IDENT, "x": xs[j], "mv": mod_cols(mod, j, [0, 1]),
                    "yf": yfs[j], "yb": ybs[j], "u": us[j], "s5d": rep(P["s5_d"][l]),
                    "wglu": P["s5_w_glu"][l], "obr": np.stack([b_[j] for b_ in brs], 0),
                    "wbr": P["w_branch"][l], "wg": P["w_gate"][l], "bg": P["b_gate"][l][None, :]})
    res = run(prog("merge_a", build_merge_a), ims)
    return tok_unshard([r["m"] for r in res], D)


ALPHA = float((2 * 2) ** 0.25)


def build_merge_b():
    c = Ctx()
    cm = Common(c)
    m = c.dram_in("m", [NTOK, D])
    x = c.dram_in("x", [NTOK, D])
    w = c.dram_in("w_out", [D, D])
    reps_d = c.dram_in("reps", [128, 8, D])
    rw_d = c.dram_in("rw", [D, 16])
    x1_o = c.dram_out("x1", [NTOK, D])
    h2_o = c.dram_out("h2", [NTOK, D])
    aff_o = c.dram_out("aff", [NTOK, 16])
    reps = c.sb([128, 8, D], F32, "reps_sb")
    for i in range(8):
        c.dma(reps[:, i, :], reps_d[:, i, :])
    c.ts(reps[:, 4, :], reps[:, 4, :], 1.0, None, ALU.add)
    c.ts(reps[:, 6, :], reps[:, 6, :], 1.0, None, ALU.add)
    rw = c.sb([128, KT, 16], F32, "rw_sb")
    c.dma(rw[:], rw_d.rearrange("(k p) n -> p k n", p=128))
    mt = c.sb([128, D], F32, "mt")
    xt = c.sb([128, D], F32, "xt")
    xh = c.sb([128, D], F32, "xh")
    x1t = c.sb([128, D], F32, "x1t")
    mT = c.sb([128, KT, 128], BF16, "mT")
    rT = c.sb([128, KT, 128], F32, "rT")
    tmp = ln_tmp(c)
    tb = [c.sb([128, 512], F32, f"tb{i}") for i in range(2)]
    sm = c.sb([128, 40], F32, "sm")
    wbufs = [c.sb([128, KT, 512], BF16, f"wb{i}") for i in range(2)]
    pst = [c.ps([128, 512]) for _ in range(2)]
    pso = [c.ps([128, 512]) for _ in range(2)]
    psr = c.ps([128, 512])
    for t in range(NT):
        rows = slice(t * 128, (t + 1) * 128)
        lat = t < 8
        c.dma(mt[:], m[rows, :])
        c.dma(xt[:], x[rows, :])
        to_fm(c, cm, mt, mT, 0, pst)
        g1 = reps[:, 0 if lat else 1, :]

        def consume(t_, nb, lo, bw, p, g1=g1):
            b_ = tb[nb % 2]
            c.tt(b_[:, 0:bw], p[:, 0:bw], g1[:, lo:lo + bw], ALU.mult)
            c.stt(xt[:, lo:lo + bw], xt[:, lo:lo + bw], ALU_ALPHA, b_[:, 0:bw], ALU.mult, ALU.add)

        stream_linear(c, mT, w, D, 1, consume, wbufs, pso, tile_list=[0])
        ln_hat(c, cm, xt[:], xh[:], tmp=tmp)
        c.tt(xh[:], xh[:], reps[:, 2, :], ALU.mult)
        c.tt(x1t[:], xh[:], reps[:, 3, :], ALU.add)
        c.dma(x1_o[rows, :], x1t[:], q="pool")
        ln_hat(c, cm, x1t[:], xh[:], tmp=tmp)
        c.tt(xh[:], xh[:], reps[:, 4 if lat else 6, :], ALU.mult)
        c.tt(mt[:], xh[:], reps[:, 5 if lat else 7, :], ALU.add)
        c.dma(h2_o[rows, :], mt[:], q="pool")
        to_fm(c, cm, mt, rT, 0, pst)
        for k in range(KT):
            c.mm(psr[:, 0:16], rT[:, k, :], rw[:, k, :], start=(k == 0), stop=(k == KT - 1))
        c.copy(sm[:, 0:16], psr[:, 0:16])
        c.reduce(sm[:, 32:33], sm[:, 0:16], ALU.max)
        c.ts(sm[:, 32:33], sm[:, 32:33], -1.0, None, ALU.mult)
        c.act(sm[:, 0:16], sm[:, 0:16], AF.Exp, bias=sm[:, 32:33], scale=1.0)
        c.reduce(sm[:, 33:34], sm[:, 0:16], ALU.add)
        c.emit("dve", [sm], [sm], lambda g: g.reciprocal(sm.h[:, 34:35], sm.h[:, 33:34]))
        c.ts(sm[:, 16:32], sm[:, 0:16], sm[:, 34:35], None, ALU.mult)
        c.dma(aff_o[rows, :], sm[:, 16:32], q="pool")
    c.finish("pool")
    return c


ALU_ALPHA = ALPHA


def host_merge_b(m_lat, m_ctx, x_lat, x_ctx, mod, P, l):
    ms = tok_shard(m_lat, m_ctx)
    xs = tok_shard(x_lat, x_ctx)
    ims = []
    for j in range(NCORES):
        b = j // 4
        seg = lambda r_, i: mod[r_, i * D:(i + 1) * D]
        reps = np.stack([rep(seg(b, 2)), rep(seg(2, 2)), rep(P["ln1_g"][l]), rep(P["ln1_b"][l]),
                         rep(seg(b, 4)), rep(seg(b, 3)), rep(seg(2, 4)), rep(seg(2, 3))], 1)
        ims.append({"ident": IDENT, "m": ms[j], "x": xs[j], "w_out": P["w_out"][l],
                    "reps": np.ascontiguousarray(reps), "rw": P["router_w"][l]})
    res = run(prog("merge_b", build_merge_b), ims)
    return (tok_unshard([r["x1"] for r in res], D), tok_unshard([r["h2"] for r in res], D),
            tok_unshard([r["aff"] for r in res], 16))


CAP_L, CAP_C = 512, 32


def build_topk():
    c = Ctx()
    a_l = c.dram_in("affT", [32, NLAT])
    a_c = c.dram_in("affcT", [32, 256])
    g_l, i_l = c.dram_out("gate", [32, CAP_L]), c.dram_out("idx", [32, CAP_L], U32)
    g_c, i_c = c.dram_out("gatec", [32, CAP_C]), c.dram_out("idxc", [32, CAP_C], U32)
    for (src, n, cap, go, io, tag) in ((a_l, NLAT, CAP_L, g_l, i_l, "l"), (a_c, 256, CAP_C, g_c, i_c, "c")):
        w = c.sb([32, n], F32, "work" + tag)
        gv = c.sb([32, cap], F32, "gv" + tag)
        iv = c.sb([32, cap], U32, "iv" + tag)
        c.dma(w[:], src[:, :])
        for r in range(cap // 8):
            sl = slice(r * 8, (r + 1) * 8)
            c.emit("dve", [gv], [w], lambda g, sl=sl, gv=gv, w=w: g.max(out=gv.h[:, sl], in_=w.h[:]))
            c.emit("dve", [iv], [gv, w], lambda g, sl=sl, gv=gv, iv=iv, w=w: g.max_index(out=iv.h[:, sl], in_max=gv.h[:, sl], in_values=w.h[:]))
            c.emit("dve", [w], [gv, w], lambda g, sl=sl, gv=gv, w=w: g.match_replace(out=w.h[:], in_to_replace=gv.h[:, sl], in_values=w.h[:], imm_value=-1.0))
        c.dma(go[:, :], gv[:], q="pool")
        c.dma(io[:, :], iv[:], q="pool")
    c.finish("pool")
    return c


def host_topk(aff_l, aff_c):
    affT = np.ascontiguousarray(aff_l.transpose(0, 2, 1).reshape(32, NLAT))
    affcT = np.ascontiguousarray(aff_c.transpose(0, 2, 1).reshape(32, 256))
    res = run(prog("topk", build_topk), [{"affT": affT, "affcT": affcT}] * NCORES)[0]
    return (res["gate"].reshape(2, 16, CAP_L), res["idx"].reshape(2, 16, CAP_L).astype(np.int64),
            res["gatec"].reshape(2, 16, CAP_C), res["idxc"].reshape(2, 16, CAP_C).astype(np.int64))


FF = 1024
ER = NTOK


def build_expert():
    c = Ctx()
    cm = Common(c)
    xs = c.dram_in("xs", [2, ER, D])
    gt_d = c.dram_in("gt", [128, 2, NT])
    wg_d, wu_d, wd_d = c.dram_in("wg", [2, D, FF]), c.dram_in("wu", [2, D, FF]), c.dram_in("wd", [2, FF, D])
    y = c.dram_out("y", [2, ER, D])
    gt = c.sb([128, 2, NT], F32, "gt_sb")
    c.dma(gt[:], gt_d[:, :, :])
    xT = c.sb([128, KT, ER], BF16, "xT")
    hT = c.sb([128, 8, ER], BF16, "hmT")
    xt = [c.sb([128, D], F32, f"xt{i}") for i in range(2)]
    wgb = [c.sb([128, KT, 128], BF16, f"wgb{i}") for i in range(2)]
    wub = [c.sb([128, KT, 128], BF16, f"wub{i}") for i in range(2)]
    wdb = [c.sb([128, 8, 512], BF16, f"wdb{i}") for i in range(2)]
    sg = [c.sb([128, 512], F32, f"sg{i}") for i in range(2)]
    ob = [c.sb([128, 512], F32, f"ob{i}") for i in range(3)]
    pst = [c.ps([128, 512]) for _ in range(2)]
    psa = [c.ps([128, 512]) for _ in range(2)]
    psu = [c.ps([128, 512]) for _ in range(2)]
    pso = [c.ps([128, 512]) for _ in range(2)]
    chunks = [(0, 512), (512, 512), (1024, 128)]
    k0 = 0
    for e in range(2):
        for t in range(NT):
            x_ = xt[t % 2]
            c.dma(x_[:], xs[e, t * 128:(t + 1) * 128, :])
            to_fm(c, cm, x_, xT, t, pst)
        for fb in range(FF // 128):
            wg_, wu_ = wgb[fb % 2], wub[fb % 2]
            c.dma(wg_[:], wg_d[e, :, fb * 128:(fb + 1) * 128].rearrange("(k p) n -> p k n", p=128), q="pool")
            c.dma(wu_[:], wu_d[e, :, fb * 128:(fb + 1) * 128].rearrange("(k p) n -> p k n", p=128), q="pool")
            for ci, (lo, w) in enumerate(chunks):
                pa, pu = psa[ci % 2], psu[ci % 2]
                for k in range(KT):
                    c.mm(pa[:, 0:w], wg_[:, k, :], xT[:, k, lo:lo + w], start=(k == 0), stop=(k == KT - 1))
                for k in range(KT):
                    c.mm(pu[:, 0:w], wu_[:, k, :], xT[:, k, lo:lo + w], start=(k == 0), stop=(k == KT - 1))
                s_ = sg[ci % 2]
                c.act(s_[:, 0:w], pa[:, 0:w], AF.Sigmoid)
                c.tt(s_[:, 0:w], s_[:, 0:w], pa[:, 0:w], ALU.mult)
                c.tt(hT[:, fb, lo:lo + w], s_[:, 0:w], pu[:, 0:w], ALU.mult)
        for nb in range(D // 512):
            wd_ = wdb[nb % 2]
            c.dma(wd_[:], wd_d[e, :, nb * 512:(nb + 1) * 512].rearrange("(k p) n -> p k n", p=128), q="pool")
            for t in range(NT):
                p = pso[t % 2]
                for k in range(8):
                    c.mm(p[:, 0:512], hT[:, k, t * 128:(t + 1) * 128], wd_[:, k, :], start=(k == 0), stop=(k == 7))
                o = ob[k0 % 3]
                k0 += 1
                c.ts(o[:], p[:, 0:512], gt[:, e, t:t + 1], None, ALU.mult)
                c.dma(y[e, t * 128:(t + 1) * 128, nb * 512:(nb + 1) * 512], o[:], q="sp")
    c.finish("sp")
    return c


def host_expert(h2l, h2c, gate, idx, gatec, idxc, P, l):
    ims = []
    for j in range(NCORES):
        xs = np.zeros((2, ER, D), np.float32)
        gt = np.zeros((2, ER), np.float32)
        for ei in range(2):
            e = 2 * j + ei
            for b in range(2):
                xs[ei, b * 512:(b + 1) * 512] = h2l[b][idx[b, e]]
                gt[ei, b * 512:(b + 1) * 512] = gate[b, e]
                xs[ei, 1024 + b * 32:1024 + (b + 1) * 32] = h2c[b][idxc[b, e]]
                gt[ei, 1024 + b * 32:1024 + (b + 1) * 32] = gatec[b, e]
        gtp = np.ascontiguousarray(gt.reshape(2, NT, 128).transpose(2, 0, 1))
        ims.append({"ident": IDENT, "xs": xs, "gt": gtp,
                    "wg": P["moe_w_gate"][l][2 * j:2 * j + 2], "wu": P["moe_w_up"][l][2 * j:2 * j + 2],
                    "wd": P["moe_w_down"][l][2 * j:2 * j + 2]})
    res = run(prog("expert", build_expert), ims)
    Y = np.stack([res[j]["y"] for j in range(NCORES)], 0).reshape(16, ER, D)
    return Y


NSL = 16 * CAP_L
NSC = 16 * CAP_C


def build_combine():
    c = Ctx()
    yl = c.dram_in("yl", [NSL, D])
    yc = c.dram_in("yc", [NSC, D])
    il_d = c.dram_in("il", [128, NSL // 128], I32)
    ic_d = c.dram_in("ic", [128, NSC // 128], I32)
    tok_d = c.dram_in("tok", [128, NTOK])
    x1 = c.dram_in("x1", [NTOK, D])
    reps_d = c.dram_in("reps", [128, 4, D])
    out = c.dram_out("x2", [NTOK, D])
    reps = c.sb([128, 4, D], F32, "reps_sb")
    for i in range(4):
        c.dma(reps[:, i, :], reps_d[:, i, :])
    tok = c.sb([128, NTOK], F32, "tok_sb")
    c.dma(tok[:], tok_d[:, :])
    ili = c.sb([128, NSL // 128], I32, "ili")
    ici = c.sb([128, NSC // 128], I32, "ici")
    c.dma(ili[:], il_d[:, :])
    c.dma(ici[:], ic_d[:, :])
    il = c.sb([128, NSL // 128], F32, "il_f")
    ic = c.sb([128, NSC // 128], F32, "ic_f")
    c.copy(il[:], ili[:])
    c.copy(ic[:], ici[:])
    X = c.sb([128, NT, D], F32, "X")
    for t in range(NT):
        c.dma(X[:, t, :], x1[t * 128:(t + 1) * 128, :])
    yb = [c.sb([128, 512], BF16, f"yb{i}") for i in range(3)]
    sb_ = [c.sb([128, 128], BF16, f"sel{i}") for i in range(4)]
    tb = [c.sb([128, 512], F32, f"tb{i}") for i in range(2)]
    acc = [c.ps([128, 512]) for _ in range(8)]
    xh = c.sb([128, D], F32, "xh")
    tmp = ln_tmp(c)
    si = 0
    for nb in range(4):
        cols = slice(nb * 512, (nb + 1) * 512)
        nkt = NSL // 128
        for kt in range(nkt):
            y_ = yb[kt % 3]
            c.dma(y_[:], yl[kt * 128:(kt + 1) * 128, cols], q="pool")
            for t in range(8):
                s_ = sb_[si % 4]
                si += 1
                c.ts(s_[:], tok[:, t * 128:(t + 1) * 128], il[:, kt:kt + 1], None, ALU.is_equal)
                c.mm(acc[t][:, 0:512], s_[:], y_[:], start=(kt == 0), stop=(kt == nkt - 1))
        for t in range(8):
            b_ = tb[t % 2]
            c.tt(b_[:], acc[t][:, 0:512], reps[:, 0, cols], ALU.mult)
            c.stt(X[:, t, cols], X[:, t, cols], ALPHA, b_[:], ALU.mult, ALU.add)
        nkc = NSC // 128
        for kt in range(nkc):
            y_ = yb[kt % 3]
            c.dma(y_[:], yc[kt * 128:(kt + 1) * 128, cols], q="pool")
            s_ = sb_[si % 4]
            si += 1
            c.ts(s_[:], tok[:, 1024:1152], ic[:, kt:kt + 1], None, ALU.is_equal)
            c.mm(acc[0][:, 0:512], s_[:], y_[:], start=(kt == 0), stop=(kt == nkc - 1))
        b_ = tb[0]
        c.tt(b_[:], acc[0][:, 0:512], reps[:, 1, cols], ALU.mult)
        c.stt(X[:, 8, cols], X[:, 8, cols], ALPHA, b_[:], ALU.mult, ALU.add)
    for t in range(NT):
        ln_hat(c, cm_none, X[:, t, :], xh[:], tmp=tmp)
        c.tt(xh[:], xh[:], reps[:, 2, :], ALU.mult)
        c.tt(X[:, t, :], xh[:], reps[:, 3, :], ALU.add)
        c.dma(out[t * 128:(t + 1) * 128, :], X[:, t, :], q="sp")
    c.finish("sp")
    return c


cm_none = None


def host_combine(Y, idx, idxc, x1l, x1c, mod, P, l):
    x1s = tok_shard(x1l, x1c)
    ims = []
    for j in range(NCORES):
        b, q = j // 4, j % 4
        yl = np.ascontiguousarray(Y[:, b * 512:(b + 1) * 512, :].reshape(NSL, D))
        il = idx[b].reshape(NSL).astype(np.int32)
        tok = np.full((NTOK,), -5.0, np.float32)
        tok[:1024] = q * 1024 + np.arange(1024)
        if j < 4:
            cb = j // 2
            yc = np.ascontiguousarray(Y[:, 1024 + cb * 32:1024 + (cb + 1) * 32, :].reshape(NSC, D))
            ic = idxc[cb].reshape(NSC).astype(np.int32)
            tok[1024:] = (j % 2) * 128 + np.arange(128)
        else:
            yc = np.zeros((NSC, D), np.float32)
            ic = np.full((NSC,), -7, np.int32)
        seg = lambda r_, i: mod[r_, i * D:(i + 1) * D]
        reps = np.stack([rep(seg(b, 5)), rep(seg(2, 5)), rep(P["ln2_g"][l]), rep(P["ln2_b"][l])], 1)
        ims.append({"yl": yl, "yc": yc, "il": np.ascontiguousarray(il.reshape(-1, 128).T),
                    "ic": np.ascontiguousarray(ic.reshape(-1, 128).T), "tok": rep(tok),
                    "x1": x1s[j], "reps": np.ascontiguousarray(reps)})
    res = run(prog("combine", build_combine), ims)
    return tok_unshard([r["x2"] for r in res], D)


def kernel(**inp):
    P = {k: np.asarray(v) for k, v in inp.items()}
    x_lat = np.asarray(P["x"], np.float32)
    x_ctx = np.asarray(P["ctx"], np.float32)
    mods = host_mod(P["c"], P["c_ctx"], P["ada_w"], P["ada_b"])
    for l in range(2):
        mod = mods[l]
        pl, pc = host_proj(x_lat, x_ctx, mod, P["w_in"][l])
        s5o = host_s5(pl, pc, P, l)
        atto = host_att(pl, pc, P, l)
        ml, mc = host_merge_a(x_lat, x_ctx, mod, pl, pc, s5o, atto, P, l)
        (x1l, x1c), (h2l, h2c), (al, ac) = host_merge_b(ml, mc, x_lat, x_ctx, mod, P, l)
        gate, idx, gatec, idxc = host_topk(al, ac)
        Y = host_expert(h2l, h2c, gate, idx, gatec, idxc, P, l)
        x_lat, x_ctx = host_combine(Y, idx, idxc, x1l, x1c, mod, P, l)
    return np.ascontiguousarray(x_lat, dtype=np.float32)
```

```python
import numpy as np
import concourse.bass as bass
import concourse.mybir as mybir
from concourse.bass_utils import run_bass_kernel_spmd

F32 = mybir.dt.float32
BF16 = mybir.dt.bfloat16
I32 = mybir.dt.int32
U32 = mybir.dt.uint32
AF = mybir.ActivationFunctionType
ALU = mybir.AluOpType
AX = mybir.AxisListType

NCORES = 8


class V:
    __slots__ = ("t", "ap")

    def __init__(self, t, ap):
        self.t = t
        self.ap = ap

    def __getitem__(self, idx):
        return V(self.t, self.ap[idx])


class T:
    def __init__(self, ctx, name, shape, dtype, psum=False):
        nc = ctx.nc
        if psum:
            self.h = nc.alloc_psum_tensor(name, shape, dtype)
        else:
            self.h = nc.alloc_sbuf_tensor(name, shape, dtype)
        self.lw = None
        self.rd = {}
        self.name = name
        self.psum = psum

    def __getitem__(self, idx):
        return V(self, self.h[idx])

    def v(self, ap):
        return V(self, ap)


def _ap(x):
    if isinstance(x, V):
        return x.ap
    if isinstance(x, T):
        return x.h[:]
    return x


def _tl(x):
    if isinstance(x, V):
        return x.t
    if isinstance(x, T):
        return x
    return None


import os as _os


class Ctx:
    SAME_ENGINE_SYNC = _os.environ.get("MK_SAME_ENGINE_SYNC", "1") == "1"

    def __init__(self):
        self.nc = bass.Bass("TRN2", target_bir_lowering=False)
        nc = self.nc
        self.eng = {"pe": nc.tensor, "act": nc.scalar, "dve": nc.vector,
                    "pool": nc.gpsimd, "sp": nc.sync}
        self.sems = {}
        self.cnt = {}
        for e in ("pe", "act", "dve", "pool"):
            self.sems[e] = nc.alloc_semaphore("sem_" + e)
            self.cnt[e] = 0
        self.known = {e: {} for e in self.eng}
        self.dma_pool = {}
        self.dma_k = {}
        self.ntile = 0
        self.out_deps = []
        self.stream = {e: [] for e in self.eng}
        self.finalized = False

    def sb(self, shape, dtype, name=None):
        self.ntile += 1
        return T(self, name or f"t{self.ntile}", shape, dtype)

    def ps(self, shape, dtype=F32, name=None):
        self.ntile += 1
        return T(self, name or f"p{self.ntile}", shape, dtype, psum=True)

    def dram_in(self, name, shape, dtype=F32):
        return self.nc.dram_tensor(name, list(shape), dtype, kind="ExternalInput").ap()

    def dram_out(self, name, shape, dtype=F32):
        return self.nc.dram_tensor(name, list(shape), dtype, kind="ExternalOutput").ap()

    def _wait(self, e, deps):
        eng = self.eng[e]
        kn = self.known[e]
        best = {}
        for d in deps:
            if d is None:
                continue
            k, val = d
            if best.get(k, 0) < val:
                best[k] = val
        for k, val in best.items():
            if isinstance(k, str):
                if k == e and (e == "pe" or not self.SAME_ENGINE_SYNC):
                    continue
                sem = self.sems[k]
            else:
                sem = k
            if kn.get(k, 0) >= val:
                continue
            self.stream[e].append(("w", sem, val))
            kn[k] = val

    def _deps(self, outs, ins):
        deps = []
        for v in ins:
            t = _tl(v)
            if t is not None:
                deps.append(t.lw)
                if t.psum:
                    deps.extend(t.rd.items())
        for v in outs:
            t = _tl(v)
            if t is not None:
                deps.append(t.lw)
                deps.extend(t.rd.items())
        return deps

    def emit(self, e, outs, ins, f):
        self._wait(e, self._deps(outs, ins))
        self.cnt[e] += 1
        n = self.cnt[e]
        self.stream[e].append(("i", f, self.sems[e], 1))
        for v in ins:
            t = _tl(v)
            if t is not None:
                t.rd[e] = n
        for v in outs:
            t = _tl(v)
            if t is not None:
                t.lw = (e, n)
                t.rd = {}
        return None

    def dma(self, out, in_, q="sp", npool=24, **kw):
        self._wait(q, self._deps([out], [in_]))
        if q not in self.dma_pool:
            self.dma_pool[q] = [[self.nc.alloc_semaphore(f"dq_{q}_{i}"), 0] for i in range(npool)]
            self.dma_k[q] = 0
        pool = self.dma_pool[q]
        slot = pool[self.dma_k[q] % len(pool)]
        self.dma_k[q] += 1
        sem, val = slot
        if val > 0 and self.known[q].get(sem, 0) < val:
            self.stream[q].append(("w", sem, val))
            self.known[q][sem] = val
        slot[1] = val + 16
        o_ap, i_ap = _ap(out), _ap(in_)
        self.stream[q].append(("i", lambda g: g.dma_start(out=o_ap, in_=i_ap, **kw), sem, 16))
        if _tl(in_) is not None:
            _tl(in_).rd[sem] = val + 16
        if _tl(out) is not None:
            _tl(out).lw = (sem, val + 16)
            _tl(out).rd = {}
        else:
            self.out_deps.append((sem, val + 16))
        return None

    def finish(self, e="sp"):
        self._wait(e, self.out_deps)
        assert not self.finalized
        self.finalized = True
        streams = self.stream

        def replay(g, items):
            for it in items:
                if it[0] == "w":
                    g.wait_ge(it[1], it[2])
                else:
                    it[1](g).then_inc(it[2], it[3])

        with self.nc.Block() as block:
            @block.sync
            def _(g):
                replay(g, streams["sp"])

            @block.tensor
            def _(g):
                replay(g, streams["pe"])

            @block.vector
            def _(g):
                replay(g, streams["dve"])

            @block.scalar
            def _(g):
                replay(g, streams["act"])

            @block.gpsimd
            def _(g):
                replay(g, streams["pool"])

    def mm(self, out, lhsT, rhs, start=True, stop=True):
        return self.emit("pe", [out], [lhsT, rhs] + ([] if start else [out]),
                         lambda g: g.matmul(_ap(out), _ap(lhsT), _ap(rhs), start=start, stop=stop))

    def transpose(self, out, in_, ident):
        return self.emit("pe", [out], [in_, ident],
                         lambda g: g.transpose(_ap(out), _ap(in_), _ap(ident)))

    def act(self, out, in_, func, bias=None, scale=None, accum_out=None, e="act"):
        ins = [in_]
        kw = {}
        if bias is not None:
            kw["bias"] = _ap(bias)
            ins.append(bias)
        if scale is not None:
            kw["scale"] = _ap(scale)
            ins.append(scale)
        outs = [out]
        if accum_out is not None:
            kw["accum_out"] = _ap(accum_out)
            outs.append(accum_out)
        return self.emit(e, outs, ins, lambda g: g.activation(_ap(out), _ap(in_), func, **kw))

    def copy(self, out, in_, e="dve"):
        if e == "act":
            return self.emit(e, [out], [in_], lambda g: g.copy(_ap(out), _ap(in_)))
        return self.emit(e, [out], [in_], lambda g: g.tensor_copy(_ap(out), _ap(in_)))

    def tt(self, out, a, b, op, e="dve"):
        return self.emit(e, [out], [a, b], lambda g: g.tensor_tensor(_ap(out), _ap(a), _ap(b), op))

    def ts(self, out, a, s1, s2, op0, op1=None, e="dve", accum_out=None):
        ins = [a] + [s for s in (s1, s2) if isinstance(s, V)]
        outs = [out] + ([accum_out] if accum_out is not None else [])
        kw = {}
        if accum_out is not None:
            kw["accum_out"] = _ap(accum_out)
        if op1 is None:
            return self.emit(e, outs, ins, lambda g: g.tensor_scalar(_ap(out), _ap(a), _ap(s1), None, op0, **kw))
        return self.emit(e, outs, ins, lambda g: g.tensor_scalar(_ap(out), _ap(a), _ap(s1), _ap(s2), op0, op1, **kw))

    def tss(self, out, a, s, op, e="pool"):
        return self.emit(e, [out], [a], lambda g: g.tensor_single_scalar(_ap(out), _ap(a), s, op))

    def stt(self, out, a, s, b, op0, op1, e="dve"):
        ins = [a, b] + ([s] if isinstance(s, V) else [])
        return self.emit(e, [out], ins,
                         lambda g: g.scalar_tensor_tensor(_ap(out), _ap(a), _ap(s), _ap(b), op0, op1))

    def memset(self, out, val, e="dve"):
        return self.emit(e, [out], [], lambda g: g.memset(_ap(out), val))

    def reduce(self, out, in_, op, axis=None, e="dve"):
        axis = axis or AX.X
        return self.emit(e, [out], [in_], lambda g: g.tensor_reduce(_ap(out), _ap(in_), axis, op))


def run(ctx, in_maps):
    if _os.environ.get("MK_TRACE", "0") == "1":
        res = run_bass_kernel_spmd(ctx.nc, in_maps, core_ids=list(range(NCORES)), trace=True)
        print("MK_TRACE exec_time_ns", res.exec_time_ns, flush=True)
        return res.results
    res = run_bass_kernel_spmd(ctx.nc, in_maps, core_ids=list(range(NCORES)))
    return res.results


D = 2048
KT = 16
NT = 9
NTOK = NT * 128
EPS = 1e-6


class Common:
    def __init__(self, c):
        self.c = c
        self.ident_d = c.dram_in("ident", [128, 128])
        self.ident = c.sb([128, 128], F32, "ident_sb")
        c.dma(self.ident[:], self.ident_d[:, :])
        self.eps = c.sb([128, 1], F32, "eps_sb")
        c.memset(self.eps[:], EPS)


def ln_hat(c, cm, xt, xh, width=D, tmp=None):
    nch = (width + 511) // 512
    st = tmp["st"]
    mv = tmp["mv"]
    rs = tmp["rs"]
    sq = tmp["sq"]
    for i in range(nch):
        lo, hi = i * 512, min(width, (i + 1) * 512)
        c.emit("dve", [st], [xt], lambda g, i=i, lo=lo, hi=hi: g.bn_stats(st.h[:, i * 6:(i + 1) * 6], xt.ap[:, lo:hi]))
    c.emit("dve", [mv], [st], lambda g: g.bn_aggr(mv.h[:], st.h[:, 0:nch * 6]))
    c.ts(sq[:], mv[:, 1:2], EPS, None, ALU.add)
    c.act(sq[:], sq[:], AF.Sqrt)
    c.emit("dve", [rs], [sq], lambda g: g.reciprocal(rs.h[:], sq.h[:]))
    c.ts(xh, xt, mv[:, 0:1], rs[:], ALU.subtract, ALU.mult)


def ln_tmp(c, tag=""):
    return {"st": c.sb([128, 24], F32, "ln_st" + tag), "mv": c.sb([128, 2], F32, "ln_mv" + tag),
            "rs": c.sb([128, 1], F32, "ln_rs" + tag), "sq": c.sb([128, 1], F32, "ln_sq" + tag)}


def to_fm(c, cm, xh, hT, tile, pst, scale_cols=None, bias_cols=None, nk=KT):
    for k in range(nk):
        p = pst[k % len(pst)]
        c.transpose(p[:, 0:128], xh[:, k * 128:(k + 1) * 128], cm.ident[:])
        dst = hT[:, k, tile * 128:(tile + 1) * 128]
        if scale_cols is not None:
            c.act(dst, p[:, 0:128], AF.Identity, bias=bias_cols[:, k:k + 1], scale=scale_cols[:, k:k + 1])
        else:
            c.copy(dst, p[:, 0:128], e="act")


def stream_linear(c, hT, w_dram, n_total, ntiles, consume, wbufs, pso, kt=KT, nbw=512, tile_list=None):
    wv = w_dram.rearrange("(k p) n -> p k n", p=128)
    nblk = (n_total + nbw - 1) // nbw
    tiles = tile_list if tile_list is not None else list(range(ntiles))
    i = 0
    for nb in range(nblk):
        lo = nb * nbw
        bw = min(nbw, n_total - lo)
        wb = wbufs[nb % len(wbufs)]
        c.dma(wb[:, 0:kt, 0:bw], wv[:, :, lo:lo + bw], q="pool")
        for t in tiles:
            p = pso[i % len(pso)]
            i += 1
            for k in range(kt):
                c.mm(p[:, 0:bw], hT[:, k, t * 128:(t + 1) * 128], wb[:, k, 0:bw],
                     start=(k == 0), stop=(k == kt - 1))
            consume(t, nb, lo, bw, p)


MODC = 2 * 12288 // NCORES


def build_mod():
    c = Ctx()
    cT = c.dram_in("cT", [128, KT, 3])
    w = c.dram_in("w", [D, MODC])
    b = c.dram_in("b", [3, MODC])
    out = c.dram_out("mod", [3, MODC])
    ct = c.sb([128, KT, 3], F32)
    sg = c.sb([128, KT, 3], F32)
    bt = c.sb([3, MODC], F32)
    ot = c.sb([3, MODC], F32)
    c.dma(ct[:], cT[:, :, :])
    c.dma(bt[:], b[:, :])
    c.act(sg[:], ct[:], AF.Sigmoid)
    c.tt(ct[:], ct[:], sg[:], ALU.mult)
    wb = [c.sb([128, KT, 512], F32, f"modw{i}") for i in range(2)]
    ps = [c.ps([128, 512]) for _ in range(2)]
    wv = w.rearrange("(k p) n -> p k n", p=128)
    for nb in range(MODC // 512):
        wt = wb[nb % 2]
        c.dma(wt[:], wv[:, :, nb * 512:(nb + 1) * 512])
        p = ps[nb % 2]
        for k in range(KT):
            c.mm(p[0:3, :], ct[:, k, :], wt[:, k, :], start=(k == 0), stop=(k == KT - 1))
        c.tt(ot[:, nb * 512:(nb + 1) * 512], p[0:3, :], bt[:, nb * 512:(nb + 1) * 512], ALU.add)
    c.dma(out[:, :], ot[:], q="pool")
    c.finish("pool")
    return c


IN_TOTAL = 3904


def load_mod_cols(c, mv_d, n):
    t = c.sb([128, n, KT], F32, "modcols")
    c.dma(t[:], mv_d[:, :, :])
    return t


def build_proj():
    c = Ctx()
    cm = Common(c)
    x = c.dram_in("x", [NTOK, D])
    mv_d = c.dram_in("mv", [128, 4, KT])
    w = c.dram_in("w_in", [D, IN_TOTAL])
    out = c.dram_out("proj", [NTOK, IN_TOTAL])
    mv = load_mod_cols(c, mv_d, 4)
    c.ts(mv[:, 1, :], mv[:, 1, :], 1.0, None, ALU.add)
    c.ts(mv[:, 3, :], mv[:, 3, :], 1.0, None, ALU.add)
    hT = c.sb([128, KT, NTOK], BF16, "hT")
    xts = [c.sb([128, D], F32, f"xt{i}") for i in range(2)]
    xh = c.sb([128, D], F32, "xh")
    tmp = ln_tmp(c)
    pst = [c.ps([128, 512]) for _ in range(2)]
    for t in range(NT):
        xt = xts[t % 2]
        c.dma(xt[:], x[t * 128:(t + 1) * 128, :])
        ln_hat(c, cm, xt[:], xh[:], tmp=tmp)
        j = 0 if t < 8 else 2
        to_fm(c, cm, xh, hT, t, pst, scale_cols=mv[:, j + 1, :], bias_cols=mv[:, j, :])
    wbufs = [c.sb([128, KT, 512], BF16, f"wb{i}") for i in range(2)]
    pso = [c.ps([128, 512]) for _ in range(3)]
    obufs = [c.sb([128, 512], F32, f"ob{i}") for i in range(3)]
    cnt = [0]

    def consume(t, nb, lo, bw, p):
        ob = obufs[cnt[0] % 3]
        cnt[0] += 1
        c.copy(ob[:, 0:bw], p[:, 0:bw], e=("dve" if cnt[0] % 2 else "act"))
        c.dma(out[t * 128:(t + 1) * 128, lo:lo + bw], ob[:, 0:bw], q="sp")

    stream_linear(c, hT, w, IN_TOTAL, NT, consume, wbufs, pso)
    c.finish("sp")
    return c


_PROGS = {}
IDENT = np.eye(128, dtype=np.float32)


def prog(name, builder):
    if name not in _PROGS:
        _PROGS[name] = builder()
    return _PROGS[name]


def fm_cols(vec):
    return np.ascontiguousarray(vec.reshape(-1, 128).T)


def host_mod(cv, c_ctx, ada_w, ada_b):
    cvec = np.concatenate([cv, c_ctx[None, :]], 0)
    cT = np.ascontiguousarray(cvec.reshape(3, KT, 128).transpose(2, 1, 0))
    ims = []
    per = 12288 // NCORES
    for j in range(NCORES):
        w = np.concatenate([ada_w[l][:, j * per:(j + 1) * per] for l in range(2)], 1)
        b = np.concatenate([np.broadcast_to(ada_b[l][None, j * per:(j + 1) * per], (3, per)) for l in range(2)], 1)
        ims.append({"cT": cT, "w": np.ascontiguousarray(w), "b": np.ascontiguousarray(b)})
    res = run(prog("mod", build_mod), ims)
    mods = []
    for l in range(2):
        mods.append(np.concatenate([res[j]["mod"][:, l * per:(l + 1) * per] for j in range(NCORES)], 1))
    return mods


def tok_shard(x_lat, x_ctx):
    outs = []
    for j in range(NCORES):
        b, q = j // 4, j % 4
        a = np.zeros((NTOK, x_lat.shape[-1]), np.float32)
        a[:1024] = x_lat[b, q * 1024:(q + 1) * 1024]
        if j < 4:
            a[1024:] = x_ctx[j // 2, (j % 2) * 128:(j % 2 + 1) * 128]
        outs.append(a)
    return outs


def tok_unshard(parts, width):
    lat = np.zeros((2, 4096, width), np.float32)
    ctx = np.zeros((2, 256, width), np.float32)
    for j in range(NCORES):
        b, q = j // 4, j % 4
        lat[b, q * 1024:(q + 1) * 1024] = parts[j][:1024]
        if j < 4:
            ctx[j // 2, (j % 2) * 128:(j % 2 + 1) * 128] = parts[j][1024:]
    return lat, ctx


def mod_cols(mod, j, idxs):
    b = j // 4
    cols = []
    for i in idxs:
        cols.append(fm_cols(mod[b, i * D:(i + 1) * D]))
    for i in idxs:
        cols.append(fm_cols(mod[2, i * D:(i + 1) * D]))
    return np.ascontiguousarray(np.stack(cols, 1))


def host_proj(x_lat, x_ctx, mod, w_in):
    xs = tok_shard(x_lat, x_ctx)
    ims = []
    for j in range(NCORES):
        ims.append({"ident": IDENT, "x": xs[j], "mv": mod_cols(mod, j, [0, 1]), "w_in": w_in})
    res = run(prog("proj", build_proj), ims)
    return tok_unshard([r["proj"] for r in res], IN_TOTAL)


NLAT = 4096
NALL = 4352
NTA = 34
NTL = 32


def ms_rstd(c, x, w, tmp, eps=EPS):
    st, mv, rs, sq = tmp["st"], tmp["mv"], tmp["rs"], tmp["sq"]
    c.emit("dve", [st], [x], lambda g: g.bn_stats(st.h[:, 0:6], _ap(x)))
    c.emit("dve", [mv], [st], lambda g: g.bn_aggr(mv.h[:], st.h[:, 0:6]))
    c.stt(sq[:], mv[:, 0:1], mv[:, 0:1], mv[:, 1:2], ALU.mult, ALU.add)
    c.ts(sq[:], sq[:], eps, None, ALU.add)
    c.act(sq[:], sq[:], AF.Sqrt)
    c.emit("dve", [rs], [sq], lambda g: g.reciprocal(rs.h[:], sq.h[:]))


def rope_tm(c, x, out, cos, sin, n, t1, t2, e="pool"):
    xv = V(_tl(x), _ap(x).rearrange("p (h t n) -> p h t n", h=2, t=2))
    ov = V(_tl(out), _ap(out).rearrange("p (h t n) -> p h t n", h=2, t=2))
    cv = V(_tl(cos), _ap(cos).rearrange("p (h n) -> p h n", h=2))
    sv = V(_tl(sin), _ap(sin).rearrange("p (h n) -> p h n", h=2))
    a = V(t1, t1.h[:, 0:2 * n].rearrange("p (h n) -> p h n", h=2))
    b = V(t2, t2.h[:, 0:2 * n].rearrange("p (h n) -> p h n", h=2))
    x1, x2 = xv[:, :, 0, :], xv[:, :, 1, :]
    c.tt(a, x1, cv, ALU.mult, e=e)
    c.tt(b, x2, sv, ALU.mult, e=e)
    c.tt(ov[:, :, 0, :], a, b, ALU.subtract, e=e)
    c.tt(a, x1, sv, ALU.mult, e=e)
    c.tt(b, x2, cv, ALU.mult, e=e)
    c.tt(ov[:, :, 1, :], a, b, ALU.add, e=e)


def attention(c, cm, qTs, kTs, vt, q_tiles, k_lo, k_hi, scale, bufs, out_cb):
    PT, pss, pst, pso = (bufs[k] for k in ("PT", "pss", "pst", "pso"))
    identb = bufs["identb"]
    nk = k_hi - k_lo
    nch = (nk + 511) // 512
    nkt = nk // 128
    dv1 = vt.h.shape[-1]
    dv = dv1 - 1
    ng = (nkt + 3) // 4
    for qi, qt in enumerate(q_tiles):
        q0 = qt * 128
        bi = bufs["ctr"][0] % 2
        bufs["ctr"][0] += 1
        S, Pb, cmax, mx, rsum = (bufs[k][bi] for k in ("S", "Pb", "cmax", "mx", "rsum"))
        for ch in range(nch):
            lo = k_lo + ch * 512
            w = min(512, k_hi - lo)
            p = pss[ch % len(pss)]
            for i, ((qT, r), (kT, _)) in enumerate(zip(qTs, kTs)):
                c.mm(p[:, 0:w], qT[0:r, q0:q0 + 128], kT[0:r, lo:lo + w], start=(i == 0), stop=(i == len(qTs) - 1))
            c.reduce(cmax[:, ch:ch + 1], p[:, 0:w], ALU.max)
            c.copy(S[:, ch * 512:ch * 512 + w], p[:, 0:w], e="act")
        c.reduce(mx[:], cmax[:, 0:nch], ALU.max)
        c.ts(mx[:], mx[:], -scale, None, ALU.mult)
        c.act(Pb[:, 0:nk], S[:, 0:nk], AF.Exp, bias=mx[:], scale=scale)
        po = pso[bi]

        def tr(g4):
            pt = pst[g4 % len(pst)]
            n4 = min(4, nkt - g4 * 4)
            for j in range(n4):
                kt = g4 * 4 + j
                c.transpose(pt[:, j * 128:(j + 1) * 128], Pb[:, kt * 128:(kt + 1) * 128], identb[:])

        tr(0)
        for g4 in range(ng):
            if g4 + 1 < ng:
                tr(g4 + 1)
            pt = pst[g4 % len(pst)]
            n4 = min(4, nkt - g4 * 4)
            ptb = PT[g4 % len(PT)]
            c.copy(ptb[:, 0:n4 * 128], pt[:, 0:n4 * 128], e=("dve" if g4 % 2 else "act"))
            for j in range(n4):
                kt = g4 * 4 + j
                c.mm(po[:, 0:dv1], ptb[:, j * 128:(j + 1) * 128], vt[:, k_lo // 128 + kt, :],
                     start=(kt == 0), stop=(kt == nkt - 1))
        c.emit("dve", [rsum], [po], lambda g, rsum=rsum, po=po, dv=dv: g.reciprocal(rsum.h[:], po.h[:, dv:dv + 1]))
        out_cb(qt, po, rsum)


def build_att():
    c = Ctx()
    cm = Common(c)
    din = c.dram_in
    gq, gk, gv = din("gq", [NALL, 128]), din("gk", [NALL, 128]), din("gv", [NALL, 128])
    gqn, gkn = din("gqn", [128, 128]), din("gkn", [128, 128])
    mcq, mckv, mkr = din("mcq", [NALL, 512]), din("mckv", [NALL, 256]), din("mkr", [NALL, 64])
    mqn, mkvn = din("mqn", [128, 512]), din("mkvn", [128, 256])
    wuq, wukv = din("wuq", [512, 192]), din("wukv", [256, 256])
    rq, rk, rv, rg = din("rq", [NALL, 64]), din("rk", [NALL, 64]), din("rv", [NALL, 128]), din("rg", [NALL, 128])
    rpar, rgain = din("rpar", [128, 2]), din("rgain", [128, 128])
    cos128, sin128 = din("cos128", [NLAT, 64]), din("sin128", [NLAT, 64])
    cos64, sin64 = din("cos64", [NLAT, 32]), din("sin64", [NLAT, 32])
    rconst = din("rconst", [128, 6, 128])
    rcol = din("rcol", [128, 2])
    o_gqa, o_mla, o_ret = c.dram_out("o_gqa", [NALL, 128]), c.dram_out("o_mla", [NALL, 128]), c.dram_out("o_ret", [NALL, 128])

    QT = c.sb([128, NALL], BF16, "QT")
    KTb = c.sb([128, NALL], BF16, "KT")
    VT = c.sb([128, NTA, 129], BF16, "VT")
    c.memset(VT[:, :, 128:129], 1.0)
    QR = c.sb([64, NALL], BF16, "QR")
    KR = c.sb([64, NALL], BF16, "KR")
    identb = c.sb([128, 128], BF16, "identb")
    c.copy(identb[:], cm.ident[:])
    bufs = {"S": [c.sb([128, NALL], F32, f"S{i}") for i in range(2)], "PT": [c.sb([128, 512], BF16, f"PT{i}") for i in range(2)],
            "Pb": [c.sb([128, NALL], BF16, f"Pb{i}") for i in range(2)], "identb": identb, "ctr": [0],
            "pss": [c.ps([128, 512]) for _ in range(2)], "pst": [c.ps([128, 512], BF16) for _ in range(2)],
            "pso": [c.ps([128, 512]) for _ in range(2)],
            "cmax": [c.sb([128, 16], F32, f"cmax{i}") for i in range(2)], "mx": [c.sb([128, 1], F32, f"mx{i}") for i in range(2)],
            "rsum": [c.sb([128, 1], F32, f"rsum{i}") for i in range(2)]}
    psx = [c.ps([128, 512]) for _ in range(2)]
    tmp = ln_tmp(c)
    xin = [c.sb([128, 512], F32, f"xin{i}") for i in range(3)]
    xa = c.sb([128, 512], F32, "xa")
    xb = c.sb([128, 512], F32, "xb")
    t1 = c.sb([128, 128], F32, "ropet1")
    t2 = c.sb([128, 128], F32, "ropet2")
    cs = [c.sb([128, 2, 64], F32, f"cs{i}") for i in range(2)]
    ob = [c.sb([128, 128], F32, f"ob{i}") for i in range(3)]
    gains = c.sb([128, 128 + 128 + 512 + 256 + 128], F32, "gains")
    G_Q, G_K, G_MQ, G_MKV, G_R = 0, 128, 256, 768, 1024
    c.dma(gains[:, G_Q:G_Q + 128], gqn[:, :])
    c.dma(gains[:, G_K:G_K + 128], gkn[:, :])
    c.dma(gains[:, G_MQ:G_MQ + 512], mqn[:, :])
    c.dma(gains[:, G_MKV:G_MKV + 256], mkvn[:, :])
    c.dma(gains[:, G_R:G_R + 128], rgain[:, :])
    cnt = [0]

    def outer(dst):
        def cb(qt, po, rsum):
            o = ob[cnt[0] % 3]
            cnt[0] += 1
            c.ts(o[:], po[:, 0:128], rsum[:], None, ALU.mult)
            c.dma(dst[qt * 128:(qt + 1) * 128, :], o[:], q="pool")
        return cb

    def load_cs(t, cos_d, sin_d, n2, k):
        t_ = cs[k % 2]
        c.dma(t_[:, 0, 0:n2], cos_d[t * 128:(t + 1) * 128, :])
        c.dma(t_[:, 1, 0:n2], sin_d[t * 128:(t + 1) * 128, :])
        return t_

    def tr_to(dst, src, rows, t):
        p = psx[t % 2]
        c.transpose(p[0:rows, 0:128], src, cm.ident[:])
        c.copy(dst[0:rows, t * 128:(t + 1) * 128], p[0:rows, 0:128], e="act")

    import os
    SECT = os.environ.get("ATT_SECT", "gmr")
    for t in range(NTA if "g" in SECT else 0):
        xq, xk = xin[0], xin[1]
        c.dma(xq[:, 0:128], gq[t * 128:(t + 1) * 128, :])
        c.dma(xk[:, 0:128], gk[t * 128:(t + 1) * 128, :])
        c.dma(VT[:, t, 0:128], gv[t * 128:(t + 1) * 128, :], q="pool")
        for (x, g0, dstT) in ((xq, G_Q, QT), (xk, G_K, KTb)):
            ms_rstd(c, x[:, 0:128], 128, tmp)
            c.stt(xa[:, 0:128], x[:, 0:128], tmp["rs"][:], gains[:, g0:g0 + 128], ALU.mult, ALU.mult)
            if t < NTL:
                cst = load_cs(t, cos128, sin128, 64, t)
                rope_tm(c, xa[:, 0:128], xb[:, 0:128], cst[:, 0, 0:64], cst[:, 1, 0:64], 32, t1, t2)
                tr_to(dstT, xb[:, 0:128], 128, t)
            else:
                tr_to(dstT, xa[:, 0:128], 128, t)
    if "g" in SECT:
        nq = int(os.environ.get("GQA_NQ", NTL))
        attention(c, cm, [(QT, 128)], [(KTb, 128)], VT, list(range(nq)), 0, NALL, 128 ** -0.5, bufs, outer(o_gqa))
        if nq == NTL:
            attention(c, cm, [(QT, 128)], [(KTb, 128)], VT, [32, 33], NLAT, NALL, 128 ** -0.5, bufs, outer(o_gqa))

    wq = c.sb([128, 4, 192], F32, "wq")
    wkv = c.sb([128, 2, 256], F32, "wkv")
    c.dma(wq[:], wuq.rearrange("(k p) n -> p k n", p=128))
    c.dma(wkv[:], wukv.rearrange("(k p) n -> p k n", p=128))
    cT = c.sb([128, 4, 128], F32, "cT")
    for t in range(NTA if "m" in SECT else 0):
        xq, xkv, xr = xin[0], xin[1], xin[2]
        c.dma(xq[:, 0:512], mcq[t * 128:(t + 1) * 128, :])
        c.dma(xkv[:, 0:256], mckv[t * 128:(t + 1) * 128, :])
        c.dma(xr[:, 0:64], mkr[t * 128:(t + 1) * 128, :])
        if t < NTL:
            cst = load_cs(t, cos64, sin64, 32, t)
        ms_rstd(c, xq[:, 0:512], 512, tmp)
        c.stt(xa[:, 0:512], xq[:, 0:512], tmp["rs"][:], gains[:, G_MQ:G_MQ + 512], ALU.mult, ALU.mult)
        for k in range(4):
            p = psx[k % 2]
            c.transpose(p[:, 0:128], xa[:, k * 128:(k + 1) * 128], cm.ident[:])
            c.copy(cT[:, k, :], p[:, 0:128], e="act")
        p = psx[0]
        for k in range(4):
            c.mm(p[:, 0:128], wq[:, k, 0:128], cT[:, k, :], start=(k == 0), stop=(k == 3))
        c.copy(QT[:, t * 128:(t + 1) * 128], p[:, 0:128], e="act")
        p = psx[1]
        for k in range(4):
            c.mm(p[:, 0:64], cT[:, k, :], wq[:, k, 128:192], start=(k == 0), stop=(k == 3))
        c.copy(xb[:, 0:64], p[:, 0:64])
        if t < NTL:
            rope_tm(c, xb[:, 0:64], xb[:, 64:128], cst[:, 0, 0:32], cst[:, 1, 0:32], 16, t1, t2)
            tr_to(QR, xb[:, 64:128], 64, t)
        else:
            tr_to(QR, xb[:, 0:64], 64, t)
        ms_rstd(c, xkv[:, 0:256], 256, tmp)
        c.stt(xa[:, 0:256], xkv[:, 0:256], tmp["rs"][:], gains[:, G_MKV:G_MKV + 256], ALU.mult, ALU.mult)
        for k in range(2):
            p = psx[k % 2]
            c.transpose(p[:, 0:128], xa[:, k * 128:(k + 1) * 128], cm.ident[:])
            c.copy(cT[:, k, :], p[:, 0:128], e="act")
        p = psx[0]
        for k in range(2):
            c.mm(p[:, 0:128], wkv[:, k, 0:128], cT[:, k, :], start=(k == 0), stop=(k == 1))
        c.copy(KTb[:, t * 128:(t + 1) * 128], p[:, 0:128], e="act")
        p = psx[1]
        for k in range(2):
            c.mm(p[:, 0:128], cT[:, k, :], wkv[:, k, 128:256], start=(k == 0), stop=(k == 1))
        c.copy(VT[:, t, 0:128], p[:, 0:128])
        if t < NTL:
            rope_tm(c, xr[:, 0:64], xb[:, 128:192], cst[:, 0, 0:32], cst[:, 1, 0:32], 16, t1, t2)
            tr_to(KR, xb[:, 128:192], 64, t)
        else:
            tr_to(KR, xr[:, 0:64], 64, t)
    sc = 192 ** -0.5
    if "m" in SECT:
        attention(c, cm, [(QT, 128), (QR, 64)], [(KTb, 128), (KR, 64)], VT, list(range(NTL)), 0, NALL, sc, bufs, outer(o_mla))
        attention(c, cm, [(QT, 128), (QR, 64)], [(KTb, 128), (KR, 64)], VT, [32, 33], NLAT, NALL, sc, bufs, outer(o_mla))
    if "r" not in SECT:
        c.finish("pool")
        return c

    RQ, RK = bufs["S"][0], bufs["S"][1]
    RV = c.sb([128, NTA, 128], F32, "RV")
    rc = c.sb([128, 6, 128], F32, "rc")
    rcl = c.sb([128, 2], F32, "rcl")
    rp = c.sb([128, 2], F32, "rp")
    c.dma(rc[:], rconst[:, :, :])
    c.dma(rcl[:], rcol[:, :])
    c.dma(rp[:], rpar[:, :])
    lg = c.sb([128, 2], F32, "lg")
    c.act(lg[:], rp[:], AF.Exp)
    c.ts(lg[:], lg[:], -1.0, None, ALU.mult)
    DT = c.sb([128, 128], F32, "DT")
    dtmp = c.sb([128, 128], F32, "dtmp")
    c.act(DT[:], rc[:, 0, :], AF.Exp, scale=lg[:, 0:1])
    c.tt(DT[:], DT[:], rc[:, 2, :], ALU.mult)
    c.act(dtmp[:], rc[:, 1, :], AF.Exp, scale=lg[:, 1:2])
    c.tt(dtmp[:], dtmp[:], rc[:, 3, :], ALU.mult)
    c.tt(DT[:], DT[:], dtmp[:], ALU.add)
    dq = c.sb([128, 2, 128], F32, "dq")
    c.act(dq[:, 0, :], rc[:, 4, :], AF.Exp, scale=lg[:, 0:1])
    c.act(dq[:, 1, :], rc[:, 5, :], AF.Exp, scale=lg[:, 1:2])
    dk = c.sb([128, 2], F32, "dk")
    c.act(dk[:, 0:1], rcl[:, 0:1], AF.Exp, scale=lg[:, 0:1])
    c.act(dk[:, 1:2], rcl[:, 1:2], AF.Exp, scale=lg[:, 1:2])
    gcd = c.sb([128, 2], F32, "gcd")
    c.ts(gcd[:], lg[:], 128.0, None, ALU.mult)
    c.act(gcd[:], gcd[:], AF.Exp)
    Ktm = c.sb([128, NTA, 64], F32, "Ktm")
    Sf = c.sb([64, NTA + 1, 128], F32, "Sf")
    Sb = c.sb([64, NTA + 1, 128], F32, "Sb")
    kd = c.sb([128, 2, 64], F32, "kd")
    fwd_order = [32, 33] + list(range(NTL))
    for t in range(NTA):
        xq, xk = xin[0], xin[1]
        c.dma(xq[:, 0:64], rq[t * 128:(t + 1) * 128, :])
        c.dma(xk[:, 0:64], rk[t * 128:(t + 1) * 128, :])
        c.dma(RV[:, t, :], rv[t * 128:(t + 1) * 128, :])
        c.ts(xk[:, 0:64], xk[:, 0:64], 0.125, None, ALU.mult)
        if t < NTL:
            cst = load_cs(t, cos64, sin64, 32, t)
            rope_tm(c, xq[:, 0:64], xb[:, 0:64], cst[:, 0, 0:32], cst[:, 1, 0:32], 16, t1, t2)
            rope_tm(c, xk[:, 0:64], Ktm[:, t, :], cst[:, 0, 0:32], cst[:, 1, 0:32], 16, t1, t2)
            tr_to(RQ, xb[:, 0:64], 64, t)
        else:
            c.copy(Ktm[:, t, :], xk[:, 0:64])
            tr_to(RQ, xq[:, 0:64], 64, t)
        tr_to(RK, Ktm[:, t, :], 64, t)
    bwd_order = [33, 32] + list(range(NTL - 1, -1, -1))
    for (S_, order, col) in ((Sf, fwd_order, 0), (Sb, bwd_order, 1)):
        c.memset(S_[:, 0, :], 0.0)
        for i, t in enumerate(order):
            c.ts(kd[:, col, :], Ktm[:, t, :], dk[:, col:col + 1], None, ALU.mult)
            p = psx[i % 2]
            c.mm(p[0:64, 0:128], kd[:, col, :], RV[:, t, :])
            c.stt(S_[:, i + 1, :], S_[:, i, :], gcd[0:64, col:col + 1], p[0:64, 0:128], ALU.mult, ALU.add)
    fpos = {t: i for i, t in enumerate(fwd_order)}
    bpos = {t: i for i, t in enumerate(bwd_order)}
    MT = [c.sb([128, 128], F32, f"MT{i}") for i in range(2)]
    qd = [c.sb([64, 2, 128], F32, f"qd{i}") for i in range(2)]
    gt = [c.sb([128, 128], F32, f"gt{i}") for i in range(2)]
    for t in range(NTA):
        sl = slice(t * 128, (t + 1) * 128)
        p = psx[0]
        c.mm(p[:, 0:128], RK[0:64, sl], RQ[0:64, sl])
        m = MT[t % 2]
        c.tt(m[:], p[:, 0:128], DT[:], ALU.mult)
        q_ = qd[t % 2]
        c.tt(q_[:, 0, :], RQ[0:64, sl], dq[0:64, 0, :], ALU.mult)
        c.tt(q_[:, 1, :], RQ[0:64, sl], dq[0:64, 1, :], ALU.mult)
        po = psx[1]
        c.mm(po[:, 0:128], m[:], RV[:, t, :], start=True, stop=False)
        c.mm(po[:, 0:128], q_[:, 0, :], Sf[:, fpos[t], :], start=False, stop=False)
        c.mm(po[:, 0:128], q_[:, 1, :], Sb[:, bpos[t], :], start=False, stop=True)
        g_ = gt[t % 2]
        c.dma(g_[:], rg[sl, :])
        o = ob[cnt[0] % 3]
        cnt[0] += 1
        c.copy(xa[:, 0:128], po[:, 0:128])
        ln_hat(c, cm, xa[:, 0:128], xb[:, 0:128], width=128, tmp=tmp)
        c.tt(xb[:, 0:128], xb[:, 0:128], gains[:, G_R:G_R + 128], ALU.mult)
        c.act(xa[:, 128:256], g_[:], AF.Sigmoid)
        c.tt(xa[:, 128:256], xa[:, 128:256], g_[:], ALU.mult)
        c.tt(o[:], xb[:, 0:128], xa[:, 128:256], ALU.mult)
        c.dma(o_ret[sl, :], o[:], q="pool")
    c.finish("pool")
    return c


def rope_tables(dim):
    n = dim // 4
    inv = (np.float32(10000.0) ** (-np.arange(n, dtype=np.float32) / np.float32(n))).astype(np.float32)
    t = np.arange(NLAT)
    row = (t // 64).astype(np.float32)
    col = (t % 64).astype(np.float32)
    ang = np.concatenate([row[:, None] * inv[None, :], col[:, None] * inv[None, :]], 1).astype(np.float32)
    return np.cos(ang).astype(np.float32), np.sin(ang).astype(np.float32)


def ret_consts():
    j = np.arange(128, dtype=np.float32)[:, None]
    t = np.arange(128, dtype=np.float32)[None, :]
    z = np.zeros((128, 128), np.float32)
    rc = np.stack([np.maximum(t - j, 0), np.maximum(j - t, 0), (t >= j).astype(np.float32),
                   (j > t).astype(np.float32), t + 1 + z, 128 - t + z], 1).astype(np.float32)
    rcol = np.stack([127 - j[:, 0], j[:, 0]], 1).astype(np.float32)
    return np.ascontiguousarray(rc), np.ascontiguousarray(rcol)


def rep(v, n=128):
    return np.ascontiguousarray(np.broadcast_to(np.asarray(v, np.float32).reshape(1, -1), (n, np.size(v))))


def host_att(pl, pc, P, l):
    c128, s128 = rope_tables(128)
    c64, s64 = rope_tables(64)
    rc, rcol = ret_consts()
    ims = []
    for j in range(NCORES):
        b, h = j // 4, j % 4
        pa = np.concatenate([pl[b], pc[b]], 0)
        kv = h // 2
        cut = lambda o, w: np.ascontiguousarray(pa[:, o:o + w])
        ims.append({
            "ident": IDENT,
            "gq": cut(512 + h * 128, 128), "gk": cut(1024 + kv * 128, 128), "gv": cut(1280 + kv * 128, 128),
            "gqn": rep(P["gqa_q_norm"][l]), "gkn": rep(P["gqa_k_norm"][l]),
            "mcq": cut(3072, 512), "mckv": cut(3584, 256), "mkr": cut(3840, 64),
            "mqn": rep(P["mla_q_norm"][l]), "mkvn": rep(P["mla_kv_norm"][l]),
            "wuq": np.ascontiguousarray(P["mla_w_uq"][l][:, h * 192:(h + 1) * 192]),
            "wukv": np.ascontiguousarray(P["mla_w_ukv"][l][:, h * 256:(h + 1) * 256]),
            "rq": cut(1536 + h * 64, 64), "rk": cut(1792 + h * 64, 64),
            "rv": cut(2048 + h * 128, 128), "rg": cut(2560 + h * 128, 128),
            "rpar": rep([P["ret_decay_f"][l][h], P["ret_decay_b"][l][h]]),
            "rgain": rep(P["ret_norm"][l][h * 128:(h + 1) * 128]),
            "cos128": c128, "sin128": s128, "cos64": c64, "sin64": s64, "rconst": rc, "rcol": rcol,
        })
    res = run(prog("att", build_att), ims)
    outs = {}
    for nm in ("o_gqa", "o_ret", "o_mla"):
        lat = np.zeros((2, NLAT, 512), np.float32)
        ctx = np.zeros((2, 256, 512), np.float32)
        for j in range(NCORES):
            b, h = j // 4, j % 4
            lat[b, :, h * 128:(h + 1) * 128] = res[j][nm][:NLAT]
            ctx[b, :, h * 128:(h + 1) * 128] = res[j][nm][NLAT:]
        outs[nm] = (lat, ctx)
    return outs


PI = float(np.pi)


def rsin(c, out, x, kf, ki):
    c.ts(kf, x, 1.0 / (2 * PI), None, ALU.mult)
    c.copy(ki, kf)
    c.copy(kf, ki)
    c.stt(kf, kf, -2 * PI, x, ALU.mult, ALU.add)
    c.ts(kf, kf, PI, -PI, ALU.min, ALU.max)
    c.act(out, kf, AF.Sin)


def build_s5():
    c = Ctx()
    cm = Common(c)
    uT = c.dram_in("uT", [2, 2, 64, NALL])
    apar = c.dram_in("apar", [128, 2, 2, 3])
    bw_d = c.dram_in("bw", [128, 2, 2, 32])
    cw_d = c.dram_in("cw", [128, 2, 2, 32])
    tidx_d = c.dram_in("tidx", [128, NALL])
    yT = c.dram_out("yT", [2, 2, 64, NALL])
    T_ = NALL
    tidx = c.sb([128, T_], F32, "tidx_sb")
    c.dma(tidx[:], tidx_d[:, :])
    ap_ = c.sb([128, 2, 2, 3], F32, "apar_sb")
    bw = c.sb([128, 2, 2, 32], F32, "bw_sb")
    cw = c.sb([128, 2, 2, 32], F32, "cw_sb")
    c.dma(ap_[:], apar[:, :, :, :])
    c.dma(bw[:], bw_d[:, :, :, :])
    c.dma(cw[:], cw_d[:, :, :, :])
    ncw = c.sb([128, 2, 32], F32, "ncw")
    c.ts(ncw[:], cw[:, :, 1, :], -1.0, None, ALU.mult)
    tabc = c.sb([128, T_], F32, "tabc")
    tabs = c.sb([128, T_], F32, "tabs")
    A1 = c.sb([128, T_], F32, "A1")
    A2 = c.sb([128, T_], F32, "A2")
    G1 = c.sb([128, T_], F32, "G1")
    G2 = c.sb([128, T_], F32, "G2")
    RT = c.sb([128, T_], F32, "RT")
    uts = [c.sb([32, 512], F32, f"ut{i}") for i in range(3)]
    KI = c.sb([128, T_], I32, "KI")
    ski = c.sb([128, 2], I32, "ski")
    sc = c.sb([128, 24], F32, "s5sc")
    pib = c.sb([128, 1], F32, "pib")
    c.memset(pib[:], PI)
    bb = c.sb([128, 2, 32], F32, "bb")
    bbT = c.sb([32, 2, 128], F32, "bbT")
    m1 = [c.sb([128, 512], F32, f"m1_{i}") for i in range(2)]
    m2 = [c.sb([128, 512], F32, f"m2_{i}") for i in range(2)]
    yo = [c.sb([32, 512], F32, f"yo{i}") for i in range(2)]
    hre = [c.sb([128, 512], F32, f"hre{i}") for i in range(2)]
    him = [c.sb([128, 512], F32, f"him{i}") for i in range(2)]
    m3 = [c.sb([128, 512], F32, f"m3_{i}") for i in range(2)]
    m4 = [c.sb([128, 512], F32, f"m4_{i}") for i in range(2)]
    psr = [c.ps([128, 512]) for _ in range(2)]
    psi = [c.ps([128, 512]) for _ in range(2)]
    psy = [c.ps([128, 512]) for _ in range(2)]
    pst = c.ps([128, 512])
    S = lambda i: sc[:, i:i + 1]
    nblk = (T_ + 511) // 512
    k = 0
    for pt in range(2):
        for d in range(2):
            a_re, a_im, ldt = ap_[:, pt, d, 0:1], ap_[:, pt, d, 1:2], ap_[:, pt, d, 2:3]
            c.act(S(0), ldt, AF.Exp)
            c.tt(S(1), a_re, S(0), ALU.mult)
            c.act(S(1), S(1), AF.Exp)
            c.tt(S(2), a_im, S(0), ALU.mult)
            rsin(c, S(6), S(2), S(8), ski[:, 0:1])
            c.ts(S(9), S(2), PI / 2, None, ALU.add)
            rsin(c, S(7), S(9), S(8), ski[:, 0:1])
            c.tt(S(10), S(1), S(7), ALU.mult)
            c.tt(S(11), S(1), S(6), ALU.mult)
            c.tt(S(12), a_re, a_re, ALU.mult)
            c.tt(S(13), a_im, a_im, ALU.mult)
            c.tt(S(12), S(12), S(13), ALU.add)
            c.emit("dve", [sc], [sc], lambda g: g.reciprocal(sc.h[:, 12:13], sc.h[:, 12:13]))
            c.ts(S(14), S(10), -1.0, None, ALU.add)
            c.tt(S(15), S(14), a_re, ALU.mult)
            c.tt(S(16), S(11), a_im, ALU.mult)
            c.tt(S(15), S(15), S(16), ALU.add)
            c.tt(S(15), S(15), S(12), ALU.mult)
            c.tt(S(16), S(11), a_re, ALU.mult)
            c.tt(S(17), S(14), a_im, ALU.mult)
            c.tt(S(16), S(16), S(17), ALU.subtract)
            c.tt(S(16), S(16), S(12), ALU.mult)
            c.ts(S(17), S(16), -1.0, None, ALU.mult)
            c.ts(bb[:, 0, :], bw[:, pt, 0, :], S(15), None, ALU.mult)
            c.stt(bb[:, 0, :], bw[:, pt, 1, :], S(17), bb[:, 0, :], ALU.mult, ALU.add)
            c.ts(bb[:, 1, :], bw[:, pt, 1, :], S(15), None, ALU.mult)
            c.stt(bb[:, 1, :], bw[:, pt, 0, :], S(16), bb[:, 1, :], ALU.mult, ALU.add)
            for ri in range(2):
                c.transpose(pst[0:32, ri * 128:(ri + 1) * 128], bb[:, ri, :], cm.ident[:])
            c.copy(bbT[:, 0, :], pst[0:32, 0:128], e="act")
            c.copy(bbT[:, 1, :], pst[0:32, 128:256], e="act")
            c.ts(A1[:], tidx[:], S(2), None, ALU.mult)
            rsin(c, tabs[:], A1[:], G1[:], KI[:])
            c.ts(A1[:], A1[:], PI / 2, None, ALU.add)
            rsin(c, tabc[:], A1[:], G1[:], KI[:])
            c.ts(RT[:], tidx[:], 0.0, S(1), ALU.mult, ALU.add)
            for b in range(2):
                for nb in range(nblk):
                    lo = nb * 512
                    w = min(512, T_ - lo)
                    pr, pi_ = psr[nb % 2], psi[nb % 2]
                    ut = uts[nb % 3]
                    c.dma(ut[:, 0:w], uT[d, b, pt * 32:(pt + 1) * 32, lo:lo + w])
                    c.mm(pr[:, 0:w], bbT[:, 0, :], ut[:, 0:w])
                    c.mm(pi_[:, 0:w], bbT[:, 1, :], ut[:, 0:w])
                    a, b_ = m1[nb % 2], m2[nb % 2]
                    cc, ss = tabc[:, lo:lo + w], tabs[:, lo:lo + w]
                    c.tt(a[:, 0:w], pr[:, 0:w], cc, ALU.mult)
                    c.tt(b_[:, 0:w], pi_[:, 0:w], ss, ALU.mult, e="pool" if False else "dve")
                    c.tt(A1[:, lo:lo + w], a[:, 0:w], b_[:, 0:w], ALU.add)
                    c.tt(a[:, 0:w], pi_[:, 0:w], cc, ALU.mult)
                    c.tt(b_[:, 0:w], pr[:, 0:w], ss, ALU.mult)
                    c.tt(A2[:, lo:lo + w], a[:, 0:w], b_[:, 0:w], ALU.subtract)
                c.emit("dve", [G1], [RT, A1], lambda g: g.tensor_tensor_scan(G1.h[:], RT.h[:], A1.h[:], 0.0, ALU.mult, ALU.add))
                c.emit("dve", [G2], [RT, A2], lambda g: g.tensor_tensor_scan(G2.h[:], RT.h[:], A2.h[:], 0.0, ALU.mult, ALU.add))
                for nb in range(nblk):
                    lo = nb * 512
                    w = min(512, T_ - lo)
                    e2 = "pool" if nb % 3 != 2 else "dve"
                    a, b_ = (m3[(nb // 3) % 2], m4[(nb // 3) % 2]) if e2 == "dve" else (m1[nb % 2], m2[nb % 2])
                    hr, hi = hre[nb % 2], him[nb % 2]
                    cc, ss = tabc[:, lo:lo + w], tabs[:, lo:lo + w]
                    c.tt(a[:, 0:w], G1[:, lo:lo + w], cc, ALU.mult, e=e2)
                    c.tt(b_[:, 0:w], G2[:, lo:lo + w], ss, ALU.mult, e=e2)
                    c.tt(hr[:, 0:w], a[:, 0:w], b_[:, 0:w], ALU.subtract, e=e2)
                    c.tt(a[:, 0:w], G1[:, lo:lo + w], ss, ALU.mult, e=e2)
                    c.tt(b_[:, 0:w], G2[:, lo:lo + w], cc, ALU.mult, e=e2)
                    c.tt(hi[:, 0:w], a[:, 0:w], b_[:, 0:w], ALU.add, e=e2)
                    py = psy[nb % 2]
                    c.mm(py[0:32, 0:w], cw[:, pt, 0, :], hr[:, 0:w], start=True, stop=False)
                    c.mm(py[0:32, 0:w], ncw[:, pt, :], hi[:, 0:w], start=False, stop=True)
                    o = yo[k % 2]
                    k += 1
                    c.copy(o[:, 0:w], py[0:32, 0:w], e="act")
                    c.dma(yT[d, b, pt * 32:(pt + 1) * 32, lo:lo + w], o[:, 0:w], q="sp")
    c.finish("sp")
    return c


def host_s5(pl, pc, P, l):
    ims = []
    tidx = rep(np.arange(NALL, dtype=np.float32))
    for j in range(NCORES):
        uT = np.zeros((2, 2, 64, NALL), np.float32)
        for b in range(2):
            ul = pl[b][:, j * 64:(j + 1) * 64]
            uc = pc[b][:, j * 64:(j + 1) * 64]
            uT[0, b] = np.concatenate([uc, ul], 0).T
            uT[1, b] = np.concatenate([uc[::-1], ul[::-1]], 0).T
        apar = np.zeros((128, 2, 2, 3), np.float32)
        bw = np.zeros((128, 2, 2, 32), np.float32)
        cw = np.zeros((128, 2, 2, 32), np.float32)
        for pt in range(2):
            for gl in range(2):
                g = j * 4 + pt * 2 + gl
                rows = slice(gl * 64, (gl + 1) * 64)
                for d, sfx in enumerate(("f", "b")):
                    apar[rows, pt, d, 0] = P["s5_a_re_" + sfx][l][g]
                    apar[rows, pt, d, 1] = P["s5_a_im_" + sfx][l][g]
                    apar[rows, pt, d, 2] = P["s5_log_dt_" + sfx][l][g]
                bw[rows, pt, 0, gl * 16:(gl + 1) * 16] = P["s5_b_re"][l][g]
                bw[rows, pt, 1, gl * 16:(gl + 1) * 16] = P["s5_b_im"][l][g]
                cw[rows, pt, 0, gl * 16:(gl + 1) * 16] = P["s5_c_re"][l][g].T
                cw[rows, pt, 1, gl * 16:(gl + 1) * 16] = P["s5_c_im"][l][g].T
        ims.append({"ident": IDENT, "uT": uT, "apar": apar, "bw": bw, "cw": cw, "tidx": tidx})
    res = run(prog("s5", build_s5), ims)
    outs = []
    for d in range(2):
        lat = np.zeros((2, NLAT, 512), np.float32)
        ctx = np.zeros((2, 256, 512), np.float32)
        for j in range(NCORES):
            for b in range(2):
                y = res[j]["yT"][d, b].T
                yc, yl = y[:256], y[256:]
                if d == 1:
                    yc, yl = yc[::-1], yl[::-1]
                lat[b, :, j * 64:(j + 1) * 64] = yl
                ctx[b, :, j * 64:(j + 1) * 64] = yc
        outs.append((lat, ctx))
    return outs


def build_merge_a():
    c = Ctx()
    cm = Common(c)
    x = c.dram_in("x", [NTOK, D])
    mv_d = c.dram_in("mv", [128, 4, KT])
    yf, yb, u = c.dram_in("yf", [NTOK, 512]), c.dram_in("yb", [NTOK, 512]), c.dram_in("u", [NTOK, 512])
    s5d = c.dram_in("s5d", [128, 512])
    wglu = c.dram_in("wglu", [512, 512])
    obr = c.dram_in("obr", [3, NTOK, 512])
    wbr = c.dram_in("wbr", [4, 512, D])
    wg = c.dram_in("wg", [D, 4 * D])
    bg = c.dram_in("bg", [1, 4 * D])
    m_out = c.dram_out("m", [NTOK, D])
    mv = load_mod_cols(c, mv_d, 4)
    c.ts(mv[:, 1, :], mv[:, 1, :], 1.0, None, ALU.add)
    c.ts(mv[:, 3, :], mv[:, 3, :], 1.0, None, ALU.add)
    dr = c.sb([128, 512], F32, "s5d_sb")
    c.dma(dr[:], s5d[:, :])
    wgl = c.sb([128, 4, 512], F32, "wglu_sb")
    c.dma(wgl[:], wglu.rearrange("(k p) n -> p k n", p=128))
    ones = c.sb([1, 128], F32, "ones1")
    c.memset(ones[:], 1.0)
    bgts = [c.sb([1, 512], F32, f"bg_sb{i}") for i in range(2)]
    TP = NT
    hT = c.sb([128, KT, TP * 128], BF16, "hT")
    oT = c.sb([128, 4, 4, TP * 128], BF16, "oT")
    macc = c.sb([128, TP, 512], F32, "macc")
    xt = c.sb([128, D], F32, "xt")
    tmp = ln_tmp(c)
    a = [c.sb([128, 512], F32, f"ma{i}") for i in range(4)]
    zT = c.sb([128, 4, 128], F32, "zT")
    pst = [c.ps([128, 512]) for _ in range(2)]
    psg = [c.ps([128, 512]) for _ in range(2)]
    psp = [c.ps([128, 512]) for _ in range(2)]
    wgb = [c.sb([128, KT, 512], BF16, f"wgb{i}") for i in range(2)]
    wbb = [c.sb([128, 4, 512], BF16, f"wbb{i}") for i in range(2)]
    gsb = [c.sb([128, 512], F32, f"gsb{i}") for i in range(2)]
    wgv = wg.rearrange("(k p) n -> p k n", p=128)
    for p0 in range(0, NT, TP):
        tiles = list(range(p0, min(NT, p0 + TP)))
        for li, t in enumerate(tiles):
            rows = slice(t * 128, (t + 1) * 128)
            c.dma(xt[:], x[rows, :])
            ln_hat(c, cm, xt[:], xt[:], tmp=tmp)
            j = 0 if t < 8 else 2
            to_fm(c, cm, xt, hT, li, pst, scale_cols=mv[:, j + 1, :], bias_cols=mv[:, j, :])
            c.dma(a[0][:], yf[rows, :])
            c.dma(a[1][:], yb[rows, :])
            c.dma(a[2][:], u[rows, :])
            c.tt(a[0][:], a[0][:], a[1][:], ALU.add)
            c.tt(a[2][:], a[2][:], dr[:], ALU.mult)
            c.tt(a[0][:], a[0][:], a[2][:], ALU.add)
            c.tt(a[1][:], a[0][:], a[0][:], ALU.mult)
            c.ts(a[1][:], a[1][:], 0.044715, 1.0, ALU.mult, ALU.add)
            c.tt(a[1][:], a[1][:], a[0][:], ALU.mult)
            c.act(a[1][:], a[1][:], AF.Tanh, scale=0.7978845608028654)
            c.stt(a[1][:], a[1][:], 1.0, a[0][:], ALU.add, ALU.mult)
            c.ts(a[1][:], a[1][:], 0.5, None, ALU.mult)
            for k in range(4):
                p = pst[k % 2]
                c.transpose(p[:, 0:128], a[1][:, k * 128:(k + 1) * 128], cm.ident[:])
                c.copy(zT[:, k, :], p[:, 0:128], e="act")
            p = psg[0]
            for k in range(4):
                c.mm(p[:, 0:512], zT[:, k, :], wgl[:, k, :], start=(k == 0), stop=(k == 3))
            c.act(a[2][:], p[:, 0:512], AF.Sigmoid)
            c.tt(a[3][:], a[1][:], a[2][:], ALU.mult)
            for k in range(4):
                p = pst[k % 2]
                c.transpose(p[:, 0:128], a[3][:, k * 128:(k + 1) * 128], cm.ident[:])
                c.copy(oT[:, 0, k, li * 128:(li + 1) * 128], p[:, 0:128], e="act")
            for br in range(3):
                c.dma(a[0][:], obr[br, rows, :])
                for k in range(4):
                    p = pst[k % 2]
                    c.transpose(p[:, 0:128], a[0][:, k * 128:(k + 1) * 128], cm.ident[:])
                    c.copy(oT[:, br + 1, k, li * 128:(li + 1) * 128], p[:, 0:128], e="act")
        i = 0
        for nb in range(4):
            for k in range(4):
                wgt, wbt = wgb[i % 2], wbb[i % 2]
                i += 1
                col = k * D + nb * 512
                bgt = bgts[i % 2]
                c.dma(bgt[:], bg[:, col:col + 512])
                c.dma(wgt[:], wgv[:, :, col:col + 512], q="pool")
                c.dma(wbt[:], wbr[k, :, nb * 512:(nb + 1) * 512].rearrange("(k p) n -> p k n", p=128), q="pool")
                for li, t in enumerate(tiles):
                    pg, pp = psg[li % 2], psp[li % 2]
                    for kk in range(KT):
                        c.mm(pg[:, 0:512], hT[:, kk, li * 128:(li + 1) * 128], wgt[:, kk, :], start=(kk == 0), stop=False)
                    c.mm(pg[:, 0:512], ones[:, :], bgt[:, :], start=False, stop=True)
                    g = gsb[li % 2]
                    c.act(g[:], pg[:, 0:512], AF.Sigmoid)
                    for kk in range(4):
                        c.mm(pp[:, 0:512], oT[:, k, kk, li * 128:(li + 1) * 128], wbt[:, kk, :], start=(kk == 0), stop=(kk == 3))
                    if k == 0:
                        c.tt(macc[:, li, :], g[:], pp[:, 0:512], ALU.mult)
                    else:
                        c.tt(g[:], g[:], pp[:, 0:512], ALU.mult)
                        c.tt(macc[:, li, :], macc[:, li, :], g[:], ALU.add)
            for li, t in enumerate(tiles):
                c.dma(m_out[t * 128:(t + 1) * 128, nb * 512:(nb + 1) * 512], macc[:, li, :], q="sp")
    c.finish("sp")
    return c


def host_merge_a(x_lat, x_ctx, mod, pl, pc, s5o, atto, P, l):
    xs = tok_shard(x_lat, x_ctx)
    (lf, cf), (lb, cb) = s5o
    yfs, ybs = tok_shard(lf, cf), tok_shard(lb, cb)
    us = tok_shard(pl[:, :, :512], pc[:, :, :512])
    brs = [tok_shard(*atto[nm]) for nm in ("o_gqa", "o_ret", "o_mla")]
    ims = []
    for j in range(NCORES):
        ims.append({"ident": IDENT, "x": xs[j], "mv": mod_cols(mod, j, [0, 1]),
                    "yf": yfs[j], "yb": ybs[j], "u": us[j], "s5d": rep(P["s5_d"][l]),
                    "wglu": P["s5_w_glu"][l], "obr": np.stack([b_[j] for b_ in brs], 0),
                    "wbr": P["w_branch"][l], "wg": P["w_gate"][l], "bg": P["b_gate"][l][None, :]})
    res = run(prog("merge_a", build_merge_a), ims)
    return tok_unshard([r["m"] for r in res], D)


ALPHA = float((2 * 2) ** 0.25)


def build_merge_b():
    c = Ctx()
    cm = Common(c)
    m = c.dram_in("m", [NTOK, D])
    x = c.dram_in("x", [NTOK, D])
    w = c.dram_in("w_out", [D, D])
    reps_d = c.dram_in("reps", [128, 8, D])
    rw_d = c.dram_in("rw", [D, 16])
    x1_o = c.dram_out("x1", [NTOK, D])
    h2_o = c.dram_out("h2", [NTOK, D])
    aff_o = c.dram_out("aff", [NTOK, 16])
    reps = c.sb([128, 8, D], F32, "reps_sb")
    for i in range(8):
        c.dma(reps[:, i, :], reps_d[:, i, :])
    c.ts(reps[:, 4, :], reps[:, 4, :], 1.0, None, ALU.add)
    c.ts(reps[:, 6, :], reps[:, 6, :], 1.0, None, ALU.add)
    rw = c.sb([128, KT, 16], F32, "rw_sb")
    c.dma(rw[:], rw_d.rearrange("(k p) n -> p k n", p=128))
    TP = 5
    mt = c.sb([128, D], F32, "mt")
    X = c.sb([128, TP, D], F32, "X")
    xh = c.sb([128, D], F32, "xh")
    x1t = c.sb([128, D], F32, "x1t")
    mT = c.sb([128, KT, TP * 128], BF16, "mT")
    rT = c.sb([128, KT, 128], F32, "rT")
    tmp = ln_tmp(c)
    tb = [c.sb([128, 512], F32, f"tb{i}") for i in range(2)]
    sm = c.sb([128, 40], F32, "sm")
    wbufs = [c.sb([128, KT, 512], BF16, f"wb{i}") for i in range(2)]
    pst = [c.ps([128, 512]) for _ in range(2)]
    pso = [c.ps([128, 512]) for _ in range(2)]
    psr = c.ps([128, 512])
    for p0 in range(0, NT, TP):
        tiles = list(range(p0, min(NT, p0 + TP)))
        for li, t in enumerate(tiles):
            rows = slice(t * 128, (t + 1) * 128)
            c.dma(mt[:], m[rows, :])
            c.dma(X[:, li, :], x[rows, :])
            to_fm(c, cm, mt, mT, li, pst)

        def consume(li, nb, lo, bw, p, tiles=tiles):
            g1 = reps[:, 0 if tiles[li] < 8 else 1, :]
            b_ = tb[(li + nb) % 2]
            c.tt(b_[:, 0:bw], p[:, 0:bw], g1[:, lo:lo + bw], ALU.mult)
            c.stt(X[:, li, lo:lo + bw], X[:, li, lo:lo + bw], ALU_ALPHA, b_[:, 0:bw], ALU.mult, ALU.add)

        stream_linear(c, mT, w, D, len(tiles), consume, wbufs, pso)
        for li, t in enumerate(tiles):
            rows = slice(t * 128, (t + 1) * 128)
            lat = t < 8
            ln_hat(c, cm, X[:, li, :], xh[:], tmp=tmp)
            c.tt(xh[:], xh[:], reps[:, 2, :], ALU.mult)
            c.tt(x1t[:], xh[:], reps[:, 3, :], ALU.add)
            c.dma(x1_o[rows, :], x1t[:], q="sp")
            ln_hat(c, cm, x1t[:], xh[:], tmp=tmp)
            c.tt(xh[:], xh[:], reps[:, 4 if lat else 6, :], ALU.mult)
            c.tt(mt[:], xh[:], reps[:, 5 if lat else 7, :], ALU.add)
            c.dma(h2_o[rows, :], mt[:], q="sp")
            to_fm(c, cm, mt, rT, 0, pst)
            for k in range(KT):
                c.mm(psr[:, 0:16], rT[:, k, :], rw[:, k, :], start=(k == 0), stop=(k == KT - 1))
            c.copy(sm[:, 0:16], psr[:, 0:16])
            c.reduce(sm[:, 32:33], sm[:, 0:16], ALU.max)
            c.ts(sm[:, 32:33], sm[:, 32:33], -1.0, None, ALU.mult)
            c.act(sm[:, 0:16], sm[:, 0:16], AF.Exp, bias=sm[:, 32:33], scale=1.0)
            c.reduce(sm[:, 33:34], sm[:, 0:16], ALU.add)
            c.emit("dve", [sm], [sm], lambda g: g.reciprocal(sm.h[:, 34:35], sm.h[:, 33:34]))
            c.ts(sm[:, 16:32], sm[:, 0:16], sm[:, 34:35], None, ALU.mult)
            c.dma(aff_o[rows, :], sm[:, 16:32], q="sp")
    c.finish("sp")
    return c


ALU_ALPHA = ALPHA


def host_merge_b(m_lat, m_ctx, x_lat, x_ctx, mod, P, l):
    ms = tok_shard(m_lat, m_ctx)
    xs = tok_shard(x_lat, x_ctx)
    ims = []
    for j in range(NCORES):
        b = j // 4
        seg = lambda r_, i: mod[r_, i * D:(i + 1) * D]
        reps = np.stack([rep(seg(b, 2)), rep(seg(2, 2)), rep(P["ln1_g"][l]), rep(P["ln1_b"][l]),
                         rep(seg(b, 4)), rep(seg(b, 3)), rep(seg(2, 4)), rep(seg(2, 3))], 1)
        ims.append({"ident": IDENT, "m": ms[j], "x": xs[j], "w_out": P["w_out"][l],
                    "reps": np.ascontiguousarray(reps), "rw": P["router_w"][l]})
    res = run(prog("merge_b", build_merge_b), ims)
    return (tok_unshard([r["x1"] for r in res], D), tok_unshard([r["h2"] for r in res], D),
            tok_unshard([r["aff"] for r in res], 16))


CAP_L, CAP_C = 512, 32


def build_topk():
    c = Ctx()
    a_l = c.dram_in("affT", [32, NLAT])
    a_c = c.dram_in("affcT", [32, 256])
    g_l, i_l = c.dram_out("gate", [32, CAP_L]), c.dram_out("idx", [32, CAP_L], U32)
    g_c, i_c = c.dram_out("gatec", [32, CAP_C]), c.dram_out("idxc", [32, CAP_C], U32)
    for (src, n, cap, go, io, tag) in ((a_l, NLAT, CAP_L, g_l, i_l, "l"), (a_c, 256, CAP_C, g_c, i_c, "c")):
        w = c.sb([32, n], F32, "work" + tag)
        gv = c.sb([32, cap], F32, "gv" + tag)
        iv = c.sb([32, cap], U32, "iv" + tag)
        c.dma(w[:], src[:, :])
        for r in range(cap // 8):
            sl = slice(r * 8, (r + 1) * 8)
            c.emit("dve", [gv], [w], lambda g, sl=sl, gv=gv, w=w: g.max(out=gv.h[:, sl], in_=w.h[:]))
            c.emit("dve", [iv], [gv, w], lambda g, sl=sl, gv=gv, iv=iv, w=w: g.max_index(out=iv.h[:, sl], in_max=gv.h[:, sl], in_values=w.h[:]))
            c.emit("dve", [w], [gv, w], lambda g, sl=sl, gv=gv, w=w: g.match_replace(out=w.h[:], in_to_replace=gv.h[:, sl], in_values=w.h[:], imm_value=-1.0))
        c.dma(go[:, :], gv[:], q="pool")
        c.dma(io[:, :], iv[:], q="pool")
    c.finish("pool")
    return c


def host_topk(aff_l, aff_c):
    affT = np.ascontiguousarray(aff_l.transpose(0, 2, 1).reshape(32, NLAT))
    affcT = np.ascontiguousarray(aff_c.transpose(0, 2, 1).reshape(32, 256))
    res = run(prog("topk", build_topk), [{"affT": affT, "affcT": affcT}] * NCORES)[0]
    return (res["gate"].reshape(2, 16, CAP_L), res["idx"].reshape(2, 16, CAP_L).astype(np.int64),
            res["gatec"].reshape(2, 16, CAP_C), res["idxc"].reshape(2, 16, CAP_C).astype(np.int64))


FF = 1024
ER = NTOK


def build_expert():
    c = Ctx()
    cm = Common(c)
    xs = c.dram_in("xs", [2, ER, D])
    gt_d = c.dram_in("gt", [128, 2, NT])
    wg_d, wu_d, wd_d = c.dram_in("wg", [2, D, FF]), c.dram_in("wu", [2, D, FF]), c.dram_in("wd", [2, FF, D])
    y = c.dram_out("y", [2, ER, D])
    gt = c.sb([128, 2, NT], F32, "gt_sb")
    c.dma(gt[:], gt_d[:, :, :])
    xT = c.sb([128, KT, ER], BF16, "xT")
    hT = c.sb([128, 8, ER], BF16, "hmT")
    xt = [c.sb([128, D], F32, f"xt{i}") for i in range(2)]
    wgb = [c.sb([128, KT, 128], BF16, f"wgb{i}") for i in range(2)]
    wub = [c.sb([128, KT, 128], BF16, f"wub{i}") for i in range(2)]
    wdb = [c.sb([128, 8, 512], BF16, f"wdb{i}") for i in range(2)]
    sg = [c.sb([128, 512], F32, f"sg{i}") for i in range(2)]
    ob = [c.sb([128, 512], F32, f"ob{i}") for i in range(3)]
    pst = [c.ps([128, 512]) for _ in range(2)]
    psa = [c.ps([128, 512]) for _ in range(2)]
    psu = [c.ps([128, 512]) for _ in range(2)]
    pso = [c.ps([128, 512]) for _ in range(2)]
    chunks = [(0, 512), (512, 512), (1024, 128)]
    k0 = 0
    for e in range(2):
        for t in range(NT):
            x_ = xt[t % 2]
            c.dma(x_[:], xs[e, t * 128:(t + 1) * 128, :])
            to_fm(c, cm, x_, xT, t, pst)
        for fb in range(FF // 128):
            wg_, wu_ = wgb[fb % 2], wub[fb % 2]
            c.dma(wg_[:], wg_d[e, :, fb * 128:(fb + 1) * 128].rearrange("(k p) n -> p k n", p=128), q="pool")
            c.dma(wu_[:], wu_d[e, :, fb * 128:(fb + 1) * 128].rearrange("(k p) n -> p k n", p=128), q="pool")
            for ci, (lo, w) in enumerate(chunks):
                pa, pu = psa[ci % 2], psu[ci % 2]
                for k in range(KT):
                    c.mm(pa[:, 0:w], wg_[:, k, :], xT[:, k, lo:lo + w], start=(k == 0), stop=(k == KT - 1))
                for k in range(KT):
                    c.mm(pu[:, 0:w], wu_[:, k, :], xT[:, k, lo:lo + w], start=(k == 0), stop=(k == KT - 1))
                s_ = sg[ci % 2]
                c.act(s_[:, 0:w], pa[:, 0:w], AF.Sigmoid)
                c.tt(s_[:, 0:w], s_[:, 0:w], pa[:, 0:w], ALU.mult)
                c.tt(hT[:, fb, lo:lo + w], s_[:, 0:w], pu[:, 0:w], ALU.mult)
        for nb in range(D // 512):
            wd_ = wdb[nb % 2]
            c.dma(wd_[:], wd_d[e, :, nb * 512:(nb + 1) * 512].rearrange("(k p) n -> p k n", p=128), q="pool")
            for t in range(NT):
                p = pso[t % 2]
                for k in range(8):
                    c.mm(p[:, 0:512], hT[:, k, t * 128:(t + 1) * 128], wd_[:, k, :], start=(k == 0), stop=(k == 7))
                o = ob[k0 % 3]
                k0 += 1
                c.ts(o[:], p[:, 0:512], gt[:, e, t:t + 1], None, ALU.mult)
                c.dma(y[e, t * 128:(t + 1) * 128, nb * 512:(nb + 1) * 512], o[:], q="sp")
    c.finish("sp")
    return c


def host_expert(h2l, h2c, gate, idx, gatec, idxc, P, l):
    ims = []
    for j in range(NCORES):
        xs = np.zeros((2, ER, D), np.float32)
        gt = np.zeros((2, ER), np.float32)
        for ei in range(2):
            e = 2 * j + ei
            for b in range(2):
                xs[ei, b * 512:(b + 1) * 512] = h2l[b][idx[b, e]]
                gt[ei, b * 512:(b + 1) * 512] = gate[b, e]
                xs[ei, 1024 + b * 32:1024 + (b + 1) * 32] = h2c[b][idxc[b, e]]
                gt[ei, 1024 + b * 32:1024 + (b + 1) * 32] = gatec[b, e]
        gtp = np.ascontiguousarray(gt.reshape(2, NT, 128).transpose(2, 0, 1))
        ims.append({"ident": IDENT, "xs": xs, "gt": gtp,
                    "wg": P["moe_w_gate"][l][2 * j:2 * j + 2], "wu": P["moe_w_up"][l][2 * j:2 * j + 2],
                    "wd": P["moe_w_down"][l][2 * j:2 * j + 2]})
    res = run(prog("expert", build_expert), ims)
    Y = np.stack([res[j]["y"] for j in range(NCORES)], 0).reshape(16, ER, D)
    return Y


NSL = 16 * CAP_L
NSC = 16 * CAP_C


def build_combine():
    c = Ctx()
    yl = c.dram_in("yl", [NSL, D])
    yc = c.dram_in("yc", [NSC, D])
    il_d = c.dram_in("il", [128, NSL // 128], I32)
    ic_d = c.dram_in("ic", [128, NSC // 128], I32)
    tok_d = c.dram_in("tok", [128, NTOK])
    x1 = c.dram_in("x1", [NTOK, D])
    reps_d = c.dram_in("reps", [128, 4, D])
    out = c.dram_out("x2", [NTOK, D])
    reps = c.sb([128, 4, D], F32, "reps_sb")
    for i in range(4):
        c.dma(reps[:, i, :], reps_d[:, i, :])
    tok = c.sb([128, NTOK], F32, "tok_sb")
    c.dma(tok[:], tok_d[:, :])
    ili = c.sb([128, NSL // 128], I32, "ili")
    ici = c.sb([128, NSC // 128], I32, "ici")
    c.dma(ili[:], il_d[:, :])
    c.dma(ici[:], ic_d[:, :])
    il = c.sb([128, NSL // 128], F32, "il_f")
    ic = c.sb([128, NSC // 128], F32, "ic_f")
    c.copy(il[:], ili[:])
    c.copy(ic[:], ici[:])
    X = c.sb([128, NT, D], F32, "X")
    for t in range(NT):
        c.dma(X[:, t, :], x1[t * 128:(t + 1) * 128, :])
    yb = [c.sb([128, 8, 512], BF16, f"yb{i}") for i in range(3)]
    sb_ = [c.sb([128, 1024], BF16, f"sel{i}") for i in range(3)]
    tb = [c.sb([128, 512], F32, f"tb{i}") for i in range(2)]
    acc = [c.ps([128, 512]) for _ in range(8)]
    xh = c.sb([128, D], F32, "xh")
    tmp = ln_tmp(c)
    si = 0
    for nb in range(4):
        cols = slice(nb * 512, (nb + 1) * 512)
        nkt = NSL // 128
        for kt in range(nkt):
            yg = yb[(kt // 8) % 3]
            if kt % 8 == 0:
                c.dma(yg[:], yl[kt * 128:(kt + 8) * 128, cols].rearrange("(k p) n -> p k n", p=128), q="pool")
            y_ = yg[:, kt % 8, :]
            s_ = sb_[si % 3]
            si += 1
            c.ts(s_[:], tok[:, 0:1024], il[:, kt:kt + 1], None, ALU.is_equal)
            for t in range(8):
                c.mm(acc[t][:, 0:512], s_[:, t * 128:(t + 1) * 128], y_, start=(kt == 0), stop=(kt == nkt - 1))
        for t in range(8):
            b_ = tb[t % 2]
            c.tt(b_[:], acc[t][:, 0:512], reps[:, 0, cols], ALU.mult)
            c.stt(X[:, t, cols], X[:, t, cols], ALPHA, b_[:], ALU.mult, ALU.add)
        nkc = NSC // 128
        ygc = yb[nb % 3]
        c.dma(ygc[:, 0:nkc, :], yc[:, cols].rearrange("(k p) n -> p k n", p=128), q="pool")
        for kt in range(nkc):
            y_ = ygc[:, kt, :]
            s_ = sb_[si % 3]
            si += 1
            c.ts(s_[:, 0:128], tok[:, 1024:1152], ic[:, kt:kt + 1], None, ALU.is_equal)
            c.mm(acc[0][:, 0:512], s_[:, 0:128], y_, start=(kt == 0), stop=(kt == nkc - 1))
        b_ = tb[0]
        c.tt(b_[:], acc[0][:, 0:512], reps[:, 1, cols], ALU.mult)
        c.stt(X[:, 8, cols], X[:, 8, cols], ALPHA, b_[:], ALU.mult, ALU.add)
    for t in range(NT):
        ln_hat(c, cm_none, X[:, t, :], xh[:], tmp=tmp)
        c.tt(xh[:], xh[:], reps[:, 2, :], ALU.mult)
        c.tt(X[:, t, :], xh[:], reps[:, 3, :], ALU.add)
        c.dma(out[t * 128:(t + 1) * 128, :], X[:, t, :], q="sp")
    c.finish("sp")
    return c


cm_none = None


def host_combine(Y, idx, idxc, x1l, x1c, mod, P, l):
    x1s = tok_shard(x1l, x1c)
    ims = []
    for j in range(NCORES):
        b, q = j // 4, j % 4
        yl = np.ascontiguousarray(Y[:, b * 512:(b + 1) * 512, :].reshape(NSL, D))
        il = idx[b].reshape(NSL).astype(np.int32)
        tok = np.full((NTOK,), -5.0, np.float32)
        tok[:1024] = q * 1024 + np.arange(1024)
        if j < 4:
            cb = j // 2
            yc = np.ascontiguousarray(Y[:, 1024 + cb * 32:1024 + (cb + 1) * 32, :].reshape(NSC, D))
            ic = idxc[cb].reshape(NSC).astype(np.int32)
            tok[1024:] = (j % 2) * 128 + np.arange(128)
        else:
            yc = np.zeros((NSC, D), np.float32)
            ic = np.full((NSC,), -7, np.int32)
        seg = lambda r_, i: mod[r_, i * D:(i + 1) * D]
        reps = np.stack([rep(seg(b, 5)), rep(seg(2, 5)), rep(P["ln2_g"][l]), rep(P["ln2_b"][l])], 1)
        ims.append({"yl": yl, "yc": yc, "il": np.ascontiguousarray(il.reshape(-1, 128).T),
                    "ic": np.ascontiguousarray(ic.reshape(-1, 128).T), "tok": rep(tok),
                    "x1": x1s[j], "reps": np.ascontiguousarray(reps)})
    res = run(prog("combine", build_combine), ims)
    return tok_unshard([r["x2"] for r in res], D)


def kernel(**inp):
    P = {k: np.asarray(v) for k, v in inp.items()}
    x_lat = np.asarray(P["x"], np.float32)
    x_ctx = np.asarray(P["ctx"], np.float32)
    mods = host_mod(P["c"], P["c_ctx"], P["ada_w"], P["ada_b"])
    for l in range(2):
        mod = mods[l]
        pl, pc = host_proj(x_lat, x_ctx, mod, P["w_in"][l])
        s5o = host_s5(pl, pc, P, l)
        atto = host_att(pl, pc, P, l)
        ml, mc = host_merge_a(x_lat, x_ctx, mod, pl, pc, s5o, atto, P, l)
        (x1l, x1c), (h2l, h2c), (al, ac) = host_merge_b(ml, mc, x_lat, x_ctx, mod, P, l)
        gate, idx, gatec, idxc = host_topk(al, ac)
        Y = host_expert(h2l, h2c, gate, idx, gatec, idxc, P, l)
        x_lat, x_ctx = host_combine(Y, idx, idxc, x1l, x1c, mod, P, l)
    return np.ascontiguousarray(x_lat, dtype=np.float32)
```

```python
import numpy as np
import concourse.bass as bass
import concourse.mybir as mybir
from concourse.bass_utils import run_bass_kernel_spmd

F32 = mybir.dt.float32
BF16 = mybir.dt.bfloat16
I32 = mybir.dt.int32
U32 = mybir.dt.uint32
AF = mybir.ActivationFunctionType
ALU = mybir.AluOpType
AX = mybir.AxisListType

NCORES = 8


class V:
    __slots__ = ("t", "ap")

    def __init__(self, t, ap):
        self.t = t
        self.ap = ap

    def __getitem__(self, idx):
        return V(self.t, self.ap[idx])


class T:
    def __init__(self, ctx, name, shape, dtype, psum=False):
        nc = ctx.nc
        if psum:
            self.h = nc.alloc_psum_tensor(name, shape, dtype)
        else:
            self.h = nc.alloc_sbuf_tensor(name, shape, dtype)
        self.lw = None
        self.rd = {}
        self.name = name
        self.psum = psum

    def __getitem__(self, idx):
        return V(self, self.h[idx])

    def v(self, ap):
        return V(self, ap)


def _ap(x):
    if isinstance(x, V):
        return x.ap
    if isinstance(x, T):
        return x.h[:]
    return x


def _tl(x):
    if isinstance(x, V):
        return x.t
    if isinstance(x, T):
        return x
    return None


import os as _os


class Ctx:
    SAME_ENGINE_SYNC = _os.environ.get("MK_SAME_ENGINE_SYNC", "1") == "1"

    def __init__(self):
        self.nc = bass.Bass("TRN2", target_bir_lowering=False)
        nc = self.nc
        self.eng = {"pe": nc.tensor, "act": nc.scalar, "dve": nc.vector,
                    "pool": nc.gpsimd, "sp": nc.sync}
        self.sems = {}
        self.cnt = {}
        for e in ("pe", "act", "dve", "pool"):
            self.sems[e] = nc.alloc_semaphore("sem_" + e)
            self.cnt[e] = 0
        self.known = {e: {} for e in self.eng}
        self.dma_pool = {}
        self.dma_k = {}
        self.ntile = 0
        self.out_deps = []
        self.stream = {e: [] for e in self.eng}
        self.finalized = False

    def sb(self, shape, dtype, name=None):
        self.ntile += 1
        return T(self, name or f"t{self.ntile}", shape, dtype)

    def ps(self, shape, dtype=F32, name=None):
        self.ntile += 1
        return T(self, name or f"p{self.ntile}", shape, dtype, psum=True)

    def dram_in(self, name, shape, dtype=F32):
        return self.nc.dram_tensor(name, list(shape), dtype, kind="ExternalInput").ap()

    def dram_out(self, name, shape, dtype=F32):
        return self.nc.dram_tensor(name, list(shape), dtype, kind="ExternalOutput").ap()

    def _wait(self, e, deps):
        eng = self.eng[e]
        kn = self.known[e]
        best = {}
        for d in deps:
            if d is None:
                continue
            k, val = d
            if best.get(k, 0) < val:
                best[k] = val
        for k, val in best.items():
            if isinstance(k, str):
                if k == e and (e == "pe" or not self.SAME_ENGINE_SYNC):
                    continue
                sem = self.sems[k]
            else:
                sem = k
            if kn.get(k, 0) >= val:
                continue
            self.stream[e].append(("w", sem, val))
            kn[k] = val

    def _deps(self, outs, ins):
        deps = []
        for v in ins:
            t = _tl(v)
            if t is not None:
                deps.append(t.lw)
                if t.psum:
                    deps.extend(t.rd.items())
        for v in outs:
            t = _tl(v)
            if t is not None:
                deps.append(t.lw)
                deps.extend(t.rd.items())
        return deps

    def emit(self, e, outs, ins, f):
        self._wait(e, self._deps(outs, ins))
        self.cnt[e] += 1
        n = self.cnt[e]
        self.stream[e].append(("i", f, self.sems[e], 1))
        for v in ins:
            t = _tl(v)
            if t is not None:
                t.rd[e] = n
        for v in outs:
            t = _tl(v)
            if t is not None:
                t.lw = (e, n)
                t.rd = {}
        return None

    def dma(self, out, in_, q="sp", npool=24, **kw):
        self._wait(q, self._deps([out], [in_]))
        if q not in self.dma_pool:
            self.dma_pool[q] = [[self.nc.alloc_semaphore(f"dq_{q}_{i}"), 0] for i in range(npool)]
            self.dma_k[q] = 0
        pool = self.dma_pool[q]
        slot = pool[self.dma_k[q] % len(pool)]
        self.dma_k[q] += 1
        sem, val = slot
        if val > 0 and self.known[q].get(sem, 0) < val:
            self.stream[q].append(("w", sem, val))
            self.known[q][sem] = val
        slot[1] = val + 16
        o_ap, i_ap = _ap(out), _ap(in_)
        self.stream[q].append(("i", lambda g: g.dma_start(out=o_ap, in_=i_ap, **kw), sem, 16))
        if _tl(in_) is not None:
            _tl(in_).rd[sem] = val + 16
        if _tl(out) is not None:
            _tl(out).lw = (sem, val + 16)
            _tl(out).rd = {}
        else:
            self.out_deps.append((sem, val + 16))
        return None

    def finish(self, e="sp"):
        self._wait(e, self.out_deps)
        assert not self.finalized
        self.finalized = True
        streams = self.stream

        def replay(g, items):
            for it in items:
                if it[0] == "w":
                    g.wait_ge(it[1], it[2])
                else:
                    it[1](g).then_inc(it[2], it[3])

        with self.nc.Block() as block:
            @block.sync
            def _(g):
                replay(g, streams["sp"])

            @block.tensor
            def _(g):
                replay(g, streams["pe"])

            @block.vector
            def _(g):
                replay(g, streams["dve"])

            @block.scalar
            def _(g):
                replay(g, streams["act"])

            @block.gpsimd
            def _(g):
                replay(g, streams["pool"])

    def mm(self, out, lhsT, rhs, start=True, stop=True):
        return self.emit("pe", [out], [lhsT, rhs] + ([] if start else [out]),
                         lambda g: g.matmul(_ap(out), _ap(lhsT), _ap(rhs), start=start, stop=stop))

    def transpose(self, out, in_, ident):
        return self.emit("pe", [out], [in_, ident],
                         lambda g: g.transpose(_ap(out), _ap(in_), _ap(ident)))

    def act(self, out, in_, func, bias=None, scale=None, accum_out=None, e="act"):
        ins = [in_]
        kw = {}
        if bias is not None:
            kw["bias"] = _ap(bias)
            ins.append(bias)
        if scale is not None:
            kw["scale"] = _ap(scale)
            ins.append(scale)
        outs = [out]
        if accum_out is not None:
            kw["accum_out"] = _ap(accum_out)
            outs.append(accum_out)
        return self.emit(e, outs, ins, lambda g: g.activation(_ap(out), _ap(in_), func, **kw))

    def copy(self, out, in_, e="dve"):
        if e == "act":
            return self.emit(e, [out], [in_], lambda g: g.copy(_ap(out), _ap(in_)))
        return self.emit(e, [out], [in_], lambda g: g.tensor_copy(_ap(out), _ap(in_)))

    def tt(self, out, a, b, op, e="dve"):
        return self.emit(e, [out], [a, b], lambda g: g.tensor_tensor(_ap(out), _ap(a), _ap(b), op))

    def ts(self, out, a, s1, s2, op0, op1=None, e="dve", accum_out=None):
        ins = [a] + [s for s in (s1, s2) if isinstance(s, V)]
        outs = [out] + ([accum_out] if accum_out is not None else [])
        kw = {}
        if accum_out is not None:
            kw["accum_out"] = _ap(accum_out)
        if op1 is None:
            return self.emit(e, outs, ins, lambda g: g.tensor_scalar(_ap(out), _ap(a), _ap(s1), None, op0, **kw))
        return self.emit(e, outs, ins, lambda g: g.tensor_scalar(_ap(out), _ap(a), _ap(s1), _ap(s2), op0, op1, **kw))

    def tss(self, out, a, s, op, e="pool"):
        return self.emit(e, [out], [a], lambda g: g.tensor_single_scalar(_ap(out), _ap(a), s, op))

    def stt(self, out, a, s, b, op0, op1, e="dve"):
        ins = [a, b] + ([s] if isinstance(s, V) else [])
        return self.emit(e, [out], ins,
                         lambda g: g.scalar_tensor_tensor(_ap(out), _ap(a), _ap(s), _ap(b), op0, op1))

    def memset(self, out, val, e="dve"):
        return self.emit(e, [out], [], lambda g: g.memset(_ap(out), val))

    def reduce(self, out, in_, op, axis=None, e="dve"):
        axis = axis or AX.X
        return self.emit(e, [out], [in_], lambda g: g.tensor_reduce(_ap(out), _ap(in_), axis, op))


def run(ctx, in_maps):
    if _os.environ.get("MK_TRACE", "0") == "1":
        res = run_bass_kernel_spmd(ctx.nc, in_maps, core_ids=list(range(NCORES)), trace=True)
        print("MK_TRACE exec_time_ns", res.exec_time_ns, flush=True)
        return res.results
    res = run_bass_kernel_spmd(ctx.nc, in_maps, core_ids=list(range(NCORES)))
    return res.results


D = 2048
KT = 16
NT = 9
NTOK = NT * 128
EPS = 1e-6


class Common:
    def __init__(self, c):
        self.c = c
        self.ident_d = c.dram_in("ident", [128, 128])
        self.ident = c.sb([128, 128], F32, "ident_sb")
        c.dma(self.ident[:], self.ident_d[:, :])
        self.eps = c.sb([128, 1], F32, "eps_sb")
        c.memset(self.eps[:], EPS)


def ln_hat(c, cm, xt, xh, width=D, tmp=None):
    nch = (width + 511) // 512
    st = tmp["st"]
    mv = tmp["mv"]
    rs = tmp["rs"]
    sq = tmp["sq"]
    for i in range(nch):
        lo, hi = i * 512, min(width, (i + 1) * 512)
        c.emit("dve", [st], [xt], lambda g, i=i, lo=lo, hi=hi: g.bn_stats(st.h[:, i * 6:(i + 1) * 6], xt.ap[:, lo:hi]))
    c.emit("dve", [mv], [st], lambda g: g.bn_aggr(mv.h[:], st.h[:, 0:nch * 6]))
    c.ts(sq[:], mv[:, 1:2], EPS, None, ALU.add)
    c.act(sq[:], sq[:], AF.Sqrt)
    c.emit("dve", [rs], [sq], lambda g: g.reciprocal(rs.h[:], sq.h[:]))
    c.ts(xh, xt, mv[:, 0:1], rs[:], ALU.subtract, ALU.mult)


def ln_tmp(c, tag=""):
    return {"st": c.sb([128, 24], F32, "ln_st" + tag), "mv": c.sb([128, 2], F32, "ln_mv" + tag),
            "rs": c.sb([128, 1], F32, "ln_rs" + tag), "sq": c.sb([128, 1], F32, "ln_sq" + tag)}


def to_fm(c, cm, xh, hT, tile, pst, scale_cols=None, bias_cols=None, nk=KT):
    for k in range(nk):
        p = pst[k % len(pst)]
        c.transpose(p[:, 0:128], xh[:, k * 128:(k + 1) * 128], cm.ident[:])
        dst = hT[:, k, tile * 128:(tile + 1) * 128]
        if scale_cols is not None:
            c.act(dst, p[:, 0:128], AF.Identity, bias=bias_cols[:, k:k + 1], scale=scale_cols[:, k:k + 1])
        else:
            c.copy(dst, p[:, 0:128], e="act")


def stream_linear(c, hT, w_dram, n_total, ntiles, consume, wbufs, pso, kt=KT, nbw=512, tile_list=None):
    wv = w_dram.rearrange("(k p) n -> p k n", p=128)
    nblk = (n_total + nbw - 1) // nbw
    tiles = tile_list if tile_list is not None else list(range(ntiles))
    i = 0
    for nb in range(nblk):
        lo = nb * nbw
        bw = min(nbw, n_total - lo)
        wb = wbufs[nb % len(wbufs)]
        c.dma(wb[:, 0:kt, 0:bw], wv[:, :, lo:lo + bw], q="pool")
        for t in tiles:
            p = pso[i % len(pso)]
            i += 1
            for k in range(kt):
                c.mm(p[:, 0:bw], hT[:, k, t * 128:(t + 1) * 128], wb[:, k, 0:bw],
                     start=(k == 0), stop=(k == kt - 1))
            consume(t, nb, lo, bw, p)


MODC = 2 * 12288 // NCORES


def build_mod():
    c = Ctx()
    cT = c.dram_in("cT", [128, KT, 3])
    w = c.dram_in("w", [D, MODC])
    b = c.dram_in("b", [3, MODC])
    out = c.dram_out("mod", [3, MODC])
    ct = c.sb([128, KT, 3], F32)
    sg = c.sb([128, KT, 3], F32)
    bt = c.sb([3, MODC], F32)
    ot = c.sb([3, MODC], F32)
    c.dma(ct[:], cT[:, :, :])
    c.dma(bt[:], b[:, :])
    c.act(sg[:], ct[:], AF.Sigmoid)
    c.tt(ct[:], ct[:], sg[:], ALU.mult)
    wb = [c.sb([128, KT, 512], F32, f"modw{i}") for i in range(2)]
    ps = [c.ps([128, 512]) for _ in range(2)]
    wv = w.rearrange("(k p) n -> p k n", p=128)
    for nb in range(MODC // 512):
        wt = wb[nb % 2]
        c.dma(wt[:], wv[:, :, nb * 512:(nb + 1) * 512])
        p = ps[nb % 2]
        for k in range(KT):
            c.mm(p[0:3, :], ct[:, k, :], wt[:, k, :], start=(k == 0), stop=(k == KT - 1))
        c.tt(ot[:, nb * 512:(nb + 1) * 512], p[0:3, :], bt[:, nb * 512:(nb + 1) * 512], ALU.add)
    c.dma(out[:, :], ot[:], q="pool")
    c.finish("pool")
    return c


IN_TOTAL = 3904


def load_mod_cols(c, mv_d, n):
    t = c.sb([128, n, KT], F32, "modcols")
    c.dma(t[:], mv_d[:, :, :])
    return t


def build_proj():
    c = Ctx()
    cm = Common(c)
    x = c.dram_in("x", [NTOK, D])
    mv_d = c.dram_in("mv", [128, 4, KT])
    w = c.dram_in("w_in", [D, IN_TOTAL])
    out = c.dram_out("proj", [NTOK, IN_TOTAL])
    mv = load_mod_cols(c, mv_d, 4)
    c.ts(mv[:, 1, :], mv[:, 1, :], 1.0, None, ALU.add)
    c.ts(mv[:, 3, :], mv[:, 3, :], 1.0, None, ALU.add)
    hT = c.sb([128, KT, NTOK], BF16, "hT")
    xts = [c.sb([128, D], F32, f"xt{i}") for i in range(2)]
    xh = c.sb([128, D], F32, "xh")
    tmp = ln_tmp(c)
    pst = [c.ps([128, 512]) for _ in range(2)]
    for t in range(NT):
        xt = xts[t % 2]
        c.dma(xt[:], x[t * 128:(t + 1) * 128, :])
        ln_hat(c, cm, xt[:], xh[:], tmp=tmp)
        j = 0 if t < 8 else 2
        to_fm(c, cm, xh, hT, t, pst, scale_cols=mv[:, j + 1, :], bias_cols=mv[:, j, :])
    wbufs = [c.sb([128, KT, 512], BF16, f"wb{i}") for i in range(2)]
    pso = [c.ps([128, 512]) for _ in range(3)]
    obufs = [c.sb([128, 512], F32, f"ob{i}") for i in range(3)]
    cnt = [0]

    def consume(t, nb, lo, bw, p):
        ob = obufs[cnt[0] % 3]
        cnt[0] += 1
        c.copy(ob[:, 0:bw], p[:, 0:bw], e=("dve" if cnt[0] % 2 else "act"))
        c.dma(out[t * 128:(t + 1) * 128, lo:lo + bw], ob[:, 0:bw], q="sp")

    stream_linear(c, hT, w, IN_TOTAL, NT, consume, wbufs, pso)
    c.finish("sp")
    return c


_PROGS = {}
IDENT = np.eye(128, dtype=np.float32)


def prog(name, builder):
    if name not in _PROGS:
        _PROGS[name] = builder()
    return _PROGS[name]


def fm_cols(vec):
    return np.ascontiguousarray(vec.reshape(-1, 128).T)


def host_mod(cv, c_ctx, ada_w, ada_b):
    cvec = np.concatenate([cv, c_ctx[None, :]], 0)
    cT = np.ascontiguousarray(cvec.reshape(3, KT, 128).transpose(2, 1, 0))
    ims = []
    per = 12288 // NCORES
    for j in range(NCORES):
        w = np.concatenate([ada_w[l][:, j * per:(j + 1) * per] for l in range(2)], 1)
        b = np.concatenate([np.broadcast_to(ada_b[l][None, j * per:(j + 1) * per], (3, per)) for l in range(2)], 1)
        ims.append({"cT": cT, "w": np.ascontiguousarray(w), "b": np.ascontiguousarray(b)})
    res = run(prog("mod", build_mod), ims)
    mods = []
    for l in range(2):
        mods.append(np.concatenate([res[j]["mod"][:, l * per:(l + 1) * per] for j in range(NCORES)], 1))
    return mods


def tok_shard(x_lat, x_ctx):
    outs = []
    for j in range(NCORES):
        b, q = j // 4, j % 4
        a = np.zeros((NTOK, x_lat.shape[-1]), np.float32)
        a[:1024] = x_lat[b, q * 1024:(q + 1) * 1024]
        if j < 4:
            a[1024:] = x_ctx[j // 2, (j % 2) * 128:(j % 2 + 1) * 128]
        outs.append(a)
    return outs


def tok_unshard(parts, width):
    lat = np.zeros((2, 4096, width), np.float32)
    ctx = np.zeros((2, 256, width), np.float32)
    for j in range(NCORES):
        b, q = j // 4, j % 4
        lat[b, q * 1024:(q + 1) * 1024] = parts[j][:1024]
        if j < 4:
            ctx[j // 2, (j % 2) * 128:(j % 2 + 1) * 128] = parts[j][1024:]
    return lat, ctx


def mod_cols(mod, j, idxs):
    b = j // 4
    cols = []
    for i in idxs:
        cols.append(fm_cols(mod[b, i * D:(i + 1) * D]))
    for i in idxs:
        cols.append(fm_cols(mod[2, i * D:(i + 1) * D]))
    return np.ascontiguousarray(np.stack(cols, 1))


def host_proj(x_lat, x_ctx, mod, w_in):
    xs = tok_shard(x_lat, x_ctx)
    ims = []
    for j in range(NCORES):
        ims.append({"ident": IDENT, "x": xs[j], "mv": mod_cols(mod, j, [0, 1]), "w_in": w_in})
    res = run(prog("proj", build_proj), ims)
    return tok_unshard([r["proj"] for r in res], IN_TOTAL)


NLAT = 4096
NALL = 4352
NTA = 34
NTL = 32


def ms_rstd(c, x, w, tmp, eps=EPS):
    st, mv, rs, sq = tmp["st"], tmp["mv"], tmp["rs"], tmp["sq"]
    c.emit("dve", [st], [x], lambda g: g.bn_stats(st.h[:, 0:6], _ap(x)))
    c.emit("dve", [mv], [st], lambda g: g.bn_aggr(mv.h[:], st.h[:, 0:6]))
    c.stt(sq[:], mv[:, 0:1], mv[:, 0:1], mv[:, 1:2], ALU.mult, ALU.add)
    c.ts(sq[:], sq[:], eps, None, ALU.add)
    c.act(sq[:], sq[:], AF.Sqrt)
    c.emit("dve", [rs], [sq], lambda g: g.reciprocal(rs.h[:], sq.h[:]))


def rope_tm(c, x, out, cos, sin, n, t1, t2, e="pool"):
    xv = V(_tl(x), _ap(x).rearrange("p (h t n) -> p h t n", h=2, t=2))
    ov = V(_tl(out), _ap(out).rearrange("p (h t n) -> p h t n", h=2, t=2))
    cv = V(_tl(cos), _ap(cos).rearrange("p (h n) -> p h n", h=2))
    sv = V(_tl(sin), _ap(sin).rearrange("p (h n) -> p h n", h=2))
    a = V(t1, t1.h[:, 0:2 * n].rearrange("p (h n) -> p h n", h=2))
    b = V(t2, t2.h[:, 0:2 * n].rearrange("p (h n) -> p h n", h=2))
    x1, x2 = xv[:, :, 0, :], xv[:, :, 1, :]
    c.tt(a, x1, cv, ALU.mult, e=e)
    c.tt(b, x2, sv, ALU.mult, e=e)
    c.tt(ov[:, :, 0, :], a, b, ALU.subtract, e=e)
    c.tt(a, x1, sv, ALU.mult, e=e)
    c.tt(b, x2, cv, ALU.mult, e=e)
    c.tt(ov[:, :, 1, :], a, b, ALU.add, e=e)


def attention(c, cm, qTs, kTs, vt, q_tiles, k_lo, k_hi, scale, bufs, out_cb):
    PT, pss, pst, pso = (bufs[k] for k in ("PT", "pss", "pst", "pso"))
    identb = bufs["identb"]
    nk = k_hi - k_lo
    nch = (nk + 511) // 512
    nkt = nk // 128
    dv1 = vt.h.shape[-1]
    dv = dv1 - 1
    ng = (nkt + 3) // 4

    def qk_phase(qt):
        q0 = qt * 128
        bi = bufs["ctr"][0] % 2
        bufs["ctr"][0] += 1
        S, Pb, cmax, mx, rsum = (bufs[k][bi] for k in ("S", "Pb", "cmax", "mx", "rsum"))
        for ch in range(nch):
            lo = k_lo + ch * 512
            w = min(512, k_hi - lo)
            p = pss[ch % len(pss)]
            for i, ((qT, r), (kT, _)) in enumerate(zip(qTs, kTs)):
                c.mm(p[:, 0:w], qT[0:r, q0:q0 + 128], kT[0:r, lo:lo + w], start=(i == 0), stop=(i == len(qTs) - 1))
            c.reduce(cmax[:, ch:ch + 1], p[:, 0:w], ALU.max)
            c.copy(S[:, ch * 512:ch * 512 + w], p[:, 0:w], e="act")
        c.reduce(mx[:], cmax[:, 0:nch], ALU.max)
        c.ts(mx[:], mx[:], -scale, None, ALU.mult)
        c.act(Pb[:, 0:nk], S[:, 0:nk], AF.Exp, bias=mx[:], scale=scale)
        return (qt, bi)

    def pv_phase(st):
        qt, bi = st
        Pb, rsum = bufs["Pb"][bi], bufs["rsum"][bi]
        po = pso[bi]

        def tr(g4):
            pt = pst[g4 % len(pst)]
            n4 = min(4, nkt - g4 * 4)
            for j in range(n4):
                kt = g4 * 4 + j
                c.transpose(pt[:, j * 128:(j + 1) * 128], Pb[:, kt * 128:(kt + 1) * 128], identb[:])

        tr(0)
        for g4 in range(ng):
            if g4 + 1 < ng:
                tr(g4 + 1)
            pt = pst[g4 % len(pst)]
            n4 = min(4, nkt - g4 * 4)
            ptb = PT[g4 % len(PT)]
            c.copy(ptb[:, 0:n4 * 128], pt[:, 0:n4 * 128], e="dve")
            for j in range(n4):
                kt = g4 * 4 + j
                c.mm(po[:, 0:dv1], ptb[:, j * 128:(j + 1) * 128], vt[:, k_lo // 128 + kt, :],
                     start=(kt == 0), stop=(kt == nkt - 1))
        c.emit("dve", [rsum], [po], lambda g, rsum=rsum, po=po, dv=dv: g.reciprocal(rsum.h[:], po.h[:, dv:dv + 1]))
        out_cb(qt, po, rsum)

    prev = None
    for qt in q_tiles:
        st = qk_phase(qt)
        if prev is not None:
            pv_phase(prev)
        prev = st
    if prev is not None:
        pv_phase(prev)


def build_att():
    c = Ctx()
    cm = Common(c)
    din = c.dram_in
    gq, gk, gv = din("gq", [NALL, 128]), din("gk", [NALL, 128]), din("gv", [NALL, 128])
    gqn, gkn = din("gqn", [128, 128]), din("gkn", [128, 128])
    mcq, mckv, mkr = din("mcq", [NALL, 512]), din("mckv", [NALL, 256]), din("mkr", [NALL, 64])
    mqn, mkvn = din("mqn", [128, 512]), din("mkvn", [128, 256])
    wuq, wukv = din("wuq", [512, 192]), din("wukv", [256, 256])
    rq, rk, rv, rg = din("rq", [NALL, 64]), din("rk", [NALL, 64]), din("rv", [NALL, 128]), din("rg", [NALL, 128])
    rpar, rgain = din("rpar", [128, 2]), din("rgain", [128, 128])
    cos128, sin128 = din("cos128", [NLAT, 64]), din("sin128", [NLAT, 64])
    cos64, sin64 = din("cos64", [NLAT, 32]), din("sin64", [NLAT, 32])
    rconst = din("rconst", [128, 6, 128])
    rcol = din("rcol", [128, 2])
    o_gqa, o_mla, o_ret = c.dram_out("o_gqa", [NALL, 128]), c.dram_out("o_mla", [NALL, 128]), c.dram_out("o_ret", [NALL, 128])

    QT = c.sb([128, NALL], BF16, "QT")
    KTb = c.sb([128, NALL], BF16, "KT")
    VT = c.sb([128, NTA, 129], BF16, "VT")
    c.memset(VT[:, :, 128:129], 1.0)
    QR = c.sb([64, NALL], BF16, "QR")
    KR = c.sb([64, NALL], BF16, "KR")
    identb = c.sb([128, 128], BF16, "identb")
    c.copy(identb[:], cm.ident[:])
    bufs = {"S": [c.sb([128, NALL], F32, f"S{i}") for i in range(2)], "PT": [c.sb([128, 512], BF16, f"PT{i}") for i in range(2)],
            "Pb": [c.sb([128, NALL], BF16, f"Pb{i}") for i in range(2)], "identb": identb, "ctr": [0],
            "pss": [c.ps([128, 512]) for _ in range(2)], "pst": [c.ps([128, 512], BF16) for _ in range(2)],
            "pso": [c.ps([128, 512]) for _ in range(2)],
            "cmax": [c.sb([128, 16], F32, f"cmax{i}") for i in range(2)], "mx": [c.sb([128, 1], F32, f"mx{i}") for i in range(2)],
            "rsum": [c.sb([128, 1], F32, f"rsum{i}") for i in range(2)]}
    psx = [c.ps([128, 512]) for _ in range(2)]
    tmp = ln_tmp(c)
    xin = [c.sb([128, 512], F32, f"xin{i}") for i in range(3)]
    xa = c.sb([128, 512], F32, "xa")
    xb = c.sb([128, 512], F32, "xb")
    t1 = c.sb([128, 128], F32, "ropet1")
    t2 = c.sb([128, 128], F32, "ropet2")
    cs = [c.sb([128, 2, 64], F32, f"cs{i}") for i in range(2)]
    ob = [c.sb([128, 128], F32, f"ob{i}") for i in range(3)]
    gains = c.sb([128, 128 + 128 + 512 + 256 + 128], F32, "gains")
    G_Q, G_K, G_MQ, G_MKV, G_R = 0, 128, 256, 768, 1024
    c.dma(gains[:, G_Q:G_Q + 128], gqn[:, :])
    c.dma(gains[:, G_K:G_K + 128], gkn[:, :])
    c.dma(gains[:, G_MQ:G_MQ + 512], mqn[:, :])
    c.dma(gains[:, G_MKV:G_MKV + 256], mkvn[:, :])
    c.dma(gains[:, G_R:G_R + 128], rgain[:, :])
    cnt = [0]

    def outer(dst):
        def cb(qt, po, rsum):
            o = ob[cnt[0] % 3]
            cnt[0] += 1
            c.ts(o[:], po[:, 0:128], rsum[:], None, ALU.mult)
            c.dma(dst[qt * 128:(qt + 1) * 128, :], o[:], q="pool")
        return cb

    def load_cs(t, cos_d, sin_d, n2, k):
        t_ = cs[k % 2]
        c.dma(t_[:, 0, 0:n2], cos_d[t * 128:(t + 1) * 128, :])
        c.dma(t_[:, 1, 0:n2], sin_d[t * 128:(t + 1) * 128, :])
        return t_

    def tr_to(dst, src, rows, t):
        p = psx[t % 2]
        c.transpose(p[0:rows, 0:128], src, cm.ident[:])
        c.copy(dst[0:rows, t * 128:(t + 1) * 128], p[0:rows, 0:128], e="act")

    import os
    SECT = os.environ.get("ATT_SECT", "gmr")
    for t in range(NTA if "g" in SECT else 0):
        xq, xk = xin[0], xin[1]
        c.dma(xq[:, 0:128], gq[t * 128:(t + 1) * 128, :])
        c.dma(xk[:, 0:128], gk[t * 128:(t + 1) * 128, :])
        c.dma(VT[:, t, 0:128], gv[t * 128:(t + 1) * 128, :], q="pool")
        for (x, g0, dstT) in ((xq, G_Q, QT), (xk, G_K, KTb)):
            ms_rstd(c, x[:, 0:128], 128, tmp)
            c.stt(xa[:, 0:128], x[:, 0:128], tmp["rs"][:], gains[:, g0:g0 + 128], ALU.mult, ALU.mult)
            if t < NTL:
                cst = load_cs(t, cos128, sin128, 64, t)
                rope_tm(c, xa[:, 0:128], xb[:, 0:128], cst[:, 0, 0:64], cst[:, 1, 0:64], 32, t1, t2)
                tr_to(dstT, xb[:, 0:128], 128, t)
            else:
                tr_to(dstT, xa[:, 0:128], 128, t)
    if "g" in SECT:
        nq = int(os.environ.get("GQA_NQ", NTL))
        attention(c, cm, [(QT, 128)], [(KTb, 128)], VT, list(range(nq)), 0, NALL, 128 ** -0.5, bufs, outer(o_gqa))
        if nq == NTL:
            attention(c, cm, [(QT, 128)], [(KTb, 128)], VT, [32, 33], NLAT, NALL, 128 ** -0.5, bufs, outer(o_gqa))

    wq = c.sb([128, 4, 192], F32, "wq")
    wkv = c.sb([128, 2, 256], F32, "wkv")
    c.dma(wq[:], wuq.rearrange("(k p) n -> p k n", p=128))
    c.dma(wkv[:], wukv.rearrange("(k p) n -> p k n", p=128))
    cT = c.sb([128, 4, 128], F32, "cT")
    for t in range(NTA if "m" in SECT else 0):
        xq, xkv, xr = xin[0], xin[1], xin[2]
        c.dma(xq[:, 0:512], mcq[t * 128:(t + 1) * 128, :])
        c.dma(xkv[:, 0:256], mckv[t * 128:(t + 1) * 128, :])
        c.dma(xr[:, 0:64], mkr[t * 128:(t + 1) * 128, :])
        if t < NTL:
            cst = load_cs(t, cos64, sin64, 32, t)
        ms_rstd(c, xq[:, 0:512], 512, tmp)
        c.stt(xa[:, 0:512], xq[:, 0:512], tmp["rs"][:], gains[:, G_MQ:G_MQ + 512], ALU.mult, ALU.mult)
        for k in range(4):
            p = psx[k % 2]
            c.transpose(p[:, 0:128], xa[:, k * 128:(k + 1) * 128], cm.ident[:])
            c.copy(cT[:, k, :], p[:, 0:128], e="act")
        p = psx[0]
        for k in range(4):
            c.mm(p[:, 0:128], wq[:, k, 0:128], cT[:, k, :], start=(k == 0), stop=(k == 3))
        c.copy(QT[:, t * 128:(t + 1) * 128], p[:, 0:128], e="act")
        p = psx[1]
        for k in range(4):
            c.mm(p[:, 0:64], cT[:, k, :], wq[:, k, 128:192], start=(k == 0), stop=(k == 3))
        c.copy(xb[:, 0:64], p[:, 0:64])
        if t < NTL:
            rope_tm(c, xb[:, 0:64], xb[:, 64:128], cst[:, 0, 0:32], cst[:, 1, 0:32], 16, t1, t2)
            tr_to(QR, xb[:, 64:128], 64, t)
        else:
            tr_to(QR, xb[:, 0:64], 64, t)
        ms_rstd(c, xkv[:, 0:256], 256, tmp)
        c.stt(xa[:, 0:256], xkv[:, 0:256], tmp["rs"][:], gains[:, G_MKV:G_MKV + 256], ALU.mult, ALU.mult)
        for k in range(2):
            p = psx[k % 2]
            c.transpose(p[:, 0:128], xa[:, k * 128:(k + 1) * 128], cm.ident[:])
            c.copy(cT[:, k, :], p[:, 0:128], e="act")
        p = psx[0]
        for k in range(2):
            c.mm(p[:, 0:128], wkv[:, k, 0:128], cT[:, k, :], start=(k == 0), stop=(k == 1))
        c.copy(KTb[:, t * 128:(t + 1) * 128], p[:, 0:128], e="act")
        p = psx[1]
        for k in range(2):
            c.mm(p[:, 0:128], cT[:, k, :], wkv[:, k, 128:256], start=(k == 0), stop=(k == 1))
        c.copy(VT[:, t, 0:128], p[:, 0:128])
        if t < NTL:
            rope_tm(c, xr[:, 0:64], xb[:, 128:192], cst[:, 0, 0:32], cst[:, 1, 0:32], 16, t1, t2)
            tr_to(KR, xb[:, 128:192], 64, t)
        else:
            tr_to(KR, xr[:, 0:64], 64, t)
    sc = 192 ** -0.5
    if "m" in SECT:
        attention(c, cm, [(QT, 128), (QR, 64)], [(KTb, 128), (KR, 64)], VT, list(range(NTL)), 0, NALL, sc, bufs, outer(o_mla))
        attention(c, cm, [(QT, 128), (QR, 64)], [(KTb, 128), (KR, 64)], VT, [32, 33], NLAT, NALL, sc, bufs, outer(o_mla))
    if "r" not in SECT:
        c.finish("pool")
        return c

    RQ, RK = bufs["S"][0], bufs["S"][1]
    RV = c.sb([128, NTA, 128], F32, "RV")
    rc = c.sb([128, 6, 128], F32, "rc")
    rcl = c.sb([128, 2], F32, "rcl")
    rp = c.sb([128, 2], F32, "rp")
    c.dma(rc[:], rconst[:, :, :])
    c.dma(rcl[:], rcol[:, :])
    c.dma(rp[:], rpar[:, :])
    lg = c.sb([128, 2], F32, "lg")
    c.act(lg[:], rp[:], AF.Exp)
    c.ts(lg[:], lg[:], -1.0, None, ALU.mult)
    DT = c.sb([128, 128], F32, "DT")
    dtmp = c.sb([128, 128], F32, "dtmp")
    c.act(DT[:], rc[:, 0, :], AF.Exp, scale=lg[:, 0:1])
    c.tt(DT[:], DT[:], rc[:, 2, :], ALU.mult)
    c.act(dtmp[:], rc[:, 1, :], AF.Exp, scale=lg[:, 1:2])
    c.tt(dtmp[:], dtmp[:], rc[:, 3, :], ALU.mult)
    c.tt(DT[:], DT[:], dtmp[:], ALU.add)
    dq = c.sb([128, 2, 128], F32, "dq")
    c.act(dq[:, 0, :], rc[:, 4, :], AF.Exp, scale=lg[:, 0:1])
    c.act(dq[:, 1, :], rc[:, 5, :], AF.Exp, scale=lg[:, 1:2])
    dk = c.sb([128, 2], F32, "dk")
    c.act(dk[:, 0:1], rcl[:, 0:1], AF.Exp, scale=lg[:, 0:1])
    c.act(dk[:, 1:2], rcl[:, 1:2], AF.Exp, scale=lg[:, 1:2])
    gcd = c.sb([128, 2], F32, "gcd")
    c.ts(gcd[:], lg[:], 128.0, None, ALU.mult)
    c.act(gcd[:], gcd[:], AF.Exp)
    Ktm = c.sb([128, NTA, 64], F32, "Ktm")
    Sf = c.sb([64, NTA + 1, 128], F32, "Sf")
    Sb = c.sb([64, NTA + 1, 128], F32, "Sb")
    kd = c.sb([128, 2, 64], F32, "kd")
    fwd_order = [32, 33] + list(range(NTL))
    for t in range(NTA):
        xq, xk = xin[0], xin[1]
        c.dma(xq[:, 0:64], rq[t * 128:(t + 1) * 128, :])
        c.dma(xk[:, 0:64], rk[t * 128:(t + 1) * 128, :])
        c.dma(RV[:, t, :], rv[t * 128:(t + 1) * 128, :])
        c.ts(xk[:, 0:64], xk[:, 0:64], 0.125, None, ALU.mult)
        if t < NTL:
            cst = load_cs(t, cos64, sin64, 32, t)
            rope_tm(c, xq[:, 0:64], xb[:, 0:64], cst[:, 0, 0:32], cst[:, 1, 0:32], 16, t1, t2)
            rope_tm(c, xk[:, 0:64], Ktm[:, t, :], cst[:, 0, 0:32], cst[:, 1, 0:32], 16, t1, t2)
            tr_to(RQ, xb[:, 0:64], 64, t)
        else:
            c.copy(Ktm[:, t, :], xk[:, 0:64])
            tr_to(RQ, xq[:, 0:64], 64, t)
        tr_to(RK, Ktm[:, t, :], 64, t)
    bwd_order = [33, 32] + list(range(NTL - 1, -1, -1))
    for (S_, order, col) in ((Sf, fwd_order, 0), (Sb, bwd_order, 1)):
        c.memset(S_[:, 0, :], 0.0)
        for i, t in enumerate(order):
            c.ts(kd[:, col, :], Ktm[:, t, :], dk[:, col:col + 1], None, ALU.mult)
            p = psx[i % 2]
            c.mm(p[0:64, 0:128], kd[:, col, :], RV[:, t, :])
            c.stt(S_[:, i + 1, :], S_[:, i, :], gcd[0:64, col:col + 1], p[0:64, 0:128], ALU.mult, ALU.add)
    fpos = {t: i for i, t in enumerate(fwd_order)}
    bpos = {t: i for i, t in enumerate(bwd_order)}
    MT = [c.sb([128, 128], F32, f"MT{i}") for i in range(2)]
    qd = [c.sb([64, 2, 128], F32, f"qd{i}") for i in range(2)]
    gt = [c.sb([128, 128], F32, f"gt{i}") for i in range(2)]
    for t in range(NTA):
        sl = slice(t * 128, (t + 1) * 128)
        p = psx[0]
        c.mm(p[:, 0:128], RK[0:64, sl], RQ[0:64, sl])
        m = MT[t % 2]
        c.tt(m[:], p[:, 0:128], DT[:], ALU.mult)
        q_ = qd[t % 2]
        c.tt(q_[:, 0, :], RQ[0:64, sl], dq[0:64, 0, :], ALU.mult)
        c.tt(q_[:, 1, :], RQ[0:64, sl], dq[0:64, 1, :], ALU.mult)
        po = psx[1]
        c.mm(po[:, 0:128], m[:], RV[:, t, :], start=True, stop=False)
        c.mm(po[:, 0:128], q_[:, 0, :], Sf[:, fpos[t], :], start=False, stop=False)
        c.mm(po[:, 0:128], q_[:, 1, :], Sb[:, bpos[t], :], start=False, stop=True)
        g_ = gt[t % 2]
        c.dma(g_[:], rg[sl, :])
        o = ob[cnt[0] % 3]
        cnt[0] += 1
        c.copy(xa[:, 0:128], po[:, 0:128])
        ln_hat(c, cm, xa[:, 0:128], xb[:, 0:128], width=128, tmp=tmp)
        c.tt(xb[:, 0:128], xb[:, 0:128], gains[:, G_R:G_R + 128], ALU.mult)
        c.act(xa[:, 128:256], g_[:], AF.Sigmoid)
        c.tt(xa[:, 128:256], xa[:, 128:256], g_[:], ALU.mult)
        c.tt(o[:], xb[:, 0:128], xa[:, 128:256], ALU.mult)
        c.dma(o_ret[sl, :], o[:], q="pool")
    c.finish("pool")
    return c


def rope_tables(dim):
    n = dim // 4
    inv = (np.float32(10000.0) ** (-np.arange(n, dtype=np.float32) / np.float32(n))).astype(np.float32)
    t = np.arange(NLAT)
    row = (t // 64).astype(np.float32)
    col = (t % 64).astype(np.float32)
    ang = np.concatenate([row[:, None] * inv[None, :], col[:, None] * inv[None, :]], 1).astype(np.float32)
    return np.cos(ang).astype(np.float32), np.sin(ang).astype(np.float32)


def ret_consts():
    j = np.arange(128, dtype=np.float32)[:, None]
    t = np.arange(128, dtype=np.float32)[None, :]
    z = np.zeros((128, 128), np.float32)
    rc = np.stack([np.maximum(t - j, 0), np.maximum(j - t, 0), (t >= j).astype(np.float32),
                   (j > t).astype(np.float32), t + 1 + z, 128 - t + z], 1).astype(np.float32)
    rcol = np.stack([127 - j[:, 0], j[:, 0]], 1).astype(np.float32)
    return np.ascontiguousarray(rc), np.ascontiguousarray(rcol)


def rep(v, n=128):
    return np.ascontiguousarray(np.broadcast_to(np.asarray(v, np.float32).reshape(1, -1), (n, np.size(v))))


def host_att(pl, pc, P, l):
    c128, s128 = rope_tables(128)
    c64, s64 = rope_tables(64)
    rc, rcol = ret_consts()
    ims = []
    for j in range(NCORES):
        b, h = j // 4, j % 4
        pa = np.concatenate([pl[b], pc[b]], 0)
        kv = h // 2
        cut = lambda o, w: np.ascontiguousarray(pa[:, o:o + w])
        ims.append({
            "ident": IDENT,
            "gq": cut(512 + h * 128, 128), "gk": cut(1024 + kv * 128, 128), "gv": cut(1280 + kv * 128, 128),
            "gqn": rep(P["gqa_q_norm"][l]), "gkn": rep(P["gqa_k_norm"][l]),
            "mcq": cut(3072, 512), "mckv": cut(3584, 256), "mkr": cut(3840, 64),
            "mqn": rep(P["mla_q_norm"][l]), "mkvn": rep(P["mla_kv_norm"][l]),
            "wuq": np.ascontiguousarray(P["mla_w_uq"][l][:, h * 192:(h + 1) * 192]),
            "wukv": np.ascontiguousarray(P["mla_w_ukv"][l][:, h * 256:(h + 1) * 256]),
            "rq": cut(1536 + h * 64, 64), "rk": cut(1792 + h * 64, 64),
            "rv": cut(2048 + h * 128, 128), "rg": cut(2560 + h * 128, 128),
            "rpar": rep([P["ret_decay_f"][l][h], P["ret_decay_b"][l][h]]),
            "rgain": rep(P["ret_norm"][l][h * 128:(h + 1) * 128]),
            "cos128": c128, "sin128": s128, "cos64": c64, "sin64": s64, "rconst": rc, "rcol": rcol,
        })
    res = run(prog("att", build_att), ims)
    outs = {}
    for nm in ("o_gqa", "o_ret", "o_mla"):
        lat = np.zeros((2, NLAT, 512), np.float32)
        ctx = np.zeros((2, 256, 512), np.float32)
        for j in range(NCORES):
            b, h = j // 4, j % 4
            lat[b, :, h * 128:(h + 1) * 128] = res[j][nm][:NLAT]
            ctx[b, :, h * 128:(h + 1) * 128] = res[j][nm][NLAT:]
        outs[nm] = (lat, ctx)
    return outs


PI = float(np.pi)


def rsin(c, out, x, kf, ki):
    c.ts(kf, x, 1.0 / (2 * PI), None, ALU.mult)
    c.copy(ki, kf)
    c.copy(kf, ki)
    c.stt(kf, kf, -2 * PI, x, ALU.mult, ALU.add)
    c.ts(kf, kf, PI, -PI, ALU.min, ALU.max)
    c.act(out, kf, AF.Sin)


def build_s5():
    c = Ctx()
    cm = Common(c)
    uT = c.dram_in("uT", [2, 2, 64, NALL])
    apar = c.dram_in("apar", [128, 2, 2, 3])
    bw_d = c.dram_in("bw", [128, 2, 2, 32])
    cw_d = c.dram_in("cw", [128, 2, 2, 32])
    tidx_d = c.dram_in("tidx", [128, NALL])
    yT = c.dram_out("yT", [2, 2, 64, NALL])
    T_ = NALL
    tidx = c.sb([128, T_], F32, "tidx_sb")
    c.dma(tidx[:], tidx_d[:, :])
    ap_ = c.sb([128, 2, 2, 3], F32, "apar_sb")
    bw = c.sb([128, 2, 2, 32], F32, "bw_sb")
    cw = c.sb([128, 2, 2, 32], F32, "cw_sb")
    c.dma(ap_[:], apar[:, :, :, :])
    c.dma(bw[:], bw_d[:, :, :, :])
    c.dma(cw[:], cw_d[:, :, :, :])
    ncw = c.sb([128, 2, 32], F32, "ncw")
    c.ts(ncw[:], cw[:, :, 1, :], -1.0, None, ALU.mult)
    tabc = c.sb([128, T_], F32, "tabc")
    tabs = c.sb([128, T_], F32, "tabs")
    A1 = c.sb([128, T_], F32, "A1")
    A2 = c.sb([128, T_], F32, "A2")
    G1 = c.sb([128, T_], F32, "G1")
    G2 = c.sb([128, T_], F32, "G2")
    RT = c.sb([128, T_], F32, "RT")
    uts = [c.sb([32, 512], F32, f"ut{i}") for i in range(3)]
    KI = c.sb([128, T_], I32, "KI")
    ski = c.sb([128, 2], I32, "ski")
    sc = c.sb([128, 24], F32, "s5sc")
    pib = c.sb([128, 1], F32, "pib")
    c.memset(pib[:], PI)
    bb = c.sb([128, 2, 32], F32, "bb")
    bbT = c.sb([32, 2, 128], F32, "bbT")
    m1 = [c.sb([128, 512], F32, f"m1_{i}") for i in range(2)]
    m2 = [c.sb([128, 512], F32, f"m2_{i}") for i in range(2)]
    yo = [c.sb([32, 512], F32, f"yo{i}") for i in range(2)]
    hre = [c.sb([128, 512], F32, f"hre{i}") for i in range(2)]
    him = [c.sb([128, 512], F32, f"him{i}") for i in range(2)]
    m3 = [c.sb([128, 512], F32, f"m3_{i}") for i in range(2)]
    m4 = [c.sb([128, 512], F32, f"m4_{i}") for i in range(2)]
    psr = [c.ps([128, 512]) for _ in range(2)]
    psi = [c.ps([128, 512]) for _ in range(2)]
    psy = [c.ps([128, 512]) for _ in range(2)]
    pst = c.ps([128, 512])
    S = lambda i: sc[:, i:i + 1]
    nblk = (T_ + 511) // 512
    k = 0
    for pt in range(2):
        for d in range(2):
            a_re, a_im, ldt = ap_[:, pt, d, 0:1], ap_[:, pt, d, 1:2], ap_[:, pt, d, 2:3]
            c.act(S(0), ldt, AF.Exp)
            c.tt(S(1), a_re, S(0), ALU.mult)
            c.act(S(1), S(1), AF.Exp)
            c.tt(S(2), a_im, S(0), ALU.mult)
            rsin(c, S(6), S(2), S(8), ski[:, 0:1])
            c.ts(S(9), S(2), PI / 2, None, ALU.add)
            rsin(c, S(7), S(9), S(8), ski[:, 0:1])
            c.tt(S(10), S(1), S(7), ALU.mult)
            c.tt(S(11), S(1), S(6), ALU.mult)
            c.tt(S(12), a_re, a_re, ALU.mult)
            c.tt(S(13), a_im, a_im, ALU.mult)
            c.tt(S(12), S(12), S(13), ALU.add)
            c.emit("dve", [sc], [sc], lambda g: g.reciprocal(sc.h[:, 12:13], sc.h[:, 12:13]))
            c.ts(S(14), S(10), -1.0, None, ALU.add)
            c.tt(S(15), S(14), a_re, ALU.mult)
            c.tt(S(16), S(11), a_im, ALU.mult)
            c.tt(S(15), S(15), S(16), ALU.add)
            c.tt(S(15), S(15), S(12), ALU.mult)
            c.tt(S(16), S(11), a_re, ALU.mult)
            c.tt(S(17), S(14), a_im, ALU.mult)
            c.tt(S(16), S(16), S(17), ALU.subtract)
            c.tt(S(16), S(16), S(12), ALU.mult)
            c.ts(S(17), S(16), -1.0, None, ALU.mult)
            c.ts(bb[:, 0, :], bw[:, pt, 0, :], S(15), None, ALU.mult)
            c.stt(bb[:, 0, :], bw[:, pt, 1, :], S(17), bb[:, 0, :], ALU.mult, ALU.add)
            c.ts(bb[:, 1, :], bw[:, pt, 1, :], S(15), None, ALU.mult)
            c.stt(bb[:, 1, :], bw[:, pt, 0, :], S(16), bb[:, 1, :], ALU.mult, ALU.add)
            for ri in range(2):
                c.transpose(pst[0:32, ri * 128:(ri + 1) * 128], bb[:, ri, :], cm.ident[:])
            c.copy(bbT[:, 0, :], pst[0:32, 0:128], e="act")
            c.copy(bbT[:, 1, :], pst[0:32, 128:256], e="act")
            c.ts(A1[:], tidx[:], S(2), None, ALU.mult)
            rsin(c, tabs[:], A1[:], G1[:], KI[:])
            c.ts(A1[:], A1[:], PI / 2, None, ALU.add)
            rsin(c, tabc[:], A1[:], G1[:], KI[:])
            c.ts(RT[:], tidx[:], 0.0, S(1), ALU.mult, ALU.add)
            for b in range(2):
                for nb in range(nblk):
                    lo = nb * 512
                    w = min(512, T_ - lo)
                    pr, pi_ = psr[nb % 2], psi[nb % 2]
                    ut = uts[nb % 3]
                    c.dma(ut[:, 0:w], uT[d, b, pt * 32:(pt + 1) * 32, lo:lo + w])
                    c.mm(pr[:, 0:w], bbT[:, 0, :], ut[:, 0:w])
                    c.mm(pi_[:, 0:w], bbT[:, 1, :], ut[:, 0:w])
                    a, b_ = m1[nb % 2], m2[nb % 2]
                    cc, ss = tabc[:, lo:lo + w], tabs[:, lo:lo + w]
                    c.tt(a[:, 0:w], pr[:, 0:w], cc, ALU.mult)
                    c.tt(b_[:, 0:w], pi_[:, 0:w], ss, ALU.mult, e="pool" if False else "dve")
                    c.tt(A1[:, lo:lo + w], a[:, 0:w], b_[:, 0:w], ALU.add)
                    c.tt(a[:, 0:w], pi_[:, 0:w], cc, ALU.mult)
                    c.tt(b_[:, 0:w], pr[:, 0:w], ss, ALU.mult)
                    c.tt(A2[:, lo:lo + w], a[:, 0:w], b_[:, 0:w], ALU.subtract)
                c.emit("dve", [G1], [RT, A1], lambda g: g.tensor_tensor_scan(G1.h[:], RT.h[:], A1.h[:], 0.0, ALU.mult, ALU.add))
                c.emit("dve", [G2], [RT, A2], lambda g: g.tensor_tensor_scan(G2.h[:], RT.h[:], A2.h[:], 0.0, ALU.mult, ALU.add))
                for nb in range(nblk):
                    lo = nb * 512
                    w = min(512, T_ - lo)
                    e2 = "pool" if nb % 3 != 2 else "dve"
                    a, b_ = (m3[(nb // 3) % 2], m4[(nb // 3) % 2]) if e2 == "dve" else (m1[nb % 2], m2[nb % 2])
                    hr, hi = hre[nb % 2], him[nb % 2]
                    cc, ss = tabc[:, lo:lo + w], tabs[:, lo:lo + w]
                    c.tt(a[:, 0:w], G1[:, lo:lo + w], cc, ALU.mult, e=e2)
                    c.tt(b_[:, 0:w], G2[:, lo:lo + w], ss, ALU.mult, e=e2)
                    c.tt(hr[:, 0:w], a[:, 0:w], b_[:, 0:w], ALU.subtract, e=e2)
                    c.tt(a[:, 0:w], G1[:, lo:lo + w], ss, ALU.mult, e=e2)
                    c.tt(b_[:, 0:w], G2[:, lo:lo + w], cc, ALU.mult, e=e2)
                    c.tt(hi[:, 0:w], a[:, 0:w], b_[:, 0:w], ALU.add, e=e2)
                    py = psy[nb % 2]
                    c.mm(py[0:32, 0:w], cw[:, pt, 0, :], hr[:, 0:w], start=True, stop=False)
                    c.mm(py[0:32, 0:w], ncw[:, pt, :], hi[:, 0:w], start=False, stop=True)
                    o = yo[k % 2]
                    k += 1
                    c.copy(o[:, 0:w], py[0:32, 0:w], e="act")
                    c.dma(yT[d, b, pt * 32:(pt + 1) * 32, lo:lo + w], o[:, 0:w], q="sp")
    c.finish("sp")
    return c


def host_s5(pl, pc, P, l):
    ims = []
    tidx = rep(np.arange(NALL, dtype=np.float32))
    for j in range(NCORES):
        uT = np.zeros((2, 2, 64, NALL), np.float32)
        for b in range(2):
            ul = pl[b][:, j * 64:(j + 1) * 64]
            uc = pc[b][:, j * 64:(j + 1) * 64]
            uT[0, b] = np.concatenate([uc, ul], 0).T
            uT[1, b] = np.concatenate([uc[::-1], ul[::-1]], 0).T
        apar = np.zeros((128, 2, 2, 3), np.float32)
        bw = np.zeros((128, 2, 2, 32), np.float32)
        cw = np.zeros((128, 2, 2, 32), np.float32)
        for pt in range(2):
            for gl in range(2):
                g = j * 4 + pt * 2 + gl
                rows = slice(gl * 64, (gl + 1) * 64)
                for d, sfx in enumerate(("f", "b")):
                    apar[rows, pt, d, 0] = P["s5_a_re_" + sfx][l][g]
                    apar[rows, pt, d, 1] = P["s5_a_im_" + sfx][l][g]
                    apar[rows, pt, d, 2] = P["s5_log_dt_" + sfx][l][g]
                bw[rows, pt, 0, gl * 16:(gl + 1) * 16] = P["s5_b_re"][l][g]
                bw[rows, pt, 1, gl * 16:(gl + 1) * 16] = P["s5_b_im"][l][g]
                cw[rows, pt, 0, gl * 16:(gl + 1) * 16] = P["s5_c_re"][l][g].T
                cw[rows, pt, 1, gl * 16:(gl + 1) * 16] = P["s5_c_im"][l][g].T
        ims.append({"ident": IDENT, "uT": uT, "apar": apar, "bw": bw, "cw": cw, "tidx": tidx})
    res = run(prog("s5", build_s5), ims)
    outs = []
    for d in range(2):
        lat = np.zeros((2, NLAT, 512), np.float32)
        ctx = np.zeros((2, 256, 512), np.float32)
        for j in range(NCORES):
            for b in range(2):
                y = res[j]["yT"][d, b].T
                yc, yl = y[:256], y[256:]
                if d == 1:
                    yc, yl = yc[::-1], yl[::-1]
                lat[b, :, j * 64:(j + 1) * 64] = yl
                ctx[b, :, j * 64:(j + 1) * 64] = yc
        outs.append((lat, ctx))
    return outs


def build_merge_a():
    c = Ctx()
    cm = Common(c)
    x = c.dram_in("x", [NTOK, D])
    mv_d = c.dram_in("mv", [128, 4, KT])
    yf, yb, u = c.dram_in("yf", [NTOK, 512]), c.dram_in("yb", [NTOK, 512]), c.dram_in("u", [NTOK, 512])
    s5d = c.dram_in("s5d", [128, 512])
    wglu = c.dram_in("wglu", [512, 512])
    obr = c.dram_in("obr", [3, NTOK, 512])
    wbr = c.dram_in("wbr", [4, 512, D])
    wg = c.dram_in("wg", [D, 4 * D])
    bg = c.dram_in("bg", [1, 4 * D])
    m_out = c.dram_out("m", [NTOK, D])
    mv = load_mod_cols(c, mv_d, 4)
    c.ts(mv[:, 1, :], mv[:, 1, :], 1.0, None, ALU.add)
    c.ts(mv[:, 3, :], mv[:, 3, :], 1.0, None, ALU.add)
    dr = c.sb([128, 512], F32, "s5d_sb")
    c.dma(dr[:], s5d[:, :])
    wgl = c.sb([128, 4, 512], F32, "wglu_sb")
    c.dma(wgl[:], wglu.rearrange("(k p) n -> p k n", p=128))
    ones = c.sb([1, 128], F32, "ones1")
    c.memset(ones[:], 1.0)
    bgts = [c.sb([1, 512], F32, f"bg_sb{i}") for i in range(2)]
    TP = NT
    hT = c.sb([128, KT, TP * 128], BF16, "hT")
    oT = c.sb([128, 4, 4, TP * 128], BF16, "oT")
    macc = c.sb([128, TP, 512], F32, "macc")
    xt = c.sb([128, D], F32, "xt")
    tmp = ln_tmp(c)
    a = [c.sb([128, 512], F32, f"ma{i}") for i in range(4)]
    zT = c.sb([128, 4, 128], F32, "zT")
    pst = [c.ps([128, 512]) for _ in range(2)]
    psg = [c.ps([128, 512]) for _ in range(2)]
    psp = [c.ps([128, 512]) for _ in range(2)]
    wgb = [c.sb([128, KT, 512], BF16, f"wgb{i}") for i in range(2)]
    wbb = [c.sb([128, 4, 512], BF16, f"wbb{i}") for i in range(2)]
    gsb = [c.sb([128, 512], F32, f"gsb{i}") for i in range(2)]
    wgv = wg.rearrange("(k p) n -> p k n", p=128)
    for p0 in range(0, NT, TP):
        tiles = list(range(p0, min(NT, p0 + TP)))
        for li, t in enumerate(tiles):
            rows = slice(t * 128, (t + 1) * 128)
            c.dma(xt[:], x[rows, :])
            ln_hat(c, cm, xt[:], xt[:], tmp=tmp)
            j = 0 if t < 8 else 2
            to_fm(c, cm, xt, hT, li, pst, scale_cols=mv[:, j + 1, :], bias_cols=mv[:, j, :])
            c.dma(a[0][:], yf[rows, :])
            c.dma(a[1][:], yb[rows, :])
            c.dma(a[2][:], u[rows, :])
            c.tt(a[0][:], a[0][:], a[1][:], ALU.add)
            c.tt(a[2][:], a[2][:], dr[:], ALU.mult)
            c.tt(a[0][:], a[0][:], a[2][:], ALU.add)
            c.tt(a[1][:], a[0][:], a[0][:], ALU.mult)
            c.ts(a[1][:], a[1][:], 0.044715, 1.0, ALU.mult, ALU.add)
            c.tt(a[1][:], a[1][:], a[0][:], ALU.mult)
            c.act(a[1][:], a[1][:], AF.Tanh, scale=0.7978845608028654)
            c.stt(a[1][:], a[1][:], 1.0, a[0][:], ALU.add, ALU.mult)
            c.ts(a[1][:], a[1][:], 0.5, None, ALU.mult)
            for k in range(4):
                p = pst[k % 2]
                c.transpose(p[:, 0:128], a[1][:, k * 128:(k + 1) * 128], cm.ident[:])
                c.copy(zT[:, k, :], p[:, 0:128], e="act")
            p = psg[0]
            for k in range(4):
                c.mm(p[:, 0:512], zT[:, k, :], wgl[:, k, :], start=(k == 0), stop=(k == 3))
            c.act(a[2][:], p[:, 0:512], AF.Sigmoid)
            c.tt(a[3][:], a[1][:], a[2][:], ALU.mult)
            for k in range(4):
                p = pst[k % 2]
                c.transpose(p[:, 0:128], a[3][:, k * 128:(k + 1) * 128], cm.ident[:])
                c.copy(oT[:, 0, k, li * 128:(li + 1) * 128], p[:, 0:128], e="act")
            for br in range(3):
                c.dma(a[0][:], obr[br, rows, :])
                for k in range(4):
                    p = pst[k % 2]
                    c.transpose(p[:, 0:128], a[0][:, k * 128:(k + 1) * 128], cm.ident[:])
                    c.copy(oT[:, br + 1, k, li * 128:(li + 1) * 128], p[:, 0:128], e="act")
        i = 0
        for nb in range(4):
            for k in range(4):
                wgt, wbt = wgb[i % 2], wbb[i % 2]
                i += 1
                col = k * D + nb * 512
                bgt = bgts[i % 2]
                c.dma(bgt[:], bg[:, col:col + 512])
                c.dma(wgt[:], wgv[:, :, col:col + 512], q="pool")
                c.dma(wbt[:], wbr[k, :, nb * 512:(nb + 1) * 512].rearrange("(k p) n -> p k n", p=128), q="pool")
                for li, t in enumerate(tiles):
                    pg, pp = psg[li % 2], psp[li % 2]
                    for kk in range(KT):
                        c.mm(pg[:, 0:512], hT[:, kk, li * 128:(li + 1) * 128], wgt[:, kk, :], start=(kk == 0), stop=False)
                    c.mm(pg[:, 0:512], ones[:, :], bgt[:, :], start=False, stop=True)
                    g = gsb[li % 2]
                    c.act(g[:], pg[:, 0:512], AF.Sigmoid)
                    for kk in range(4):
                        c.mm(pp[:, 0:512], oT[:, k, kk, li * 128:(li + 1) * 128], wbt[:, kk, :], start=(kk == 0), stop=(kk == 3))
                    if k == 0:
                        c.tt(macc[:, li, :], g[:], pp[:, 0:512], ALU.mult)
                    else:
                        c.tt(g[:], g[:], pp[:, 0:512], ALU.mult)
                        c.tt(macc[:, li, :], macc[:, li, :], g[:], ALU.add)
            for li, t in enumerate(tiles):
                c.dma(m_out[t * 128:(t + 1) * 128, nb * 512:(nb + 1) * 512], macc[:, li, :], q="sp")
    c.finish("sp")
    return c


def host_merge_a(x_lat, x_ctx, mod, pl, pc, s5o, atto, P, l):
    xs = tok_shard(x_lat, x_ctx)
    (lf, cf), (lb, cb) = s5o
    yfs, ybs = tok_shard(lf, cf), tok_shard(lb, cb)
    us = tok_shard(pl[:, :, :512], pc[:, :, :512])
    brs = [tok_shard(*atto[nm]) for nm in ("o_gqa", "o_ret", "o_mla")]
    ims = []
    for j in range(NCORES):
        ims.append({"ident": IDENT, "x": xs[j], "mv": mod_cols(mod, j, [0, 1]),
                    "yf": yfs[j], "yb": ybs[j], "u": us[j], "s5d": rep(P["s5_d"][l]),
                    "wglu": P["s5_w_glu"][l], "obr": np.stack([b_[j] for b_ in brs], 0),
                    "wbr": P["w_branch"][l], "wg": P["w_gate"][l], "bg": P["b_gate"][l][None, :]})
    res = run(prog("merge_a", build_merge_a), ims)
    return tok_unshard([r["m"] for r in res], D)


ALPHA = float((2 * 2) ** 0.25)


def build_merge_b():
    c = Ctx()
    cm = Common(c)
    m = c.dram_in("m", [NTOK, D])
    x = c.dram_in("x", [NTOK, D])
    w = c.dram_in("w_out", [D, D])
    reps_d = c.dram_in("reps", [128, 8, D])
    rw_d = c.dram_in("rw", [D, 16])
    x1_o = c.dram_out("x1", [NTOK, D])
    h2_o = c.dram_out("h2", [NTOK, D])
    aff_o = c.dram_out("aff", [NTOK, 16])
    reps = c.sb([128, 8, D], F32, "reps_sb")
    for i in range(8):
        c.dma(reps[:, i, :], reps_d[:, i, :])
    c.ts(reps[:, 4, :], reps[:, 4, :], 1.0, None, ALU.add)
    c.ts(reps[:, 6, :], reps[:, 6, :], 1.0, None, ALU.add)
    rw = c.sb([128, KT, 16], F32, "rw_sb")
    c.dma(rw[:], rw_d.rearrange("(k p) n -> p k n", p=128))
    TP = 5
    mt = c.sb([128, D], F32, "mt")
    X = c.sb([128, TP, D], F32, "X")
    xh = c.sb([128, D], F32, "xh")
    x1t = c.sb([128, D], F32, "x1t")
    mT = c.sb([128, KT, TP * 128], BF16, "mT")
    rT = c.sb([128, KT, 128], F32, "rT")
    tmp = ln_tmp(c)
    tb = [c.sb([128, 512], F32, f"tb{i}") for i in range(2)]
    sm = c.sb([128, 40], F32, "sm")
    wbufs = [c.sb([128, KT, 512], BF16, f"wb{i}") for i in range(2)]
    pst = [c.ps([128, 512]) for _ in range(2)]
    pso = [c.ps([128, 512]) for _ in range(2)]
    psr = c.ps([128, 512])
    for p0 in range(0, NT, TP):
        tiles = list(range(p0, min(NT, p0 + TP)))
        for li, t in enumerate(tiles):
            rows = slice(t * 128, (t + 1) * 128)
            c.dma(mt[:], m[rows, :])
            c.dma(X[:, li, :], x[rows, :])
            to_fm(c, cm, mt, mT, li, pst)

        def consume(li, nb, lo, bw, p, tiles=tiles):
            g1 = reps[:, 0 if tiles[li] < 8 else 1, :]
            b_ = tb[(li + nb) % 2]
            c.tt(b_[:, 0:bw], p[:, 0:bw], g1[:, lo:lo + bw], ALU.mult)
            c.stt(X[:, li, lo:lo + bw], X[:, li, lo:lo + bw], ALU_ALPHA, b_[:, 0:bw], ALU.mult, ALU.add)

        stream_linear(c, mT, w, D, len(tiles), consume, wbufs, pso)
        for li, t in enumerate(tiles):
            rows = slice(t * 128, (t + 1) * 128)
            lat = t < 8
            ln_hat(c, cm, X[:, li, :], xh[:], tmp=tmp)
            c.tt(xh[:], xh[:], reps[:, 2, :], ALU.mult)
            c.tt(x1t[:], xh[:], reps[:, 3, :], ALU.add)
            c.dma(x1_o[rows, :], x1t[:], q="sp")
            ln_hat(c, cm, x1t[:], xh[:], tmp=tmp)
            c.tt(xh[:], xh[:], reps[:, 4 if lat else 6, :], ALU.mult)
            c.tt(mt[:], xh[:], reps[:, 5 if lat else 7, :], ALU.add)
            c.dma(h2_o[rows, :], mt[:], q="sp")
            to_fm(c, cm, mt, rT, 0, pst)
            for k in range(KT):
                c.mm(psr[:, 0:16], rT[:, k, :], rw[:, k, :], start=(k == 0), stop=(k == KT - 1))
            c.copy(sm[:, 0:16], psr[:, 0:16])
            c.reduce(sm[:, 32:33], sm[:, 0:16], ALU.max)
            c.ts(sm[:, 32:33], sm[:, 32:33], -1.0, None, ALU.mult)
            c.act(sm[:, 0:16], sm[:, 0:16], AF.Exp, bias=sm[:, 32:33], scale=1.0)
            c.reduce(sm[:, 33:34], sm[:, 0:16], ALU.add)
            c.emit("dve", [sm], [sm], lambda g: g.reciprocal(sm.h[:, 34:35], sm.h[:, 33:34]))
            c.ts(sm[:, 16:32], sm[:, 0:16], sm[:, 34:35], None, ALU.mult)
            c.dma(aff_o[rows, :], sm[:, 16:32], q="sp")
    c.finish("sp")
    return c


ALU_ALPHA = ALPHA


def host_merge_b(m_lat, m_ctx, x_lat, x_ctx, mod, P, l):
    ms = tok_shard(m_lat, m_ctx)
    xs = tok_shard(x_lat, x_ctx)
    ims = []
    for j in range(NCORES):
        b = j // 4
        seg = lambda r_, i: mod[r_, i * D:(i + 1) * D]
        reps = np.stack([rep(seg(b, 2)), rep(seg(2, 2)), rep(P["ln1_g"][l]), rep(P["ln1_b"][l]),
                         rep(seg(b, 4)), rep(seg(b, 3)), rep(seg(2, 4)), rep(seg(2, 3))], 1)
        ims.append({"ident": IDENT, "m": ms[j], "x": xs[j], "w_out": P["w_out"][l],
                    "reps": np.ascontiguousarray(reps), "rw": P["router_w"][l]})
    res = run(prog("merge_b", build_merge_b), ims)
    return (tok_unshard([r["x1"] for r in res], D), tok_unshard([r["h2"] for r in res], D),
            tok_unshard([r["aff"] for r in res], 16))


CAP_L, CAP_C = 512, 32


def build_topk():
    c = Ctx()
    a_l = c.dram_in("affT", [32, NLAT])
    a_c = c.dram_in("affcT", [32, 256])
    g_l, i_l = c.dram_out("gate", [32, CAP_L]), c.dram_out("idx", [32, CAP_L], U32)
    g_c, i_c = c.dram_out("gatec", [32, CAP_C]), c.dram_out("idxc", [32, CAP_C], U32)
    for (src, n, cap, go, io, tag) in ((a_l, NLAT, CAP_L, g_l, i_l, "l"), (a_c, 256, CAP_C, g_c, i_c, "c")):
        w = c.sb([32, n], F32, "work" + tag)
        gv = c.sb([32, cap], F32, "gv" + tag)
        iv = c.sb([32, cap], U32, "iv" + tag)
        c.dma(w[:], src[:, :])
        for r in range(cap // 8):
            sl = slice(r * 8, (r + 1) * 8)
            c.emit("dve", [gv], [w], lambda g, sl=sl, gv=gv, w=w: g.max(out=gv.h[:, sl], in_=w.h[:]))
            c.emit("dve", [iv], [gv, w], lambda g, sl=sl, gv=gv, iv=iv, w=w: g.max_index(out=iv.h[:, sl], in_max=gv.h[:, sl], in_values=w.h[:]))
            c.emit("dve", [w], [gv, w], lambda g, sl=sl, gv=gv, w=w: g.match_replace(out=w.h[:], in_to_replace=gv.h[:, sl], in_values=w.h[:], imm_value=-1.0))
        c.dma(go[:, :], gv[:], q="pool")
        c.dma(io[:, :], iv[:], q="pool")
    c.finish("pool")
    return c


def host_topk(aff_l, aff_c):
    affT = np.ascontiguousarray(aff_l.transpose(0, 2, 1).reshape(32, NLAT))
    affcT = np.ascontiguousarray(aff_c.transpose(0, 2, 1).reshape(32, 256))
    res = run(prog("topk", build_topk), [{"affT": affT, "affcT": affcT}] * NCORES)[0]
    return (res["gate"].reshape(2, 16, CAP_L), res["idx"].reshape(2, 16, CAP_L).astype(np.int64),
            res["gatec"].reshape(2, 16, CAP_C), res["idxc"].reshape(2, 16, CAP_C).astype(np.int64))


FF = 1024
ER = NTOK


def build_expert():
    c = Ctx()
    cm = Common(c)
    xs = c.dram_in("xs", [2, ER, D])
    gt_d = c.dram_in("gt", [128, 2, NT])
    wg_d, wu_d, wd_d = c.dram_in("wg", [2, D, FF]), c.dram_in("wu", [2, D, FF]), c.dram_in("wd", [2, FF, D])
    y = c.dram_out("y", [2, ER, D])
    gt = c.sb([128, 2, NT], F32, "gt_sb")
    c.dma(gt[:], gt_d[:, :, :])
    xT = c.sb([128, KT, ER], BF16, "xT")
    hT = c.sb([128, 8, ER], BF16, "hmT")
    xt = [c.sb([128, D], F32, f"xt{i}") for i in range(2)]
    wgb = [c.sb([128, KT, 128], BF16, f"wgb{i}") for i in range(2)]
    wub = [c.sb([128, KT, 128], BF16, f"wub{i}") for i in range(2)]
    wdb = [c.sb([128, 8, 512], BF16, f"wdb{i}") for i in range(2)]
    sg = [c.sb([128, 512], F32, f"sg{i}") for i in range(2)]
    ob = [c.sb([128, 512], F32, f"ob{i}") for i in range(3)]
    pst = [c.ps([128, 512]) for _ in range(2)]
    psa = [c.ps([128, 512]) for _ in range(2)]
    psu = [c.ps([128, 512]) for _ in range(2)]
    pso = [c.ps([128, 512]) for _ in range(2)]
    chunks = [(0, 512), (512, 512), (1024, 128)]
    k0 = 0
    for e in range(2):
        for t in range(NT):
            x_ = xt[t % 2]
            c.dma(x_[:], xs[e, t * 128:(t + 1) * 128, :])
            to_fm(c, cm, x_, xT, t, pst)
        for fb in range(FF // 128):
            wg_, wu_ = wgb[fb % 2], wub[fb % 2]
            c.dma(wg_[:], wg_d[e, :, fb * 128:(fb + 1) * 128].rearrange("(k p) n -> p k n", p=128), q="pool")
            c.dma(wu_[:], wu_d[e, :, fb * 128:(fb + 1) * 128].rearrange("(k p) n -> p k n", p=128), q="pool")
            for ci, (lo, w) in enumerate(chunks):
                pa, pu = psa[ci % 2], psu[ci % 2]
                for k in range(KT):
                    c.mm(pa[:, 0:w], wg_[:, k, :], xT[:, k, lo:lo + w], start=(k == 0), stop=(k == KT - 1))
                for k in range(KT):
                    c.mm(pu[:, 0:w], wu_[:, k, :], xT[:, k, lo:lo + w], start=(k == 0), stop=(k == KT - 1))
                s_ = sg[ci % 2]
                c.act(s_[:, 0:w], pa[:, 0:w], AF.Sigmoid)
                c.tt(s_[:, 0:w], s_[:, 0:w], pa[:, 0:w], ALU.mult)
                c.tt(hT[:, fb, lo:lo + w], s_[:, 0:w], pu[:, 0:w], ALU.mult)
        for nb in range(D // 512):
            wd_ = wdb[nb % 2]
            c.dma(wd_[:], wd_d[e, :, nb * 512:(nb + 1) * 512].rearrange("(k p) n -> p k n", p=128), q="pool")
            for t in range(NT):
                p = pso[t % 2]
                for k in range(8):
                    c.mm(p[:, 0:512], hT[:, k, t * 128:(t + 1) * 128], wd_[:, k, :], start=(k == 0), stop=(k == 7))
                o = ob[k0 % 3]
                k0 += 1
                c.ts(o[:], p[:, 0:512], gt[:, e, t:t + 1], None, ALU.mult)
                c.dma(y[e, t * 128:(t + 1) * 128, nb * 512:(nb + 1) * 512], o[:], q="sp")
    c.finish("sp")
    return c


def host_expert(h2l, h2c, gate, idx, gatec, idxc, P, l):
    ims = []
    for j in range(NCORES):
        xs = np.zeros((2, ER, D), np.float32)
        gt = np.zeros((2, ER), np.float32)
        for ei in range(2):
            e = 2 * j + ei
            for b in range(2):
                xs[ei, b * 512:(b + 1) * 512] = h2l[b][idx[b, e]]
                gt[ei, b * 512:(b + 1) * 512] = gate[b, e]
                xs[ei, 1024 + b * 32:1024 + (b + 1) * 32] = h2c[b][idxc[b, e]]
                gt[ei, 1024 + b * 32:1024 + (b + 1) * 32] = gatec[b, e]
        gtp = np.ascontiguousarray(gt.reshape(2, NT, 128).transpose(2, 0, 1))
        ims.append({"ident": IDENT, "xs": xs, "gt": gtp,
                    "wg": P["moe_w_gate"][l][2 * j:2 * j + 2], "wu": P["moe_w_up"][l][2 * j:2 * j + 2],
                    "wd": P["moe_w_down"][l][2 * j:2 * j + 2]})
    res = run(prog("expert", build_expert), ims)
    Y = np.stack([res[j]["y"] for j in range(NCORES)], 0).reshape(16, ER, D)
    return Y


NSL = 16 * CAP_L
NSC = 16 * CAP_C


def build_combine():
    c = Ctx()
    yl = c.dram_in("yl", [NSL, D])
    yc = c.dram_in("yc", [NSC, D])
    il_d = c.dram_in("il", [128, NSL // 128], I32)
    ic_d = c.dram_in("ic", [128, NSC // 128], I32)
    tok_d = c.dram_in("tok", [128, NTOK])
    x1 = c.dram_in("x1", [NTOK, D])
    reps_d = c.dram_in("reps", [128, 4, D])
    out = c.dram_out("x2", [NTOK, D])
    reps = c.sb([128, 4, D], F32, "reps_sb")
    for i in range(4):
        c.dma(reps[:, i, :], reps_d[:, i, :])
    tok = c.sb([128, NTOK], F32, "tok_sb")
    c.dma(tok[:], tok_d[:, :])
    ili = c.sb([128, NSL // 128], I32, "ili")
    ici = c.sb([128, NSC // 128], I32, "ici")
    c.dma(ili[:], il_d[:, :])
    c.dma(ici[:], ic_d[:, :])
    il = c.sb([128, NSL // 128], F32, "il_f")
    ic = c.sb([128, NSC // 128], F32, "ic_f")
    c.copy(il[:], ili[:])
    c.copy(ic[:], ici[:])
    X = c.sb([128, NT, D], F32, "X")
    for t in range(NT):
        c.dma(X[:, t, :], x1[t * 128:(t + 1) * 128, :])
    yb = [c.sb([128, 8, 512], BF16, f"yb{i}") for i in range(3)]
    sb_ = [c.sb([128, 1024], BF16, f"sel{i}") for i in range(3)]
    tb = [c.sb([128, 512], F32, f"tb{i}") for i in range(2)]
    acc = [c.ps([128, 512]) for _ in range(8)]
    xh = c.sb([128, D], F32, "xh")
    tmp = ln_tmp(c)
    si = 0
    for nb in range(4):
        cols = slice(nb * 512, (nb + 1) * 512)
        nkt = NSL // 128
        for kt in range(nkt):
            yg = yb[(kt // 8) % 3]
            if kt % 8 == 0:
                c.dma(yg[:], yl[kt * 128:(kt + 8) * 128, cols].rearrange("(k p) n -> p k n", p=128), q="pool")
            y_ = yg[:, kt % 8, :]
            s_ = sb_[si % 3]
            si += 1
            c.ts(s_[:], tok[:, 0:1024], il[:, kt:kt + 1], None, ALU.is_equal)
            for t in range(8):
                c.mm(acc[t][:, 0:512], s_[:, t * 128:(t + 1) * 128], y_, start=(kt == 0), stop=(kt == nkt - 1))
        for t in range(8):
            b_ = tb[t % 2]
            c.tt(b_[:], acc[t][:, 0:512], reps[:, 0, cols], ALU.mult)
            c.stt(X[:, t, cols], X[:, t, cols], ALPHA, b_[:], ALU.mult, ALU.add)
        nkc = NSC // 128
        ygc = yb[nb % 3]
        c.dma(ygc[:, 0:nkc, :], yc[:, cols].rearrange("(k p) n -> p k n", p=128), q="pool")
        for kt in range(nkc):
            y_ = ygc[:, kt, :]
            s_ = sb_[si % 3]
            si += 1
            c.ts(s_[:, 0:128], tok[:, 1024:1152], ic[:, kt:kt + 1], None, ALU.is_equal)
            c.mm(acc[0][:, 0:512], s_[:, 0:128], y_, start=(kt == 0), stop=(kt == nkc - 1))
        b_ = tb[0]
        c.tt(b_[:], acc[0][:, 0:512], reps[:, 1, cols], ALU.mult)
        c.stt(X[:, 8, cols], X[:, 8, cols], ALPHA, b_[:], ALU.mult, ALU.add)
    for t in range(NT):
        ln_hat(c, cm_none, X[:, t, :], xh[:], tmp=tmp)
        c.tt(xh[:], xh[:], reps[:, 2, :], ALU.mult)
        c.tt(X[:, t, :], xh[:], reps[:, 3, :], ALU.add)
        c.dma(out[t * 128:(t + 1) * 128, :], X[:, t, :], q="sp")
    c.finish("sp")
    return c


cm_none = None


def host_combine(Y, idx, idxc, x1l, x1c, mod, P, l):
    x1s = tok_shard(x1l, x1c)
    ims = []
    for j in range(NCORES):
        b, q = j // 4, j % 4
        yl = np.ascontiguousarray(Y[:, b * 512:(b + 1) * 512, :].reshape(NSL, D))
        il = idx[b].reshape(NSL).astype(np.int32)
        tok = np.full((NTOK,), -5.0, np.float32)
        tok[:1024] = q * 1024 + np.arange(1024)
        if j < 4:
            cb = j // 2
            yc = np.ascontiguousarray(Y[:, 1024 + cb * 32:1024 + (cb + 1) * 32, :].reshape(NSC, D))
            ic = idxc[cb].reshape(NSC).astype(np.int32)
            tok[1024:] = (j % 2) * 128 + np.arange(128)
        else:
            yc = np.zeros((NSC, D), np.float32)
            ic = np.full((NSC,), -7, np.int32)
        seg = lambda r_, i: mod[r_, i * D:(i + 1) * D]
        reps = np.stack([rep(seg(b, 5)), rep(seg(2, 5)), rep(P["ln2_g"][l]), rep(P["ln2_b"][l])], 1)
        ims.append({"yl": yl, "yc": yc, "il": np.ascontiguousarray(il.reshape(-1, 128).T),
                    "ic": np.ascontiguousarray(ic.reshape(-1, 128).T), "tok": rep(tok),
                    "x1": x1s[j], "reps": np.ascontiguousarray(reps)})
    res = run(prog("combine", build_combine), ims)
    return tok_unshard([r["x2"] for r in res], D)


def kernel(**inp):
    P = {k: np.asarray(v) for k, v in inp.items()}
    x_lat = np.asarray(P["x"], np.float32)
    x_ctx = np.asarray(P["ctx"], np.float32)
    mods = host_mod(P["c"], P["c_ctx"], P["ada_w"], P["ada_b"])
    for l in range(2):
        mod = mods[l]
        pl, pc = host_proj(x_lat, x_ctx, mod, P["w_in"][l])
        s5o = host_s5(pl, pc, P, l)
        atto = host_att(pl, pc, P, l)
        ml, mc = host_merge_a(x_lat, x_ctx, mod, pl, pc, s5o, atto, P, l)
        (x1l, x1c), (h2l, h2c), (al, ac) = host_merge_b(ml, mc, x_lat, x_ctx, mod, P, l)
        gate, idx, gatec, idxc = host_topk(al, ac)
        Y = host_expert(h2l, h2c, gate, idx, gatec, idxc, P, l)
        x_lat, x_ctx = host_combine(Y, idx, idxc, x1l, x1c, mod, P, l)
    return np.ascontiguousarray(x_lat, dtype=np.float32)
```

```python
import numpy as np
import concourse.bass as bass
import concourse.mybir as mybir
from concourse.bass_utils import run_bass_kernel_spmd

F32 = mybir.dt.float32
BF16 = mybir.dt.bfloat16
I32 = mybir.dt.int32
U32 = mybir.dt.uint32
AF = mybir.ActivationFunctionType
ALU = mybir.AluOpType
AX = mybir.AxisListType

NCORES = 8


class V:
    __slots__ = ("t", "ap")

    def __init__(self, t, ap):
        self.t = t
        self.ap = ap

    def __getitem__(self, idx):
        return V(self.t, self.ap[idx])


class T:
    def __init__(self, ctx, name, shape, dtype, psum=False):
        nc = ctx.nc
        if psum:
            self.h = nc.alloc_psum_tensor(name, shape, dtype)
        else:
            self.h = nc.alloc_sbuf_tensor(name, shape, dtype)
        self.lw = None
        self.rd = {}
        self.name = name
        self.psum = psum

    def __getitem__(self, idx):
        return V(self, self.h[idx])

    def v(self, ap):
        return V(self, ap)


def _ap(x):
    if isinstance(x, V):
        return x.ap
    if isinstance(x, T):
        return x.h[:]
    return x


def _tl(x):
    if isinstance(x, V):
        return x.t
    if isinstance(x, T):
        return x
    return None


import os as _os


class Ctx:
    SAME_ENGINE_SYNC = _os.environ.get("MK_SAME_ENGINE_SYNC", "1") == "1"

    def __init__(self):
        self.nc = bass.Bass("TRN2", target_bir_lowering=False)
        nc = self.nc
        self.eng = {"pe": nc.tensor, "act": nc.scalar, "dve": nc.vector,
                    "pool": nc.gpsimd, "sp": nc.sync}
        self.sems = {}
        self.cnt = {}
        for e in ("pe", "act", "dve", "pool"):
            self.sems[e] = nc.alloc_semaphore("sem_" + e)
            self.cnt[e] = 0
        self.known = {e: {} for e in self.eng}
        self.dma_pool = {}
        self.dma_k = {}
        self.ntile = 0
        self.out_deps = []
        self.stream = {e: [] for e in self.eng}
        self.finalized = False

    def sb(self, shape, dtype, name=None):
        self.ntile += 1
        return T(self, name or f"t{self.ntile}", shape, dtype)

    def ps(self, shape, dtype=F32, name=None):
        self.ntile += 1
        return T(self, name or f"p{self.ntile}", shape, dtype, psum=True)

    def dram_in(self, name, shape, dtype=F32):
        return self.nc.dram_tensor(name, list(shape), dtype, kind="ExternalInput").ap()

    def dram_out(self, name, shape, dtype=F32):
        return self.nc.dram_tensor(name, list(shape), dtype, kind="ExternalOutput").ap()

    def _wait(self, e, deps):
        eng = self.eng[e]
        kn = self.known[e]
        best = {}
        for d in deps:
            if d is None:
                continue
            k, val = d
            if best.get(k, 0) < val:
                best[k] = val
        for k, val in best.items():
            if isinstance(k, str):
                if k == e and (e == "pe" or not self.SAME_ENGINE_SYNC):
                    continue
                sem = self.sems[k]
            else:
                sem = k
            if kn.get(k, 0) >= val:
                continue
            self.stream[e].append(("w", sem, val))
            kn[k] = val

    def _deps(self, outs, ins):
        deps = []
        for v in ins:
            t = _tl(v)
            if t is not None:
                deps.append(t.lw)
                if t.psum:
                    deps.extend(t.rd.items())
        for v in outs:
            t = _tl(v)
            if t is not None:
                deps.append(t.lw)
                deps.extend(t.rd.items())
        return deps

    def emit(self, e, outs, ins, f):
        self._wait(e, self._deps(outs, ins))
        self.cnt[e] += 1
        n = self.cnt[e]
        self.stream[e].append(("i", f, self.sems[e], 1))
        for v in ins:
            t = _tl(v)
            if t is not None:
                t.rd[e] = n
        for v in outs:
            t = _tl(v)
            if t is not None:
                t.lw = (e, n)
                t.rd = {}
        return None

    def dma(self, out, in_, q="sp", npool=24, **kw):
        self._wait(q, self._deps([out], [in_]))
        if q not in self.dma_pool:
            self.dma_pool[q] = [[self.nc.alloc_semaphore(f"dq_{q}_{i}"), 0] for i in range(npool)]
            self.dma_k[q] = 0
        pool = self.dma_pool[q]
        slot = pool[self.dma_k[q] % len(pool)]
        self.dma_k[q] += 1
        sem, val = slot
        if val > 0 and self.known[q].get(sem, 0) < val:
            self.stream[q].append(("w", sem, val))
            self.known[q][sem] = val
        slot[1] = val + 16
        o_ap, i_ap = _ap(out), _ap(in_)
        self.stream[q].append(("i", lambda g: g.dma_start(out=o_ap, in_=i_ap, **kw), sem, 16))
        if _tl(in_) is not None:
            _tl(in_).rd[sem] = val + 16
        if _tl(out) is not None:
            _tl(out).lw = (sem, val + 16)
            _tl(out).rd = {}
        else:
            self.out_deps.append((sem, val + 16))
        return None

    def finish(self, e="sp"):
        self._wait(e, self.out_deps)
        assert not self.finalized
        self.finalized = True
        streams = self.stream

        def replay(g, items):
            for it in items:
                if it[0] == "w":
                    g.wait_ge(it[1], it[2])
                else:
                    it[1](g).then_inc(it[2], it[3])

        with self.nc.Block() as block:
            @block.sync
            def _(g):
                replay(g, streams["sp"])

            @block.tensor
            def _(g):
                replay(g, streams["pe"])

            @block.vector
            def _(g):
                replay(g, streams["dve"])

            @block.scalar
            def _(g):
                replay(g, streams["act"])

            @block.gpsimd
            def _(g):
                replay(g, streams["pool"])

    def mm(self, out, lhsT, rhs, start=True, stop=True):
        return self.emit("pe", [out], [lhsT, rhs] + ([] if start else [out]),
                         lambda g: g.matmul(_ap(out), _ap(lhsT), _ap(rhs), start=start, stop=stop))

    def transpose(self, out, in_, ident):
        return self.emit("pe", [out], [in_, ident],
                         lambda g: g.transpose(_ap(out), _ap(in_), _ap(ident)))

    def act(self, out, in_, func, bias=None, scale=None, accum_out=None, e="act"):
        ins = [in_]
        kw = {}
        if bias is not None:
            kw["bias"] = _ap(bias)
            ins.append(bias)
        if scale is not None:
            kw["scale"] = _ap(scale)
            ins.append(scale)
        outs = [out]
        if accum_out is not None:
            kw["accum_out"] = _ap(accum_out)
            outs.append(accum_out)
        return self.emit(e, outs, ins, lambda g: g.activation(_ap(out), _ap(in_), func, **kw))

    def copy(self, out, in_, e="dve"):
        if e == "act":
            return self.emit(e, [out], [in_], lambda g: g.copy(_ap(out), _ap(in_)))
        return self.emit(e, [out], [in_], lambda g: g.tensor_copy(_ap(out), _ap(in_)))

    def tt(self, out, a, b, op, e="dve"):
        return self.emit(e, [out], [a, b], lambda g: g.tensor_tensor(_ap(out), _ap(a), _ap(b), op))

    def ts(self, out, a, s1, s2, op0, op1=None, e="dve", accum_out=None):
        ins = [a] + [s for s in (s1, s2) if isinstance(s, V)]
        outs = [out] + ([accum_out] if accum_out is not None else [])
        kw = {}
        if accum_out is not None:
            kw["accum_out"] = _ap(accum_out)
        if op1 is None:
            return self.emit(e, outs, ins, lambda g: g.tensor_scalar(_ap(out), _ap(a), _ap(s1), None, op0, **kw))
        return self.emit(e, outs, ins, lambda g: g.tensor_scalar(_ap(out), _ap(a), _ap(s1), _ap(s2), op0, op1, **kw))

    def tss(self, out, a, s, op, e="pool"):
        return self.emit(e, [out], [a], lambda g: g.tensor_single_scalar(_ap(out), _ap(a), s, op))

    def stt(self, out, a, s, b, op0, op1, e="dve"):
        ins = [a, b] + ([s] if isinstance(s, V) else [])
        return self.emit(e, [out], ins,
                         lambda g: g.scalar_tensor_tensor(_ap(out), _ap(a), _ap(s), _ap(b), op0, op1))

    def memset(self, out, val, e="dve"):
        return self.emit(e, [out], [], lambda g: g.memset(_ap(out), val))

    def reduce(self, out, in_, op, axis=None, e="dve"):
        axis = axis or AX.X
        return self.emit(e, [out], [in_], lambda g: g.tensor_reduce(_ap(out), _ap(in_), axis, op))


def run(ctx, in_maps):
    if _os.environ.get("MK_TRACE", "0") == "1":
        res = run_bass_kernel_spmd(ctx.nc, in_maps, core_ids=list(range(NCORES)), trace=True)
        print("MK_TRACE exec_time_ns", res.exec_time_ns, flush=True)
        return res.results
    res = run_bass_kernel_spmd(ctx.nc, in_maps, core_ids=list(range(NCORES)))
    return res.results


D = 2048
KT = 16
NT = 9
NTOK = NT * 128
EPS = 1e-6


class Common:
    def __init__(self, c):
        self.c = c
        self.ident_d = c.dram_in("ident", [128, 128])
        self.ident = c.sb([128, 128], F32, "ident_sb")
        c.dma(self.ident[:], self.ident_d[:, :])
        self.eps = c.sb([128, 1], F32, "eps_sb")
        c.memset(self.eps[:], EPS)


def ln_hat(c, cm, xt, xh, width=D, tmp=None):
    nch = (width + 511) // 512
    st = tmp["st"]
    mv = tmp["mv"]
    rs = tmp["rs"]
    sq = tmp["sq"]
    for i in range(nch):
        lo, hi = i * 512, min(width, (i + 1) * 512)
        c.emit("dve", [st], [xt], lambda g, i=i, lo=lo, hi=hi: g.bn_stats(st.h[:, i * 6:(i + 1) * 6], xt.ap[:, lo:hi]))
    c.emit("dve", [mv], [st], lambda g: g.bn_aggr(mv.h[:], st.h[:, 0:nch * 6]))
    c.ts(sq[:], mv[:, 1:2], EPS, None, ALU.add)
    c.act(sq[:], sq[:], AF.Sqrt)
    c.emit("dve", [rs], [sq], lambda g: g.reciprocal(rs.h[:], sq.h[:]))
    c.ts(xh, xt, mv[:, 0:1], rs[:], ALU.subtract, ALU.mult)


def ln_tmp(c, tag=""):
    return {"st": c.sb([128, 24], F32, "ln_st" + tag), "mv": c.sb([128, 2], F32, "ln_mv" + tag),
            "rs": c.sb([128, 1], F32, "ln_rs" + tag), "sq": c.sb([128, 1], F32, "ln_sq" + tag)}


def to_fm(c, cm, xh, hT, tile, pst, scale_cols=None, bias_cols=None, nk=KT):
    for k in range(nk):
        p = pst[k % len(pst)]
        c.transpose(p[:, 0:128], xh[:, k * 128:(k + 1) * 128], cm.ident[:])
        dst = hT[:, k, tile * 128:(tile + 1) * 128]
        if scale_cols is not None:
            c.act(dst, p[:, 0:128], AF.Identity, bias=bias_cols[:, k:k + 1], scale=scale_cols[:, k:k + 1])
        else:
            c.copy(dst, p[:, 0:128], e="act")


def stream_linear(c, hT, w_dram, n_total, ntiles, consume, wbufs, pso, kt=KT, nbw=512, tile_list=None):
    wv = w_dram.rearrange("(k p) n -> p k n", p=128)
    nblk = (n_total + nbw - 1) // nbw
    tiles = tile_list if tile_list is not None else list(range(ntiles))
    i = 0
    for nb in range(nblk):
        lo = nb * nbw
        bw = min(nbw, n_total - lo)
        wb = wbufs[nb % len(wbufs)]
        c.dma(wb[:, 0:kt, 0:bw], wv[:, :, lo:lo + bw], q="pool")
        for t in tiles:
            p = pso[i % len(pso)]
            i += 1
            for k in range(kt):
                c.mm(p[:, 0:bw], hT[:, k, t * 128:(t + 1) * 128], wb[:, k, 0:bw],
                     start=(k == 0), stop=(k == kt - 1))
            consume(t, nb, lo, bw, p)


MODC = 2 * 12288 // NCORES


def build_mod():
    c = Ctx()
    cT = c.dram_in("cT", [128, KT, 3])
    w = c.dram_in("w", [D, MODC])
    b = c.dram_in("b", [3, MODC])
    out = c.dram_out("mod", [3, MODC])
    ct = c.sb([128, KT, 3], F32)
    sg = c.sb([128, KT, 3], F32)
    bt = c.sb([3, MODC], F32)
    ot = c.sb([3, MODC], F32)
    c.dma(ct[:], cT[:, :, :])
    c.dma(bt[:], b[:, :])
    c.act(sg[:], ct[:], AF.Sigmoid)
    c.tt(ct[:], ct[:], sg[:], ALU.mult)
    wb = [c.sb([128, KT, 512], F32, f"modw{i}") for i in range(2)]
    ps = [c.ps([128, 512]) for _ in range(2)]
    wv = w.rearrange("(k p) n -> p k n", p=128)
    for nb in range(MODC // 512):
        wt = wb[nb % 2]
        c.dma(wt[:], wv[:, :, nb * 512:(nb + 1) * 512])
        p = ps[nb % 2]
        for k in range(KT):
            c.mm(p[0:3, :], ct[:, k, :], wt[:, k, :], start=(k == 0), stop=(k == KT - 1))
        c.tt(ot[:, nb * 512:(nb + 1) * 512], p[0:3, :], bt[:, nb * 512:(nb + 1) * 512], ALU.add)
    c.dma(out[:, :], ot[:], q="pool")
    c.finish("pool")
    return c


IN_TOTAL = 3904


def load_mod_cols(c, mv_d, n):
    t = c.sb([128, n, KT], F32, "modcols")
    c.dma(t[:], mv_d[:, :, :])
    return t


def build_proj():
    c = Ctx()
    cm = Common(c)
    x = c.dram_in("x", [NTOK, D])
    mv_d = c.dram_in("mv", [128, 4, KT])
    w = c.dram_in("w_in", [D, IN_TOTAL])
    out = c.dram_out("proj", [NTOK, IN_TOTAL])
    mv = load_mod_cols(c, mv_d, 4)
    c.ts(mv[:, 1, :], mv[:, 1, :], 1.0, None, ALU.add)
    c.ts(mv[:, 3, :], mv[:, 3, :], 1.0, None, ALU.add)
    hT = c.sb([128, KT, NTOK], BF16, "hT")
    xts = [c.sb([128, D], F32, f"xt{i}") for i in range(2)]
    xh = c.sb([128, D], F32, "xh")
    tmp = ln_tmp(c)
    pst = [c.ps([128, 512]) for _ in range(2)]
    for t in range(NT):
        xt = xts[t % 2]
        c.dma(xt[:], x[t * 128:(t + 1) * 128, :])
        ln_hat(c, cm, xt[:], xh[:], tmp=tmp)
        j = 0 if t < 8 else 2
        to_fm(c, cm, xh, hT, t, pst, scale_cols=mv[:, j + 1, :], bias_cols=mv[:, j, :])
    wbufs = [c.sb([128, KT, 512], BF16, f"wb{i}") for i in range(2)]
    pso = [c.ps([128, 512]) for _ in range(3)]
    obufs = [c.sb([128, 512], F32, f"ob{i}") for i in range(3)]
    cnt = [0]

    def consume(t, nb, lo, bw, p):
        ob = obufs[cnt[0] % 3]
        cnt[0] += 1
        c.copy(ob[:, 0:bw], p[:, 0:bw], e=("dve" if cnt[0] % 2 else "act"))
        c.dma(out[t * 128:(t + 1) * 128, lo:lo + bw], ob[:, 0:bw], q="sp")

    stream_linear(c, hT, w, IN_TOTAL, NT, consume, wbufs, pso)
    c.finish("sp")
    return c


_PROGS = {}
IDENT = np.eye(128, dtype=np.float32)


def prog(name, builder):
    if name not in _PROGS:
        _PROGS[name] = builder()
    return _PROGS[name]


def fm_cols(vec):
    return np.ascontiguousarray(vec.reshape(-1, 128).T)


def host_mod(cv, c_ctx, ada_w, ada_b):
    cvec = np.concatenate([cv, c_ctx[None, :]], 0)
    cT = np.ascontiguousarray(cvec.reshape(3, KT, 128).transpose(2, 1, 0))
    ims = []
    per = 12288 // NCORES
    for j in range(NCORES):
        w = np.concatenate([ada_w[l][:, j * per:(j + 1) * per] for l in range(2)], 1)
        b = np.concatenate([np.broadcast_to(ada_b[l][None, j * per:(j + 1) * per], (3, per)) for l in range(2)], 1)
        ims.append({"cT": cT, "w": np.ascontiguousarray(w), "b": np.ascontiguousarray(b)})
    res = run(prog("mod", build_mod), ims)
    mods = []
    for l in range(2):
        mods.append(np.concatenate([res[j]["mod"][:, l * per:(l + 1) * per] for j in range(NCORES)], 1))
    return mods


def tok_shard(x_lat, x_ctx):
    outs = []
    for j in range(NCORES):
        b, q = j // 4, j % 4
        a = np.zeros((NTOK, x_lat.shape[-1]), np.float32)
        a[:1024] = x_lat[b, q * 1024:(q + 1) * 1024]
        if j < 4:
            a[1024:] = x_ctx[j // 2, (j % 2) * 128:(j % 2 + 1) * 128]
        outs.append(a)
    return outs


def tok_unshard(parts, width):
    lat = np.zeros((2, 4096, width), np.float32)
    ctx = np.zeros((2, 256, width), np.float32)
    for j in range(NCORES):
        b, q = j // 4, j % 4
        lat[b, q * 1024:(q + 1) * 1024] = parts[j][:1024]
        if j < 4:
            ctx[j // 2, (j % 2) * 128:(j % 2 + 1) * 128] = parts[j][1024:]
    return lat, ctx


def mod_cols(mod, j, idxs):
    b = j // 4
    cols = []
    for i in idxs:
        cols.append(fm_cols(mod[b, i * D:(i + 1) * D]))
    for i in idxs:
        cols.append(fm_cols(mod[2, i * D:(i + 1) * D]))
    return np.ascontiguousarray(np.stack(cols, 1))


def host_proj(x_lat, x_ctx, mod, w_in):
    xs = tok_shard(x_lat, x_ctx)
    ims = []
    for j in range(NCORES):
        ims.append({"ident": IDENT, "x": xs[j], "mv": mod_cols(mod, j, [0, 1]), "w_in": w_in})
    res = run(prog("proj", build_proj), ims)
    return tok_unshard([r["proj"] for r in res], IN_TOTAL)


NLAT = 4096
NALL = 4352
NTA = 34
NTL = 32


def ms_rstd(c, x, w, tmp, eps=EPS):
    st, mv, rs, sq = tmp["st"], tmp["mv"], tmp["rs"], tmp["sq"]
    c.emit("dve", [st], [x], lambda g: g.bn_stats(st.h[:, 0:6], _ap(x)))
    c.emit("dve", [mv], [st], lambda g: g.bn_aggr(mv.h[:], st.h[:, 0:6]))
    c.stt(sq[:], mv[:, 0:1], mv[:, 0:1], mv[:, 1:2], ALU.mult, ALU.add)
    c.ts(sq[:], sq[:], eps, None, ALU.add)
    c.act(sq[:], sq[:], AF.Sqrt)
    c.emit("dve", [rs], [sq], lambda g: g.reciprocal(rs.h[:], sq.h[:]))


def rope_tm(c, x, out, cos, sin, n, t1, t2, e="pool"):
    xv = V(_tl(x), _ap(x).rearrange("p (h t n) -> p h t n", h=2, t=2))
    ov = V(_tl(out), _ap(out).rearrange("p (h t n) -> p h t n", h=2, t=2))
    cv = V(_tl(cos), _ap(cos).rearrange("p (h n) -> p h n", h=2))
    sv = V(_tl(sin), _ap(sin).rearrange("p (h n) -> p h n", h=2))
    a = V(t1, t1.h[:, 0:2 * n].rearrange("p (h n) -> p h n", h=2))
    b = V(t2, t2.h[:, 0:2 * n].rearrange("p (h n) -> p h n", h=2))
    x1, x2 = xv[:, :, 0, :], xv[:, :, 1, :]
    c.tt(a, x1, cv, ALU.mult, e=e)
    c.tt(b, x2, sv, ALU.mult, e=e)
    c.tt(ov[:, :, 0, :], a, b, ALU.subtract, e=e)
    c.tt(a, x1, sv, ALU.mult, e=e)
    c.tt(b, x2, cv, ALU.mult, e=e)
    c.tt(ov[:, :, 1, :], a, b, ALU.add, e=e)


def attention(c, cm, qTs, kTs, vt, q_tiles, k_lo, k_hi, scale, bufs, out_cb):
    PT, pss, pst, pso = (bufs[k] for k in ("PT", "pss", "pst", "pso"))
    identb = bufs["identb"]
    nk = k_hi - k_lo
    nch = (nk + 511) // 512
    nkt = nk // 128
    dv1 = vt.h.shape[-1]
    dv = dv1 - 1
    ng = (nkt + 3) // 4

    def qk_phase(qt):
        q0 = qt * 128
        bi = bufs["ctr"][0] % 2
        bufs["ctr"][0] += 1
        S, Pb, cmax, mx, rsum = (bufs[k][bi] for k in ("S", "Pb", "cmax", "mx", "rsum"))
        for ch in range(nch):
            lo = k_lo + ch * 512
            w = min(512, k_hi - lo)
            p = pss[ch % len(pss)]
            for i, ((qT, r), (kT, _)) in enumerate(zip(qTs, kTs)):
                c.mm(p[:, 0:w], qT[0:r, q0:q0 + 128], kT[0:r, lo:lo + w], start=(i == 0), stop=(i == len(qTs) - 1))
            c.reduce(cmax[:, ch:ch + 1], p[:, 0:w], ALU.max)
            c.copy(S[:, ch * 512:ch * 512 + w], p[:, 0:w], e="act")
        c.reduce(mx[:], cmax[:, 0:nch], ALU.max)
        c.ts(mx[:], mx[:], -scale, None, ALU.mult)
        c.act(Pb[:, 0:nk], S[:, 0:nk], AF.Exp, bias=mx[:], scale=scale)
        return (qt, bi)

    def pv_phase(st):
        qt, bi = st
        Pb, rsum = bufs["Pb"][bi], bufs["rsum"][bi]
        po = pso[bi]

        def tr(g4):
            pt = pst[g4 % len(pst)]
            n4 = min(4, nkt - g4 * 4)
            for j in range(n4):
                kt = g4 * 4 + j
                c.transpose(pt[:, j * 128:(j + 1) * 128], Pb[:, kt * 128:(kt + 1) * 128], identb[:])

        tr(0)
        for g4 in range(ng):
            if g4 + 1 < ng:
                tr(g4 + 1)
            pt = pst[g4 % len(pst)]
            n4 = min(4, nkt - g4 * 4)
            ptb = PT[g4 % len(PT)]
            c.copy(ptb[:, 0:n4 * 128], pt[:, 0:n4 * 128], e="dve")
            for j in range(n4):
                kt = g4 * 4 + j
                c.mm(po[:, 0:dv1], ptb[:, j * 128:(j + 1) * 128], vt[:, k_lo // 128 + kt, :],
                     start=(kt == 0), stop=(kt == nkt - 1))
        c.emit("dve", [rsum], [po], lambda g, rsum=rsum, po=po, dv=dv: g.reciprocal(rsum.h[:], po.h[:, dv:dv + 1]))
        out_cb(qt, po, rsum)

    prev = None
    for qt in q_tiles:
        st = qk_phase(qt)
        if prev is not None:
            pv_phase(prev)
        prev = st
    if prev is not None:
        pv_phase(prev)


def build_att():
    c = Ctx()
    cm = Common(c)
    din = c.dram_in
    gq, gk, gv = din("gq", [NALL, 128]), din("gk", [NALL, 128]), din("gv", [NALL, 128])
    gqn, gkn = din("gqn", [128, 128]), din("gkn", [128, 128])
    mcq, mckv, mkr = din("mcq", [NALL, 512]), din("mckv", [NALL, 256]), din("mkr", [NALL, 64])
    mqn, mkvn = din("mqn", [128, 512]), din("mkvn", [128, 256])
    wuq, wukv = din("wuq", [512, 192]), din("wukv", [256, 256])
    rq, rk, rv, rg = din("rq", [NALL, 64]), din("rk", [NALL, 64]), din("rv", [NALL, 128]), din("rg", [NALL, 128])
    rpar, rgain = din("rpar", [128, 2]), din("rgain", [128, 128])
    cos128, sin128 = din("cos128", [NLAT, 64]), din("sin128", [NLAT, 64])
    cos64, sin64 = din("cos64", [NLAT, 32]), din("sin64", [NLAT, 32])
    rconst = din("rconst", [128, 6, 128])
    rcol = din("rcol", [128, 2])
    o_gqa, o_mla, o_ret = c.dram_out("o_gqa", [NALL, 128]), c.dram_out("o_mla", [NALL, 128]), c.dram_out("o_ret", [NALL, 128])

    QT = c.sb([128, NALL], BF16, "QT")
    KTb = c.sb([128, NALL], BF16, "KT")
    VT = c.sb([128, NTA, 129], BF16, "VT")
    c.memset(VT[:, :, 128:129], 1.0)
    QR = c.sb([64, NALL], BF16, "QR")
    KR = c.sb([64, NALL], BF16, "KR")
    identb = c.sb([128, 128], BF16, "identb")
    c.copy(identb[:], cm.ident[:])
    bufs = {"S": [c.sb([128, NALL], F32, f"S{i}") for i in range(2)], "PT": [c.sb([128, 512], BF16, f"PT{i}") for i in range(2)],
            "Pb": [c.sb([128, NALL], BF16, f"Pb{i}") for i in range(2)], "identb": identb, "ctr": [0],
            "pss": [c.ps([128, 512]) for _ in range(2)], "pst": [c.ps([128, 512], BF16) for _ in range(2)],
            "pso": [c.ps([128, 512]) for _ in range(2)],
            "cmax": [c.sb([128, 16], F32, f"cmax{i}") for i in range(2)], "mx": [c.sb([128, 1], F32, f"mx{i}") for i in range(2)],
            "rsum": [c.sb([128, 1], F32, f"rsum{i}") for i in range(2)]}
    psx = [c.ps([128, 512]) for _ in range(2)]
    xin = [c.sb([128, 512], F32, f"xin{i}") for i in range(6)]
    WS = [(ln_tmp(c, f"_w{i}"), c.sb([128, 512], F32, f"xa{i}"), c.sb([128, 512], F32, f"xb{i}"),
           c.sb([128, 128], F32, f"ropet1_{i}"), c.sb([128, 128], F32, f"ropet2_{i}")) for i in range(2)]
    wsi = [0]

    def rot():
        wsi[0] += 1
        return WS[wsi[0] % 2]

    tmp, xa, xb, t1, t2 = WS[0]
    cs = [c.sb([128, 2, 64], F32, f"cs{i}") for i in range(2)]
    ob = [c.sb([128, 128], F32, f"ob{i}") for i in range(3)]
    gains = c.sb([128, 128 + 128 + 512 + 256 + 128], F32, "gains")
    G_Q, G_K, G_MQ, G_MKV, G_R = 0, 128, 256, 768, 1024
    c.dma(gains[:, G_Q:G_Q + 128], gqn[:, :])
    c.dma(gains[:, G_K:G_K + 128], gkn[:, :])
    c.dma(gains[:, G_MQ:G_MQ + 512], mqn[:, :])
    c.dma(gains[:, G_MKV:G_MKV + 256], mkvn[:, :])
    c.dma(gains[:, G_R:G_R + 128], rgain[:, :])
    cnt = [0]

    def outer(dst):
        def cb(qt, po, rsum):
            o = ob[cnt[0] % 3]
            cnt[0] += 1
            c.ts(o[:], po[:, 0:128], rsum[:], None, ALU.mult)
            c.dma(dst[qt * 128:(qt + 1) * 128, :], o[:], q="pool")
        return cb

    def load_cs(t, cos_d, sin_d, n2, k):
        t_ = cs[k % 2]
        c.dma(t_[:, 0, 0:n2], cos_d[t * 128:(t + 1) * 128, :])
        c.dma(t_[:, 1, 0:n2], sin_d[t * 128:(t + 1) * 128, :])
        return t_

    def tr_to(dst, src, rows, t):
        p = psx[t % 2]
        c.transpose(p[0:rows, 0:128], src, cm.ident[:])
        c.copy(dst[0:rows, t * 128:(t + 1) * 128], p[0:rows, 0:128], e="act")

    import os
    SECT = os.environ.get("ATT_SECT", "gmr")
    for t in range(NTA if "g" in SECT else 0):
        xq, xk = xin[3 * (t % 2)], xin[3 * (t % 2) + 1]
        c.dma(xq[:, 0:128], gq[t * 128:(t + 1) * 128, :])
        c.dma(xk[:, 0:128], gk[t * 128:(t + 1) * 128, :])
        c.dma(VT[:, t, 0:128], gv[t * 128:(t + 1) * 128, :], q="pool")
        for (x, g0, dstT) in ((xq, G_Q, QT), (xk, G_K, KTb)):
            tmp, xa, xb, t1, t2 = rot()
            ms_rstd(c, x[:, 0:128], 128, tmp)
            c.stt(xa[:, 0:128], x[:, 0:128], tmp["rs"][:], gains[:, g0:g0 + 128], ALU.mult, ALU.mult)
            if t < NTL:
                if g0 == G_Q:
                    cst = load_cs(t, cos128, sin128, 64, t)
                rope_tm(c, xa[:, 0:128], xb[:, 0:128], cst[:, 0, 0:64], cst[:, 1, 0:64], 32, t1, t2)
                tr_to(dstT, xb[:, 0:128], 128, t)
            else:
                tr_to(dstT, xa[:, 0:128], 128, t)
    if "g" in SECT:
        nq = int(os.environ.get("GQA_NQ", NTL))
        attention(c, cm, [(QT, 128)], [(KTb, 128)], VT, list(range(nq)), 0, NALL, 128 ** -0.5, bufs, outer(o_gqa))
        if nq == NTL:
            attention(c, cm, [(QT, 128)], [(KTb, 128)], VT, [32, 33], NLAT, NALL, 128 ** -0.5, bufs, outer(o_gqa))

    wq = c.sb([128, 4, 192], F32, "wq")
    wkv = c.sb([128, 2, 256], F32, "wkv")
    c.dma(wq[:], wuq.rearrange("(k p) n -> p k n", p=128))
    c.dma(wkv[:], wukv.rearrange("(k p) n -> p k n", p=128))
    cT = c.sb([128, 4, 128], F32, "cT")
    for t in range(NTA if "m" in SECT else 0):
        xq, xkv, xr = xin[3 * (t % 2)], xin[3 * (t % 2) + 1], xin[3 * (t % 2) + 2]
        c.dma(xq[:, 0:512], mcq[t * 128:(t + 1) * 128, :])
        c.dma(xkv[:, 0:256], mckv[t * 128:(t + 1) * 128, :])
        c.dma(xr[:, 0:64], mkr[t * 128:(t + 1) * 128, :])
        if t < NTL:
            cst = load_cs(t, cos64, sin64, 32, t)
        tmp, xa, xb, t1, t2 = rot()
        ms_rstd(c, xq[:, 0:512], 512, tmp)
        c.stt(xa[:, 0:512], xq[:, 0:512], tmp["rs"][:], gains[:, G_MQ:G_MQ + 512], ALU.mult, ALU.mult)
        for k in range(4):
            p = psx[k % 2]
            c.transpose(p[:, 0:128], xa[:, k * 128:(k + 1) * 128], cm.ident[:])
            c.copy(cT[:, k, :], p[:, 0:128], e="act")
        p = psx[0]
        for k in range(4):
            c.mm(p[:, 0:128], wq[:, k, 0:128], cT[:, k, :], start=(k == 0), stop=(k == 3))
        c.copy(QT[:, t * 128:(t + 1) * 128], p[:, 0:128], e="act")
        p = psx[1]
        for k in range(4):
            c.mm(p[:, 0:64], cT[:, k, :], wq[:, k, 128:192], start=(k == 0), stop=(k == 3))
        c.copy(xb[:, 0:64], p[:, 0:64])
        if t < NTL:
            rope_tm(c, xb[:, 0:64], xb[:, 64:128], cst[:, 0, 0:32], cst[:, 1, 0:32], 16, t1, t2)
            tr_to(QR, xb[:, 64:128], 64, t)
        else:
            tr_to(QR, xb[:, 0:64], 64, t)
        tmp, xa, xb, t1, t2 = rot()
        ms_rstd(c, xkv[:, 0:256], 256, tmp)
        c.stt(xa[:, 0:256], xkv[:, 0:256], tmp["rs"][:], gains[:, G_MKV:G_MKV + 256], ALU.mult, ALU.mult)
        for k in range(2):
            p = psx[k % 2]
            c.transpose(p[:, 0:128], xa[:, k * 128:(k + 1) * 128], cm.ident[:])
            c.copy(cT[:, k, :], p[:, 0:128], e="act")
        p = psx[0]
        for k in range(2):
            c.mm(p[:, 0:128], wkv[:, k, 0:128], cT[:, k, :], start=(k == 0), stop=(k == 1))
        c.copy(KTb[:, t * 128:(t + 1) * 128], p[:, 0:128], e="act")
        p = psx[1]
        for k in range(2):
            c.mm(p[:, 0:128], cT[:, k, :], wkv[:, k, 128:256], start=(k == 0), stop=(k == 1))
        c.copy(VT[:, t, 0:128], p[:, 0:128])
        if t < NTL:
            rope_tm(c, xr[:, 0:64], xb[:, 128:192], cst[:, 0, 0:32], cst[:, 1, 0:32], 16, t1, t2)
            tr_to(KR, xb[:, 128:192], 64, t)
        else:
            tr_to(KR, xr[:, 0:64], 64, t)
    sc = 192 ** -0.5
    if "m" in SECT:
        attention(c, cm, [(QT, 128), (QR, 64)], [(KTb, 128), (KR, 64)], VT, list(range(NTL)), 0, NALL, sc, bufs, outer(o_mla))
        attention(c, cm, [(QT, 128), (QR, 64)], [(KTb, 128), (KR, 64)], VT, [32, 33], NLAT, NALL, sc, bufs, outer(o_mla))
    if "r" not in SECT:
        c.finish("pool")
        return c

    RQ, RK = bufs["S"][0], bufs["S"][1]
    RV = c.sb([128, NTA, 128], F32, "RV")
    rc = c.sb([128, 6, 128], F32, "rc")
    rcl = c.sb([128, 2], F32, "rcl")
    rp = c.sb([128, 2], F32, "rp")
    c.dma(rc[:], rconst[:, :, :])
    c.dma(rcl[:], rcol[:, :])
    c.dma(rp[:], rpar[:, :])
    lg = c.sb([128, 2], F32, "lg")
    c.act(lg[:], rp[:], AF.Exp)
    c.ts(lg[:], lg[:], -1.0, None, ALU.mult)
    DT = c.sb([128, 128], F32, "DT")
    dtmp = c.sb([128, 128], F32, "dtmp")
    c.act(DT[:], rc[:, 0, :], AF.Exp, scale=lg[:, 0:1])
    c.tt(DT[:], DT[:], rc[:, 2, :], ALU.mult)
    c.act(dtmp[:], rc[:, 1, :], AF.Exp, scale=lg[:, 1:2])
    c.tt(dtmp[:], dtmp[:], rc[:, 3, :], ALU.mult)
    c.tt(DT[:], DT[:], dtmp[:], ALU.add)
    dq = c.sb([128, 2, 128], F32, "dq")
    c.act(dq[:, 0, :], rc[:, 4, :], AF.Exp, scale=lg[:, 0:1])
    c.act(dq[:, 1, :], rc[:, 5, :], AF.Exp, scale=lg[:, 1:2])
    dk = c.sb([128, 2], F32, "dk")
    c.act(dk[:, 0:1], rcl[:, 0:1], AF.Exp, scale=lg[:, 0:1])
    c.act(dk[:, 1:2], rcl[:, 1:2], AF.Exp, scale=lg[:, 1:2])
    gcd = c.sb([128, 2], F32, "gcd")
    c.ts(gcd[:], lg[:], 128.0, None, ALU.mult)
    c.act(gcd[:], gcd[:], AF.Exp)
    Ktm = c.sb([128, NTA, 64], F32, "Ktm")
    Sf = c.sb([64, NTA + 1, 128], F32, "Sf")
    Sb = c.sb([64, NTA + 1, 128], F32, "Sb")
    kd = c.sb([128, 2, 64], F32, "kd")
    fwd_order = [32, 33] + list(range(NTL))
    for t in range(NTA):
        xq, xk = xin[3 * (t % 2)], xin[3 * (t % 2) + 1]
        c.dma(xq[:, 0:64], rq[t * 128:(t + 1) * 128, :])
        c.dma(xk[:, 0:64], rk[t * 128:(t + 1) * 128, :])
        c.dma(RV[:, t, :], rv[t * 128:(t + 1) * 128, :])
        tmp, xa, xb, t1, t2 = rot()
        c.ts(xk[:, 0:64], xk[:, 0:64], 0.125, None, ALU.mult)
        if t < NTL:
            cst = load_cs(t, cos64, sin64, 32, t)
            rope_tm(c, xq[:, 0:64], xb[:, 0:64], cst[:, 0, 0:32], cst[:, 1, 0:32], 16, t1, t2)
            rope_tm(c, xk[:, 0:64], Ktm[:, t, :], cst[:, 0, 0:32], cst[:, 1, 0:32], 16, t1, t2)
            tr_to(RQ, xb[:, 0:64], 64, t)
        else:
            c.copy(Ktm[:, t, :], xk[:, 0:64])
            tr_to(RQ, xq[:, 0:64], 64, t)
        tr_to(RK, Ktm[:, t, :], 64, t)
    bwd_order = [33, 32] + list(range(NTL - 1, -1, -1))
    kds = [c.sb([128, 64], F32, f"kds{i}") for i in range(2)]
    for (S_, order, col) in ((Sf, fwd_order, 0), (Sb, bwd_order, 1)):
        c.memset(S_[:, 0, :], 0.0)

        def kv(i, order=order, col=col):
            c.ts(kds[i % 2][:], Ktm[:, order[i], :], dk[:, col:col + 1], None, ALU.mult)
            c.mm(psx[i % 2][0:64, 0:128], kds[i % 2][:], RV[:, order[i], :])

        kv(0)
        for i, t in enumerate(order):
            if i + 1 < len(order):
                kv(i + 1)
            c.stt(S_[:, i + 1, :], S_[:, i, :], gcd[0:64, col:col + 1], psx[i % 2][0:64, 0:128], ALU.mult, ALU.add)
    fpos = {t: i for i, t in enumerate(fwd_order)}
    bpos = {t: i for i, t in enumerate(bwd_order)}
    MT = [c.sb([128, 128], F32, f"MT{i}") for i in range(2)]
    qd = [c.sb([64, 2, 128], F32, f"qd{i}") for i in range(2)]
    gt = [c.sb([128, 128], F32, f"gt{i}") for i in range(2)]
    for t in range(NTA):
        sl = slice(t * 128, (t + 1) * 128)
        tmp, xa, xb, t1, t2 = rot()
        p = psx[0]
        c.mm(p[:, 0:128], RK[0:64, sl], RQ[0:64, sl])
        m = MT[t % 2]
        c.tt(m[:], p[:, 0:128], DT[:], ALU.mult)
        q_ = qd[t % 2]
        c.tt(q_[:, 0, :], RQ[0:64, sl], dq[0:64, 0, :], ALU.mult)
        c.tt(q_[:, 1, :], RQ[0:64, sl], dq[0:64, 1, :], ALU.mult)
        po = psx[1]
        c.mm(po[:, 0:128], m[:], RV[:, t, :], start=True, stop=False)
        c.mm(po[:, 0:128], q_[:, 0, :], Sf[:, fpos[t], :], start=False, stop=False)
        c.mm(po[:, 0:128], q_[:, 1, :], Sb[:, bpos[t], :], start=False, stop=True)
        g_ = gt[t % 2]
        c.dma(g_[:], rg[sl, :])
        o = ob[cnt[0] % 3]
        cnt[0] += 1
        c.copy(xa[:, 0:128], po[:, 0:128])
        ln_hat(c, cm, xa[:, 0:128], xb[:, 0:128], width=128, tmp=tmp)
        c.tt(xb[:, 0:128], xb[:, 0:128], gains[:, G_R:G_R + 128], ALU.mult)
        c.act(xa[:, 128:256], g_[:], AF.Sigmoid)
        c.tt(xa[:, 128:256], xa[:, 128:256], g_[:], ALU.mult)
        c.tt(o[:], xb[:, 0:128], xa[:, 128:256], ALU.mult)
        c.dma(o_ret[sl, :], o[:], q="pool")
    c.finish("pool")
    return c


def rope_tables(dim):
    n = dim // 4
    inv = (np.float32(10000.0) ** (-np.arange(n, dtype=np.float32) / np.float32(n))).astype(np.float32)
    t = np.arange(NLAT)
    row = (t // 64).astype(np.float32)
    col = (t % 64).astype(np.float32)
    ang = np.concatenate([row[:, None] * inv[None, :], col[:, None] * inv[None, :]], 1).astype(np.float32)
    return np.cos(ang).astype(np.float32), np.sin(ang).astype(np.float32)


def ret_consts():
    j = np.arange(128, dtype=np.float32)[:, None]
    t = np.arange(128, dtype=np.float32)[None, :]
    z = np.zeros((128, 128), np.float32)
    rc = np.stack([np.maximum(t - j, 0), np.maximum(j - t, 0), (t >= j).astype(np.float32),
                   (j > t).astype(np.float32), t + 1 + z, 128 - t + z], 1).astype(np.float32)
    rcol = np.stack([127 - j[:, 0], j[:, 0]], 1).astype(np.float32)
    return np.ascontiguousarray(rc), np.ascontiguousarray(rcol)


def rep(v, n=128):
    return np.ascontiguousarray(np.broadcast_to(np.asarray(v, np.float32).reshape(1, -1), (n, np.size(v))))


def host_att(pl, pc, P, l):
    c128, s128 = rope_tables(128)
    c64, s64 = rope_tables(64)
    rc, rcol = ret_consts()
    ims = []
    for j in range(NCORES):
        b, h = j // 4, j % 4
        pa = np.concatenate([pl[b], pc[b]], 0)
        kv = h // 2
        cut = lambda o, w: np.ascontiguousarray(pa[:, o:o + w])
        ims.append({
            "ident": IDENT,
            "gq": cut(512 + h * 128, 128), "gk": cut(1024 + kv * 128, 128), "gv": cut(1280 + kv * 128, 128),
            "gqn": rep(P["gqa_q_norm"][l]), "gkn": rep(P["gqa_k_norm"][l]),
            "mcq": cut(3072, 512), "mckv": cut(3584, 256), "mkr": cut(3840, 64),
            "mqn": rep(P["mla_q_norm"][l]), "mkvn": rep(P["mla_kv_norm"][l]),
            "wuq": np.ascontiguousarray(P["mla_w_uq"][l][:, h * 192:(h + 1) * 192]),
            "wukv": np.ascontiguousarray(P["mla_w_ukv"][l][:, h * 256:(h + 1) * 256]),
            "rq": cut(1536 + h * 64, 64), "rk": cut(1792 + h * 64, 64),
            "rv": cut(2048 + h * 128, 128), "rg": cut(2560 + h * 128, 128),
            "rpar": rep([P["ret_decay_f"][l][h], P["ret_decay_b"][l][h]]),
            "rgain": rep(P["ret_norm"][l][h * 128:(h + 1) * 128]),
            "cos128": c128, "sin128": s128, "cos64": c64, "sin64": s64, "rconst": rc, "rcol": rcol,
        })
    res = run(prog("att", build_att), ims)
    outs = {}
    for nm in ("o_gqa", "o_ret", "o_mla"):
        lat = np.zeros((2, NLAT, 512), np.float32)
        ctx = np.zeros((2, 256, 512), np.float32)
        for j in range(NCORES):
            b, h = j // 4, j % 4
            lat[b, :, h * 128:(h + 1) * 128] = res[j][nm][:NLAT]
            ctx[b, :, h * 128:(h + 1) * 128] = res[j][nm][NLAT:]
        outs[nm] = (lat, ctx)
    return outs


PI = float(np.pi)


def rsin(c, out, x, kf, ki):
    c.ts(kf, x, 1.0 / (2 * PI), None, ALU.mult)
    c.copy(ki, kf)
    c.copy(kf, ki)
    c.stt(kf, kf, -2 * PI, x, ALU.mult, ALU.add)
    c.ts(kf, kf, PI, -PI, ALU.min, ALU.max)
    c.act(out, kf, AF.Sin)


def build_s5():
    c = Ctx()
    cm = Common(c)
    uT = c.dram_in("uT", [2, 2, 64, NALL])
    apar = c.dram_in("apar", [128, 2, 2, 3])
    bw_d = c.dram_in("bw", [128, 2, 2, 32])
    cw_d = c.dram_in("cw", [128, 2, 2, 32])
    tidx_d = c.dram_in("tidx", [128, NALL])
    yT = c.dram_out("yT", [2, 2, 64, NALL])
    T_ = NALL
    tidx = c.sb([128, T_], F32, "tidx_sb")
    c.dma(tidx[:], tidx_d[:, :])
    ap_ = c.sb([128, 2, 2, 3], F32, "apar_sb")
    bw = c.sb([128, 2, 2, 32], F32, "bw_sb")
    cw = c.sb([128, 2, 2, 32], F32, "cw_sb")
    c.dma(ap_[:], apar[:, :, :, :])
    c.dma(bw[:], bw_d[:, :, :, :])
    c.dma(cw[:], cw_d[:, :, :, :])
    ncw = c.sb([128, 2, 32], F32, "ncw")
    c.ts(ncw[:], cw[:, :, 1, :], -1.0, None, ALU.mult)
    tabc = c.sb([128, T_], F32, "tabc")
    tabs = c.sb([128, T_], F32, "tabs")
    A1 = c.sb([128, T_], F32, "A1")
    A2 = c.sb([128, T_], F32, "A2")
    G1 = c.sb([128, T_], F32, "G1")
    G2 = c.sb([128, T_], F32, "G2")
    RT = c.sb([128, T_], F32, "RT")
    uts = [c.sb([32, 512], F32, f"ut{i}") for i in range(3)]
    KI = c.sb([128, T_], I32, "KI")
    ski = c.sb([128, 2], I32, "ski")
    sc = c.sb([128, 24], F32, "s5sc")
    pib = c.sb([128, 1], F32, "pib")
    c.memset(pib[:], PI)
    bb = c.sb([128, 2, 32], F32, "bb")
    bbT = c.sb([32, 2, 128], F32, "bbT")
    m1 = [c.sb([128, 512], F32, f"m1_{i}") for i in range(2)]
    m2 = [c.sb([128, 512], F32, f"m2_{i}") for i in range(2)]
    yo = [c.sb([32, 512], F32, f"yo{i}") for i in range(2)]
    hre = [c.sb([128, 512], F32, f"hre{i}") for i in range(2)]
    him = [c.sb([128, 512], F32, f"him{i}") for i in range(2)]
    m3 = [c.sb([128, 512], F32, f"m3_{i}") for i in range(2)]
    m4 = [c.sb([128, 512], F32, f"m4_{i}") for i in range(2)]
    psr = [c.ps([128, 512]) for _ in range(2)]
    psi = [c.ps([128, 512]) for _ in range(2)]
    psy = [c.ps([128, 512]) for _ in range(2)]
    pst = c.ps([128, 512])
    S = lambda i: sc[:, i:i + 1]
    nblk = (T_ + 511) // 512
    k = 0
    for pt in range(2):
        for d in range(2):
            a_re, a_im, ldt = ap_[:, pt, d, 0:1], ap_[:, pt, d, 1:2], ap_[:, pt, d, 2:3]
            c.act(S(0), ldt, AF.Exp)
            c.tt(S(1), a_re, S(0), ALU.mult)
            c.act(S(1), S(1), AF.Exp)
            c.tt(S(2), a_im, S(0), ALU.mult)
            rsin(c, S(6), S(2), S(8), ski[:, 0:1])
            c.ts(S(9), S(2), PI / 2, None, ALU.add)
            rsin(c, S(7), S(9), S(8), ski[:, 0:1])
            c.tt(S(10), S(1), S(7), ALU.mult)
            c.tt(S(11), S(1), S(6), ALU.mult)
            c.tt(S(12), a_re, a_re, ALU.mult)
            c.tt(S(13), a_im, a_im, ALU.mult)
            c.tt(S(12), S(12), S(13), ALU.add)
            c.emit("dve", [sc], [sc], lambda g: g.reciprocal(sc.h[:, 12:13], sc.h[:, 12:13]))
            c.ts(S(14), S(10), -1.0, None, ALU.add)
            c.tt(S(15), S(14), a_re, ALU.mult)
            c.tt(S(16), S(11), a_im, ALU.mult)
            c.tt(S(15), S(15), S(16), ALU.add)
            c.tt(S(15), S(15), S(12), ALU.mult)
            c.tt(S(16), S(11), a_re, ALU.mult)
            c.tt(S(17), S(14), a_im, ALU.mult)
            c.tt(S(16), S(16), S(17), ALU.subtract)
            c.tt(S(16), S(16), S(12), ALU.mult)
            c.ts(S(17), S(16), -1.0, None, ALU.mult)
            c.ts(bb[:, 0, :], bw[:, pt, 0, :], S(15), None, ALU.mult)
            c.stt(bb[:, 0, :], bw[:, pt, 1, :], S(17), bb[:, 0, :], ALU.mult, ALU.add)
            c.ts(bb[:, 1, :], bw[:, pt, 1, :], S(15), None, ALU.mult)
            c.stt(bb[:, 1, :], bw[:, pt, 0, :], S(16), bb[:, 1, :], ALU.mult, ALU.add)
            for ri in range(2):
                c.transpose(pst[0:32, ri * 128:(ri + 1) * 128], bb[:, ri, :], cm.ident[:])
            c.copy(bbT[:, 0, :], pst[0:32, 0:128], e="act")
            c.copy(bbT[:, 1, :], pst[0:32, 128:256], e="act")
            c.ts(A1[:], tidx[:], S(2), None, ALU.mult)
            rsin(c, tabs[:], A1[:], G1[:], KI[:])
            c.ts(A1[:], A1[:], PI / 2, None, ALU.add)
            rsin(c, tabc[:], A1[:], G1[:], KI[:])
            c.ts(RT[:], tidx[:], 0.0, S(1), ALU.mult, ALU.add)
            for b in range(2):
                for nb in range(nblk):
                    lo = nb * 512
                    w = min(512, T_ - lo)
                    pr, pi_ = psr[nb % 2], psi[nb % 2]
                    ut = uts[nb % 3]
                    c.dma(ut[:, 0:w], uT[d, b, pt * 32:(pt + 1) * 32, lo:lo + w])
                    c.mm(pr[:, 0:w], bbT[:, 0, :], ut[:, 0:w])
                    c.mm(pi_[:, 0:w], bbT[:, 1, :], ut[:, 0:w])
                    a, b_ = m1[nb % 2], m2[nb % 2]
                    cc, ss = tabc[:, lo:lo + w], tabs[:, lo:lo + w]
                    c.tt(a[:, 0:w], pr[:, 0:w], cc, ALU.mult)
                    c.tt(b_[:, 0:w], pi_[:, 0:w], ss, ALU.mult, e="pool" if False else "dve")
                    c.tt(A1[:, lo:lo + w], a[:, 0:w], b_[:, 0:w], ALU.add)
                    c.tt(a[:, 0:w], pi_[:, 0:w], cc, ALU.mult)
                    c.tt(b_[:, 0:w], pr[:, 0:w], ss, ALU.mult)
                    c.tt(A2[:, lo:lo + w], a[:, 0:w], b_[:, 0:w], ALU.subtract)
                c.emit("dve", [G1], [RT, A1], lambda g: g.tensor_tensor_scan(G1.h[:], RT.h[:], A1.h[:], 0.0, ALU.mult, ALU.add))
                c.emit("dve", [G2], [RT, A2], lambda g: g.tensor_tensor_scan(G2.h[:], RT.h[:], A2.h[:], 0.0, ALU.mult, ALU.add))
                for nb in range(nblk):
                    lo = nb * 512
                    w = min(512, T_ - lo)
                    e2 = "pool" if nb % 3 != 2 else "dve"
                    a, b_ = (m3[(nb // 3) % 2], m4[(nb // 3) % 2]) if e2 == "dve" else (m1[nb % 2], m2[nb % 2])
                    hr, hi = hre[nb % 2], him[nb % 2]
                    cc, ss = tabc[:, lo:lo + w], tabs[:, lo:lo + w]
                    c.tt(a[:, 0:w], G1[:, lo:lo + w], cc, ALU.mult, e=e2)
                    c.tt(b_[:, 0:w], G2[:, lo:lo + w], ss, ALU.mult, e=e2)
                    c.tt(hr[:, 0:w], a[:, 0:w], b_[:, 0:w], ALU.subtract, e=e2)
                    c.tt(a[:, 0:w], G1[:, lo:lo + w], ss, ALU.mult, e=e2)
                    c.tt(b_[:, 0:w], G2[:, lo:lo + w], cc, ALU.mult, e=e2)
                    c.tt(hi[:, 0:w], a[:, 0:w], b_[:, 0:w], ALU.add, e=e2)
                    py = psy[nb % 2]
                    c.mm(py[0:32, 0:w], cw[:, pt, 0, :], hr[:, 0:w], start=True, stop=False)
                    c.mm(py[0:32, 0:w], ncw[:, pt, :], hi[:, 0:w], start=False, stop=True)
                    o = yo[k % 2]
                    k += 1
                    c.copy(o[:, 0:w], py[0:32, 0:w], e="act")
                    c.dma(yT[d, b, pt * 32:(pt + 1) * 32, lo:lo + w], o[:, 0:w], q="sp")
    c.finish("sp")
    return c


def host_s5(pl, pc, P, l):
    ims = []
    tidx = rep(np.arange(NALL, dtype=np.float32))
    for j in range(NCORES):
        uT = np.zeros((2, 2, 64, NALL), np.float32)
        for b in range(2):
            ul = pl[b][:, j * 64:(j + 1) * 64]
            uc = pc[b][:, j * 64:(j + 1) * 64]
            uT[0, b] = np.concatenate([uc, ul], 0).T
            uT[1, b] = np.concatenate([uc[::-1], ul[::-1]], 0).T
        apar = np.zeros((128, 2, 2, 3), np.float32)
        bw = np.zeros((128, 2, 2, 32), np.float32)
        cw = np.zeros((128, 2, 2, 32), np.float32)
        for pt in range(2):
            for gl in range(2):
                g = j * 4 + pt * 2 + gl
                rows = slice(gl * 64, (gl + 1) * 64)
                for d, sfx in enumerate(("f", "b")):
                    apar[rows, pt, d, 0] = P["s5_a_re_" + sfx][l][g]
                    apar[rows, pt, d, 1] = P["s5_a_im_" + sfx][l][g]
                    apar[rows, pt, d, 2] = P["s5_log_dt_" + sfx][l][g]
                bw[rows, pt, 0, gl * 16:(gl + 1) * 16] = P["s5_b_re"][l][g]
                bw[rows, pt, 1, gl * 16:(gl + 1) * 16] = P["s5_b_im"][l][g]
                cw[rows, pt, 0, gl * 16:(gl + 1) * 16] = P["s5_c_re"][l][g].T
                cw[rows, pt, 1, gl * 16:(gl + 1) * 16] = P["s5_c_im"][l][g].T
        ims.append({"ident": IDENT, "uT": uT, "apar": apar, "bw": bw, "cw": cw, "tidx": tidx})
    res = run(prog("s5", build_s5), ims)
    outs = []
    for d in range(2):
        lat = np.zeros((2, NLAT, 512), np.float32)
        ctx = np.zeros((2, 256, 512), np.float32)
        for j in range(NCORES):
            for b in range(2):
                y = res[j]["yT"][d, b].T
                yc, yl = y[:256], y[256:]
                if d == 1:
                    yc, yl = yc[::-1], yl[::-1]
                lat[b, :, j * 64:(j + 1) * 64] = yl
                ctx[b, :, j * 64:(j + 1) * 64] = yc
        outs.append((lat, ctx))
    return outs


def build_merge_a():
    c = Ctx()
    cm = Common(c)
    x = c.dram_in("x", [NTOK, D])
    mv_d = c.dram_in("mv", [128, 4, KT])
    yf, yb, u = c.dram_in("yf", [NTOK, 512]), c.dram_in("yb", [NTOK, 512]), c.dram_in("u", [NTOK, 512])
    s5d = c.dram_in("s5d", [128, 512])
    wglu = c.dram_in("wglu", [512, 512])
    obr = c.dram_in("obr", [3, NTOK, 512])
    wbr = c.dram_in("wbr", [4, 512, D])
    wg = c.dram_in("wg", [D, 4 * D])
    bg = c.dram_in("bg", [1, 4 * D])
    m_out = c.dram_out("m", [NTOK, D])
    mv = load_mod_cols(c, mv_d, 4)
    c.ts(mv[:, 1, :], mv[:, 1, :], 1.0, None, ALU.add)
    c.ts(mv[:, 3, :], mv[:, 3, :], 1.0, None, ALU.add)
    dr = c.sb([128, 512], F32, "s5d_sb")
    c.dma(dr[:], s5d[:, :])
    wgl = c.sb([128, 4, 512], F32, "wglu_sb")
    c.dma(wgl[:], wglu.rearrange("(k p) n -> p k n", p=128))
    ones = c.sb([1, 128], F32, "ones1")
    c.memset(ones[:], 1.0)
    bgts = [c.sb([1, 512], F32, f"bg_sb{i}") for i in range(2)]
    TP = NT
    hT = c.sb([128, KT, TP * 128], BF16, "hT")
    oT = c.sb([128, 4, 4, TP * 128], BF16, "oT")
    macc = c.sb([128, TP, 512], F32, "macc")
    xt = c.sb([128, D], F32, "xt")
    tmp = ln_tmp(c)
    a = [c.sb([128, 512], F32, f"ma{i}") for i in range(4)]
    zT = c.sb([128, 4, 128], F32, "zT")
    pst = [c.ps([128, 512]) for _ in range(2)]
    psg = [c.ps([128, 512]) for _ in range(2)]
    psp = [c.ps([128, 512]) for _ in range(2)]
    wgb = [c.sb([128, KT, 512], BF16, f"wgb{i}") for i in range(2)]
    wbb = [c.sb([128, 4, 512], BF16, f"wbb{i}") for i in range(2)]
    gsb = [c.sb([128, 512], F32, f"gsb{i}") for i in range(2)]
    wgv = wg.rearrange("(k p) n -> p k n", p=128)
    for p0 in range(0, NT, TP):
        tiles = list(range(p0, min(NT, p0 + TP)))
        for li, t in enumerate(tiles):
            rows = slice(t * 128, (t + 1) * 128)
            c.dma(xt[:], x[rows, :])
            ln_hat(c, cm, xt[:], xt[:], tmp=tmp)
            j = 0 if t < 8 else 2
            to_fm(c, cm, xt, hT, li, pst, scale_cols=mv[:, j + 1, :], bias_cols=mv[:, j, :])
            c.dma(a[0][:], yf[rows, :])
            c.dma(a[1][:], yb[rows, :])
            c.dma(a[2][:], u[rows, :])
            c.tt(a[0][:], a[0][:], a[1][:], ALU.add)
            c.tt(a[2][:], a[2][:], dr[:], ALU.mult)
            c.tt(a[0][:], a[0][:], a[2][:], ALU.add)
            c.tt(a[1][:], a[0][:], a[0][:], ALU.mult)
            c.ts(a[1][:], a[1][:], 0.044715, 1.0, ALU.mult, ALU.add)
            c.tt(a[1][:], a[1][:], a[0][:], ALU.mult)
            c.act(a[1][:], a[1][:], AF.Tanh, scale=0.7978845608028654)
            c.stt(a[1][:], a[1][:], 1.0, a[0][:], ALU.add, ALU.mult)
            c.ts(a[1][:], a[1][:], 0.5, None, ALU.mult)
            for k in range(4):
                p = pst[k % 2]
                c.transpose(p[:, 0:128], a[1][:, k * 128:(k + 1) * 128], cm.ident[:])
                c.copy(zT[:, k, :], p[:, 0:128], e="act")
            p = psg[0]
            for k in range(4):
                c.mm(p[:, 0:512], zT[:, k, :], wgl[:, k, :], start=(k == 0), stop=(k == 3))
            c.act(a[2][:], p[:, 0:512], AF.Sigmoid)
            c.tt(a[3][:], a[1][:], a[2][:], ALU.mult)
            for k in range(4):
                p = pst[k % 2]
                c.transpose(p[:, 0:128], a[3][:, k * 128:(k + 1) * 128], cm.ident[:])
                c.copy(oT[:, 0, k, li * 128:(li + 1) * 128], p[:, 0:128], e="act")
            for br in range(3):
                c.dma(a[0][:], obr[br, rows, :])
                for k in range(4):
                    p = pst[k % 2]
                    c.transpose(p[:, 0:128], a[0][:, k * 128:(k + 1) * 128], cm.ident[:])
                    c.copy(oT[:, br + 1, k, li * 128:(li + 1) * 128], p[:, 0:128], e="act")
        i = 0
        for nb in range(4):
            for k in range(4):
                wgt, wbt = wgb[i % 2], wbb[i % 2]
                i += 1
                col = k * D + nb * 512
                bgt = bgts[i % 2]
                c.dma(bgt[:], bg[:, col:col + 512])
                c.dma(wgt[:], wgv[:, :, col:col + 512], q="pool")
                c.dma(wbt[:], wbr[k, :, nb * 512:(nb + 1) * 512].rearrange("(k p) n -> p k n", p=128), q="pool")
                for li, t in enumerate(tiles):
                    pg, pp = psg[li % 2], psp[li % 2]
                    for kk in range(KT):
                        c.mm(pg[:, 0:512], hT[:, kk, li * 128:(li + 1) * 128], wgt[:, kk, :], start=(kk == 0), stop=False)
                    c.mm(pg[:, 0:512], ones[:, :], bgt[:, :], start=False, stop=True)
                    g = gsb[li % 2]
                    c.act(g[:], pg[:, 0:512], AF.Sigmoid)
                    for kk in range(4):
                        c.mm(pp[:, 0:512], oT[:, k, kk, li * 128:(li + 1) * 128], wbt[:, kk, :], start=(kk == 0), stop=(kk == 3))
                    if k == 0:
                        c.tt(macc[:, li, :], g[:], pp[:, 0:512], ALU.mult)
                    else:
                        c.tt(g[:], g[:], pp[:, 0:512], ALU.mult)
                        c.tt(macc[:, li, :], macc[:, li, :], g[:], ALU.add)
            for li, t in enumerate(tiles):
                c.dma(m_out[t * 128:(t + 1) * 128, nb * 512:(nb + 1) * 512], macc[:, li, :], q="sp")
    c.finish("sp")
    return c


def host_merge_a(x_lat, x_ctx, mod, pl, pc, s5o, atto, P, l):
    xs = tok_shard(x_lat, x_ctx)
    (lf, cf), (lb, cb) = s5o
    yfs, ybs = tok_shard(lf, cf), tok_shard(lb, cb)
    us = tok_shard(pl[:, :, :512], pc[:, :, :512])
    brs = [tok_shard(*atto[nm]) for nm in ("o_gqa", "o_ret", "o_mla")]
    ims = []
    for j in range(NCORES):
        ims.append({"ident": IDENT, "x": xs[j], "mv": mod_cols(mod, j, [0, 1]),
                    "yf": yfs[j], "yb": ybs[j], "u": us[j], "s5d": rep(P["s5_d"][l]),
                    "wglu": P["s5_w_glu"][l], "obr": np.stack([b_[j] for b_ in brs], 0),
                    "wbr": P["w_branch"][l], "wg": P["w_gate"][l], "bg": P["b_gate"][l][None, :]})
    res = run(prog("merge_a", build_merge_a), ims)
    return tok_unshard([r["m"] for r in res], D)


ALPHA = float((2 * 2) ** 0.25)


def build_merge_b():
    c = Ctx()
    cm = Common(c)
    m = c.dram_in("m", [NTOK, D])
    x = c.dram_in("x", [NTOK, D])
    w = c.dram_in("w_out", [D, D])
    reps_d = c.dram_in("reps", [128, 8, D])
    rw_d = c.dram_in("rw", [D, 16])
    x1_o = c.dram_out("x1", [NTOK, D])
    h2_o = c.dram_out("h2", [NTOK, D])
    aff_o = c.dram_out("aff", [NTOK, 16])
    reps = c.sb([128, 8, D], F32, "reps_sb")
    for i in range(8):
        c.dma(reps[:, i, :], reps_d[:, i, :])
    c.ts(reps[:, 4, :], reps[:, 4, :], 1.0, None, ALU.add)
    c.ts(reps[:, 6, :], reps[:, 6, :], 1.0, None, ALU.add)
    rw = c.sb([128, KT, 16], F32, "rw_sb")
    c.dma(rw[:], rw_d.rearrange("(k p) n -> p k n", p=128))
    TP = 5
    mt = c.sb([128, D], F32, "mt")
    X = c.sb([128, TP, D], F32, "X")
    xh = c.sb([128, D], F32, "xh")
    x1t = c.sb([128, D], F32, "x1t")
    mT = c.sb([128, KT, TP * 128], BF16, "mT")
    rT = c.sb([128, KT, 128], F32, "rT")
    tmp = ln_tmp(c)
    tb = [c.sb([128, 512], F32, f"tb{i}") for i in range(2)]
    sm = c.sb([128, 40], F32, "sm")
    wbufs = [c.sb([128, KT, 512], BF16, f"wb{i}") for i in range(2)]
    pst = [c.ps([128, 512]) for _ in range(2)]
    pso = [c.ps([128, 512]) for _ in range(2)]
    psr = c.ps([128, 512])
    for p0 in range(0, NT, TP):
        tiles = list(range(p0, min(NT, p0 + TP)))
        for li, t in enumerate(tiles):
            rows = slice(t * 128, (t + 1) * 128)
            c.dma(mt[:], m[rows, :])
            c.dma(X[:, li, :], x[rows, :])
            to_fm(c, cm, mt, mT, li, pst)

        def consume(li, nb, lo, bw, p, tiles=tiles):
            g1 = reps[:, 0 if tiles[li] < 8 else 1, :]
            b_ = tb[(li + nb) % 2]
            c.tt(b_[:, 0:bw], p[:, 0:bw], g1[:, lo:lo + bw], ALU.mult)
            c.stt(X[:, li, lo:lo + bw], X[:, li, lo:lo + bw], ALU_ALPHA, b_[:, 0:bw], ALU.mult, ALU.add)

        stream_linear(c, mT, w, D, len(tiles), consume, wbufs, pso)
        for li, t in enumerate(tiles):
            rows = slice(t * 128, (t + 1) * 128)
            lat = t < 8
            ln_hat(c, cm, X[:, li, :], xh[:], tmp=tmp)
            c.tt(xh[:], xh[:], reps[:, 2, :], ALU.mult)
            c.tt(x1t[:], xh[:], reps[:, 3, :], ALU.add)
            c.dma(x1_o[rows, :], x1t[:], q="sp")
            ln_hat(c, cm, x1t[:], xh[:], tmp=tmp)
            c.tt(xh[:], xh[:], reps[:, 4 if lat else 6, :], ALU.mult)
            c.tt(mt[:], xh[:], reps[:, 5 if lat else 7, :], ALU.add)
            c.dma(h2_o[rows, :], mt[:], q="sp")
            to_fm(c, cm, mt, rT, 0, pst)
            for k in range(KT):
                c.mm(psr[:, 0:16], rT[:, k, :], rw[:, k, :], start=(k == 0), stop=(k == KT - 1))
            c.copy(sm[:, 0:16], psr[:, 0:16])
            c.reduce(sm[:, 32:33], sm[:, 0:16], ALU.max)
            c.ts(sm[:, 32:33], sm[:, 32:33], -1.0, None, ALU.mult)
            c.act(sm[:, 0:16], sm[:, 0:16], AF.Exp, bias=sm[:, 32:33], scale=1.0)
            c.reduce(sm[:, 33:34], sm[:, 0:16], ALU.add)
            c.emit("dve", [sm], [sm], lambda g: g.reciprocal(sm.h[:, 34:35], sm.h[:, 33:34]))
            c.ts(sm[:, 16:32], sm[:, 0:16], sm[:, 34:35], None, ALU.mult)
            c.dma(aff_o[rows, :], sm[:, 16:32], q="sp")
    c.finish("sp")
    return c


ALU_ALPHA = ALPHA


def host_merge_b(m_lat, m_ctx, x_lat, x_ctx, mod, P, l):
    ms = tok_shard(m_lat, m_ctx)
    xs = tok_shard(x_lat, x_ctx)
    ims = []
    for j in range(NCORES):
        b = j // 4
        seg = lambda r_, i: mod[r_, i * D:(i + 1) * D]
        reps = np.stack([rep(seg(b, 2)), rep(seg(2, 2)), rep(P["ln1_g"][l]), rep(P["ln1_b"][l]),
                         rep(seg(b, 4)), rep(seg(b, 3)), rep(seg(2, 4)), rep(seg(2, 3))], 1)
        ims.append({"ident": IDENT, "m": ms[j], "x": xs[j], "w_out": P["w_out"][l],
                    "reps": np.ascontiguousarray(reps), "rw": P["router_w"][l]})
    res = run(prog("merge_b", build_merge_b), ims)
    return (tok_unshard([r["x1"] for r in res], D), tok_unshard([r["h2"] for r in res], D),
            tok_unshard([r["aff"] for r in res], 16))


CAP_L, CAP_C = 512, 32


def build_topk():
    c = Ctx()
    a_l = c.dram_in("affT", [32, NLAT])
    a_c = c.dram_in("affcT", [32, 256])
    m_l, m_c = c.dram_out("markl", [32, NLAT]), c.dram_out("markc", [32, 256])
    for (src, n, cap, mo, tag) in ((a_l, NLAT, CAP_L, m_l, "l"), (a_c, 256, CAP_C, m_c, "c")):
        w = c.sb([32, n], F32, "work" + tag)
        gv = [c.sb([32, 8], F32, f"gv{tag}{i}") for i in range(2)]
        c.dma(w[:], src[:, :])
        for r in range(cap // 8):
            g_ = gv[r % 2]
            c.emit("dve", [g_], [w], lambda g, g_=g_, w=w: g.max(out=g_.h[:], in_=w.h[:]))
            c.emit("dve", [w], [g_, w], lambda g, g_=g_, w=w: g.match_replace(out=w.h[:], in_to_replace=g_.h[:], in_values=w.h[:], imm_value=-1.0))
        c.dma(mo[:, :], w[:], q="pool")
    c.finish("pool")
    return c


def host_topk(aff_l, aff_c):
    affT = np.ascontiguousarray(aff_l.transpose(0, 2, 1).reshape(32, NLAT))
    affcT = np.ascontiguousarray(aff_c.transpose(0, 2, 1).reshape(32, 256))
    res = run(prog("topk", build_topk), [{"affT": affT, "affcT": affcT}] * NCORES)[0]

    def pick(mark, src, cap):
        idx = np.zeros((32, cap), np.int64)
        for r_ in range(32):
            sel = np.flatnonzero(mark[r_] == -1.0)
            assert sel.size == cap, (r_, sel.size)
            idx[r_] = sel
        return np.take_along_axis(src, idx, 1), idx

    gate, idx = pick(res["markl"], affT, CAP_L)
    gatec, idxc = pick(res["markc"], affcT, CAP_C)
    return (gate.reshape(2, 16, CAP_L), idx.reshape(2, 16, CAP_L),
            gatec.reshape(2, 16, CAP_C), idxc.reshape(2, 16, CAP_C))


FF = 1024
ER = NTOK


def build_expert():
    c = Ctx()
    cm = Common(c)
    xs = c.dram_in("xs", [2, ER, D])
    gt_d = c.dram_in("gt", [128, 2, NT])
    wg_d, wu_d, wd_d = c.dram_in("wg", [2, D, FF]), c.dram_in("wu", [2, D, FF]), c.dram_in("wd", [2, FF, D])
    y = c.dram_out("y", [2, ER, D])
    gt = c.sb([128, 2, NT], F32, "gt_sb")
    c.dma(gt[:], gt_d[:, :, :])
    xT = c.sb([128, KT, ER], BF16, "xT")
    hT = c.sb([128, 8, ER], BF16, "hmT")
    xt = [c.sb([128, D], F32, f"xt{i}") for i in range(2)]
    wgb = [c.sb([128, KT, 128], BF16, f"wgb{i}") for i in range(2)]
    wub = [c.sb([128, KT, 128], BF16, f"wub{i}") for i in range(2)]
    wdb = [c.sb([128, 8, 512], BF16, f"wdb{i}") for i in range(2)]
    sg = [c.sb([128, 512], F32, f"sg{i}") for i in range(2)]
    ob = [c.sb([128, 512], F32, f"ob{i}") for i in range(3)]
    pst = [c.ps([128, 512]) for _ in range(2)]
    psa = [c.ps([128, 512]) for _ in range(2)]
    psu = [c.ps([128, 512]) for _ in range(2)]
    pso = [c.ps([128, 512]) for _ in range(2)]
    chunks = [(0, 512), (512, 512), (1024, 128)]
    k0 = 0
    for e in range(2):
        for t in range(NT):
            x_ = xt[t % 2]
            c.dma(x_[:], xs[e, t * 128:(t + 1) * 128, :])
            to_fm(c, cm, x_, xT, t, pst)
        for fb in range(FF // 128):
            wg_, wu_ = wgb[fb % 2], wub[fb % 2]
            c.dma(wg_[:], wg_d[e, :, fb * 128:(fb + 1) * 128].rearrange("(k p) n -> p k n", p=128), q="pool")
            c.dma(wu_[:], wu_d[e, :, fb * 128:(fb + 1) * 128].rearrange("(k p) n -> p k n", p=128), q="pool")
            for ci, (lo, w) in enumerate(chunks):
                pa, pu = psa[ci % 2], psu[ci % 2]
                for k in range(KT):
                    c.mm(pa[:, 0:w], wg_[:, k, :], xT[:, k, lo:lo + w], start=(k == 0), stop=(k == KT - 1))
                for k in range(KT):
                    c.mm(pu[:, 0:w], wu_[:, k, :], xT[:, k, lo:lo + w], start=(k == 0), stop=(k == KT - 1))
                s_ = sg[ci % 2]
                c.act(s_[:, 0:w], pa[:, 0:w], AF.Sigmoid)
                c.tt(s_[:, 0:w], s_[:, 0:w], pa[:, 0:w], ALU.mult)
                c.tt(hT[:, fb, lo:lo + w], s_[:, 0:w], pu[:, 0:w], ALU.mult)
        for nb in range(D // 512):
            wd_ = wdb[nb % 2]
            c.dma(wd_[:], wd_d[e, :, nb * 512:(nb + 1) * 512].rearrange("(k p) n -> p k n", p=128), q="pool")
            for t in range(NT):
                p = pso[t % 2]
                for k in range(8):
                    c.mm(p[:, 0:512], hT[:, k, t * 128:(t + 1) * 128], wd_[:, k, :], start=(k == 0), stop=(k == 7))
                o = ob[k0 % 3]
                k0 += 1
                c.ts(o[:], p[:, 0:512], gt[:, e, t:t + 1], None, ALU.mult)
                c.dma(y[e, t * 128:(t + 1) * 128, nb * 512:(nb + 1) * 512], o[:], q="sp")
    c.finish("sp")
    return c


def host_expert(h2l, h2c, gate, idx, gatec, idxc, P, l):
    ims = []
    for j in range(NCORES):
        xs = np.zeros((2, ER, D), np.float32)
        gt = np.zeros((2, ER), np.float32)
        for ei in range(2):
            e = 2 * j + ei
            for b in range(2):
                xs[ei, b * 512:(b + 1) * 512] = h2l[b][idx[b, e]]
                gt[ei, b * 512:(b + 1) * 512] = gate[b, e]
                xs[ei, 1024 + b * 32:1024 + (b + 1) * 32] = h2c[b][idxc[b, e]]
                gt[ei, 1024 + b * 32:1024 + (b + 1) * 32] = gatec[b, e]
        gtp = np.ascontiguousarray(gt.reshape(2, NT, 128).transpose(2, 0, 1))
        ims.append({"ident": IDENT, "xs": xs, "gt": gtp,
                    "wg": P["moe_w_gate"][l][2 * j:2 * j + 2], "wu": P["moe_w_up"][l][2 * j:2 * j + 2],
                    "wd": P["moe_w_down"][l][2 * j:2 * j + 2]})
    res = run(prog("expert", build_expert), ims)
    Y = np.stack([res[j]["y"] for j in range(NCORES)], 0).reshape(16, ER, D)
    return Y


NSL = 16 * CAP_L
NSC = 16 * CAP_C


def build_combine():
    c = Ctx()
    yl = c.dram_in("yl", [NSL, D])
    yc = c.dram_in("yc", [NSC, D])
    il_d = c.dram_in("il", [128, NSL // 128], I32)
    ic_d = c.dram_in("ic", [128, NSC // 128], I32)
    tok_d = c.dram_in("tok", [128, NTOK])
    x1 = c.dram_in("x1", [NTOK, D])
    reps_d = c.dram_in("reps", [128, 4, D])
    out = c.dram_out("x2", [NTOK, D])
    reps = c.sb([128, 4, D], F32, "reps_sb")
    for i in range(4):
        c.dma(reps[:, i, :], reps_d[:, i, :])
    tok = c.sb([128, NTOK], F32, "tok_sb")
    c.dma(tok[:], tok_d[:, :])
    ili = c.sb([128, NSL // 128], I32, "ili")
    ici = c.sb([128, NSC // 128], I32, "ici")
    c.dma(ili[:], il_d[:, :])
    c.dma(ici[:], ic_d[:, :])
    il = c.sb([128, NSL // 128], F32, "il_f")
    ic = c.sb([128, NSC // 128], F32, "ic_f")
    c.copy(il[:], ili[:])
    c.copy(ic[:], ici[:])
    X = c.sb([128, NT, D], F32, "X")
    for t in range(NT):
        c.dma(X[:, t, :], x1[t * 128:(t + 1) * 128, :])
    yb = [c.sb([128, 8, 512], BF16, f"yb{i}") for i in range(3)]
    sb_ = [c.sb([128, 1024], BF16, f"sel{i}") for i in range(3)]
    tb = [c.sb([128, 512], F32, f"tb{i}") for i in range(2)]
    acc = [c.ps([128, 512]) for _ in range(8)]
    xh = c.sb([128, D], F32, "xh")
    tmp = ln_tmp(c)
    si = 0
    for nb in range(4):
        cols = slice(nb * 512, (nb + 1) * 512)
        nkt = NSL // 128
        for kt in range(nkt):
            yg = yb[(kt // 8) % 3]
            if kt % 8 == 0:
                c.dma(yg[:], yl[kt * 128:(kt + 8) * 128, cols].rearrange("(k p) n -> p k n", p=128), q="pool")
            y_ = yg[:, kt % 8, :]
            s_ = sb_[si % 3]
            si += 1
            c.ts(s_[:], tok[:, 0:1024], il[:, kt:kt + 1], None, ALU.is_equal)
            for t in range(8):
                c.mm(acc[t][:, 0:512], s_[:, t * 128:(t + 1) * 128], y_, start=(kt == 0), stop=(kt == nkt - 1))
        for t in range(8):
            b_ = tb[t % 2]
            c.tt(b_[:], acc[t][:, 0:512], reps[:, 0, cols], ALU.mult)
            c.stt(X[:, t, cols], X[:, t, cols], ALPHA, b_[:], ALU.mult, ALU.add)
        nkc = NSC // 128
        ygc = yb[nb % 3]
        c.dma(ygc[:, 0:nkc, :], yc[:, cols].rearrange("(k p) n -> p k n", p=128), q="pool")
        for kt in range(nkc):
            y_ = ygc[:, kt, :]
            s_ = sb_[si % 3]
            si += 1
            c.ts(s_[:, 0:128], tok[:, 1024:1152], ic[:, kt:kt + 1], None, ALU.is_equal)
            c.mm(acc[0][:, 0:512], s_[:, 0:128], y_, start=(kt == 0), stop=(kt == nkc - 1))
        b_ = tb[0]
        c.tt(b_[:], acc[0][:, 0:512], reps[:, 1, cols], ALU.mult)
        c.stt(X[:, 8, cols], X[:, 8, cols], ALPHA, b_[:], ALU.mult, ALU.add)
    for t in range(NT):
        ln_hat(c, cm_none, X[:, t, :], xh[:], tmp=tmp)
        c.tt(xh[:], xh[:], reps[:, 2, :], ALU.mult)
        c.tt(X[:, t, :], xh[:], reps[:, 3, :], ALU.add)
        c.dma(out[t * 128:(t + 1) * 128, :], X[:, t, :], q="sp")
    c.finish("sp")
    return c


cm_none = None


def host_combine(Y, idx, idxc, x1l, x1c, mod, P, l):
    x1s = tok_shard(x1l, x1c)
    ims = []
    for j in range(NCORES):
        b, q = j // 4, j % 4
        yl = np.ascontiguousarray(Y[:, b * 512:(b + 1) * 512, :].reshape(NSL, D))
        il = idx[b].reshape(NSL).astype(np.int32)
        tok = np.full((NTOK,), -5.0, np.float32)
        tok[:1024] = q * 1024 + np.arange(1024)
        if j < 4:
            cb = j // 2
            yc = np.ascontiguousarray(Y[:, 1024 + cb * 32:1024 + (cb + 1) * 32, :].reshape(NSC, D))
            ic = idxc[cb].reshape(NSC).astype(np.int32)
            tok[1024:] = (j % 2) * 128 + np.arange(128)
        else:
            yc = np.zeros((NSC, D), np.float32)
            ic = np.full((NSC,), -7, np.int32)
        seg = lambda r_, i: mod[r_, i * D:(i + 1) * D]
        reps = np.stack([rep(seg(b, 5)), rep(seg(2, 5)), rep(P["ln2_g"][l]), rep(P["ln2_b"][l])], 1)
        ims.append({"yl": yl, "yc": yc, "il": np.ascontiguousarray(il.reshape(-1, 128).T),
                    "ic": np.ascontiguousarray(ic.reshape(-1, 128).T), "tok": rep(tok),
                    "x1": x1s[j], "reps": np.ascontiguousarray(reps)})
    res = run(prog("combine", build_combine), ims)
    return tok_unshard([r["x2"] for r in res], D)


def kernel(**inp):
    P = {k: np.asarray(v) for k, v in inp.items()}
    x_lat = np.asarray(P["x"], np.float32)
    x_ctx = np.asarray(P["ctx"], np.float32)
    mods = host_mod(P["c"], P["c_ctx"], P["ada_w"], P["ada_b"])
    for l in range(2):
        mod = mods[l]
        pl, pc = host_proj(x_lat, x_ctx, mod, P["w_in"][l])
        s5o = host_s5(pl, pc, P, l)
        atto = host_att(pl, pc, P, l)
        ml, mc = host_merge_a(x_lat, x_ctx, mod, pl, pc, s5o, atto, P, l)
        (x1l, x1c), (h2l, h2c), (al, ac) = host_merge_b(ml, mc, x_lat, x_ctx, mod, P, l)
        gate, idx, gatec, idxc = host_topk(al, ac)
        Y = host_expert(h2l, h2c, gate, idx, gatec, idxc, P, l)
        x_lat, x_ctx = host_combine(Y, idx, idxc, x1l, x1c, mod, P, l)
    return np.ascontiguousarray(x_lat, dtype=np.float32)
```
